# Optimizing a Trainium2 kernel written in Bass

```python
import jax, jax.numpy as jnp
from jax import lax
import numpy as np

D_MODEL = 1024
BATCH = 4
SEQ = 4096
DEPTH = 1

N_META = 16
BLOCK = 128
PAD_FRONT = BLOCK - N_META
N_Q_HEADS = 8
N_KV_HEADS = 2
HEAD_DIM = 64
WINDOW = 128
ROPE_DIM = HEAD_DIM // 4
ROPE_THETA = 500000.0
N_R_HEADS = 4
R_KEY_DIM = 128
R_VAL_DIM = 128
CHUNK = 64
SUB = 16
ATTN_WIDTH = N_Q_HEADS * HEAD_DIM
KV_WIDTH = N_KV_HEADS * HEAD_DIM
REC_K_WIDTH = N_R_HEADS * R_KEY_DIM
REC_V_WIDTH = N_R_HEADS * R_VAL_DIM
SPLIT_WIDTHS = (ATTN_WIDTH, KV_WIDTH, KV_WIDTH, REC_K_WIDTH, REC_K_WIDTH, REC_V_WIDTH, REC_V_WIDTH, D_MODEL, D_MODEL)
IN_WIDTH = sum(SPLIT_WIDTHS)
N_SUBKEYS = 128
N_EXPERTS = N_SUBKEYS * N_SUBKEYS
PEER_HEADS = 8
PEER_QUERY_DIM = 128
PEER_TOPK = 16
PEER_BLOCK = 256
EPS = 1e-6
MASK_VALUE = -1e30

kernel_name = 'hybrid_swa_hgrn2_peer_block'


def rmsnorm(x, g):
    xf = x.astype(jnp.float32)
    y = xf * lax.rsqrt(jnp.mean(xf * xf, axis=-1, keepdims=True) + EPS)
    return (y * g.astype(jnp.float32)).astype(x.dtype)


def apply_partial_rope(t, pos):
    half = ROPE_DIM // 2
    inv_freq = jnp.power(ROPE_THETA, -jnp.arange(half, dtype=jnp.float32) * 2.0 / ROPE_DIM)
    ang = pos.astype(jnp.float32)[:, None] * inv_freq[None, :]
    cos = jnp.cos(ang)[None, :, None, :]
    sin = jnp.sin(ang)[None, :, None, :]
    tf = t.astype(jnp.float32)
    t1, t2 = tf[..., :half], tf[..., half:ROPE_DIM]
    out = jnp.concatenate([t1 * cos - t2 * sin, t2 * cos + t1 * sin, tf[..., ROPE_DIM:]], axis=-1)
    return out.astype(t.dtype)


def sliding_window_attention(q, k, v, sinks):
    B, Lp = q.shape[0], q.shape[1]
    nb = Lp // BLOCK
    G = N_Q_HEADS // N_KV_HEADS
    qb = q.astype(jnp.float32).reshape(B, nb, BLOCK, N_KV_HEADS, G, HEAD_DIM)
    kb = k.astype(jnp.float32).reshape(B, nb, BLOCK, N_KV_HEADS, HEAD_DIM)
    vb = v.astype(jnp.float32).reshape(B, nb, BLOCK, N_KV_HEADS, HEAD_DIM)
    k_band = jnp.concatenate([jnp.concatenate([jnp.zeros_like(kb[:, :1]), kb[:, :-1]], axis=1), kb], axis=2)
    v_band = jnp.concatenate([jnp.concatenate([jnp.zeros_like(vb[:, :1]), vb[:, :-1]], axis=1), vb], axis=2)
    meta_k = k[:, PAD_FRONT:BLOCK].astype(jnp.float32)
    meta_v = v[:, PAD_FRONT:BLOCK].astype(jnp.float32)
    scale = HEAD_DIM ** -0.5
    s_band = jnp.einsum('bnqhgd,bnkhd->bnhgqk', qb, k_band) * scale
    s_meta = jnp.einsum('bnqhgd,bmhd->bnhgqm', qb, meta_k) * scale
    p_q = jnp.arange(Lp).reshape(nb, BLOCK)
    p_k = p_q[:, :1] - BLOCK + jnp.arange(2 * BLOCK)[None, :]
    dq = p_q[:, :, None] - p_k[:, None, :]
    band_ok = (dq >= 0) & (dq < WINDOW) & (p_k[:, None, :] >= BLOCK)
    meta_ok = (PAD_FRONT + jnp.arange(N_META))[None, None, :] <= p_q[:, :, None]
    s_band = jnp.where(band_ok[None, :, None, None], s_band, MASK_VALUE)
    s_meta = jnp.where(meta_ok[None, :, None, None], s_meta, MASK_VALUE)
    sink = jnp.broadcast_to(sinks.astype(jnp.float32).reshape(1, 1, N_KV_HEADS, G, 1, 1), s_band.shape[:-1] + (1,))
    probs = jax.nn.softmax(jnp.concatenate([s_band, s_meta, sink], axis=-1), axis=-1)
    p_band = probs[..., :2 * BLOCK]
    p_meta = probs[..., 2 * BLOCK:2 * BLOCK + N_META]
    out = (jnp.einsum('bnhgqk,bnkhd->bnqhgd', p_band, v_band)
           + jnp.einsum('bnhgqm,bmhd->bnqhgd', p_meta, meta_v))
    return out.reshape(B, Lp, N_Q_HEADS * HEAD_DIM)


def hgrn2_chunkwise(q, k, v, log_f):
    B, Lp, H, dk = q.shape
    dv = v.shape[-1]
    nc = Lp // CHUNK
    ns = CHUNK // SUB

    def to_chunks(t):
        return t.astype(jnp.float32).reshape(B, nc, CHUNK, H, t.shape[-1]).transpose(0, 3, 1, 2, 4)

    qc, kc, vc, lf = to_chunks(q), to_chunks(k), to_chunks(v), to_chunks(log_f)
    b = jnp.cumsum(lf, axis=3)
    b_last = b[:, :, :, -1]
    dS = jnp.einsum('bhncd,bhnce->bhnde', kc * jnp.exp(b_last[:, :, :, None] - b), vc)
    decay = jnp.exp(b_last)

    def step(S, inp):
        a, u = inp
        return a[..., None] * S + u, S

    _, S_prev = lax.scan(step, jnp.zeros((B, H, dk, dv), jnp.float32),
                         (decay.transpose(2, 0, 1, 3), dS.transpose(2, 0, 1, 3, 4)))
    S_prev = S_prev.transpose(1, 2, 0, 3, 4)
    o_inter = jnp.einsum('bhncd,bhnde->bhnce', qc * jnp.exp(b), S_prev)
    qs = qc.reshape(B, H, nc, ns, SUB, dk)
    ks = kc.reshape(B, H, nc, ns, SUB, dk)
    vs = vc.reshape(B, H, nc, ns, SUB, dv)
    bs = b.reshape(B, H, nc, ns, SUB, dk)
    b_ref = jnp.concatenate([jnp.zeros_like(bs[:, :, :, :1, 0]), bs[:, :, :, :-1, -1]], axis=3)
    q_t = qs * jnp.exp(bs - b_ref[:, :, :, :, None, :])
    off_mask = jnp.arange(ns)[None, :] < jnp.arange(ns)[:, None]
    expo_off = b_ref[:, :, :, :, None, None, :] - bs[:, :, :, None]
    k_t = ks[:, :, :, None] * jnp.exp(jnp.where(off_mask[:, :, None, None], expo_off, MASK_VALUE))
    a_off = jnp.einsum('bhnitd,bhnijsd->bhnitjs', q_t, k_t)
    o_off = jnp.einsum('bhnitjs,bhnjse->bhnite', a_off, vs)
    causal = jnp.tril(jnp.ones((SUB, SUB), dtype=bool))
    expo_diag = bs[..., :, None, :] - bs[..., None, :, :]
    dec_diag = jnp.exp(jnp.where(causal[:, :, None], expo_diag, MASK_VALUE))
    a_diag = jnp.einsum('bhnitd,bhnisd,bhnitsd->bhnits', qs, ks, dec_diag)
    o_diag = jnp.einsum('bhnits,bhnise->bhnite', a_diag, vs)
    o = o_inter + (o_off + o_diag).reshape(B, H, nc, CHUNK, dv)
    return o.transpose(0, 2, 3, 1, 4).reshape(B, Lp, H, dv)


def hybrid_mixer(xn, w_in, b_in, sinks, lower_bound, rec_norm_g, w_up_attn, w_up_rec, w_out, pos):
    B, L, _ = xn.shape
    Lp = L + PAD_FRONT
    xp = jnp.pad(xn, ((0, 0), (PAD_FRONT, 0), (0, 0)))
    proj = xp @ w_in + b_in
    q, k, v, rq, rf, ri, rg, ga, gr = jnp.split(proj, np.cumsum(SPLIT_WIDTHS)[:-1].tolist(), axis=-1)
    q = apply_partial_rope(q.reshape(B, Lp, N_Q_HEADS, HEAD_DIM), pos)
    k = apply_partial_rope(k.reshape(B, Lp, N_KV_HEADS, HEAD_DIM), pos)
    v = v.reshape(B, Lp, N_KV_HEADS, HEAD_DIM)
    attn_out = sliding_window_attention(q, k, v, sinks)[:, PAD_FRONT:].astype(xn.dtype)
    lb = lower_bound.reshape(N_R_HEADS, R_KEY_DIM)
    valid = (jnp.arange(Lp) >= PAD_FRONT)[None, :, None, None]
    z_f = rf.reshape(B, Lp, N_R_HEADS, R_KEY_DIM).astype(jnp.float32)
    log_f = jnp.logaddexp(jnp.log(lb), jnp.log1p(-lb) + jax.nn.log_sigmoid(z_f))
    log_f = jnp.where(valid, log_f, 0.0)
    k_r = -jnp.expm1(log_f)
    o_r = hgrn2_chunkwise(rq.reshape(B, Lp, N_R_HEADS, R_KEY_DIM), k_r,
                          ri.reshape(B, Lp, N_R_HEADS, R_VAL_DIM), log_f)[:, PAD_FRONT:]
    o_r = o_r * lax.rsqrt(jnp.mean(o_r * o_r, axis=-1, keepdims=True) + EPS)
    o_r = o_r * rec_norm_g.astype(jnp.float32).reshape(N_R_HEADS, R_VAL_DIM)
    o_r = o_r * jax.nn.silu(rg[:, PAD_FRONT:].astype(jnp.float32).reshape(B, L, N_R_HEADS, R_VAL_DIM))
    rec_out = o_r.reshape(B, L, REC_V_WIDTH).astype(xn.dtype)
    merged = (jax.nn.sigmoid(ga[:, PAD_FRONT:]) * (attn_out @ w_up_attn)
              + jax.nn.sigmoid(gr[:, PAD_FRONT:]) * (rec_out @ w_up_rec))
    return merged @ w_out


def peer_ffn(x, w_query, sub_keys, expert_down, expert_up):
    T = x.shape[0]
    T_pad = -(-T // PEER_BLOCK) * PEER_BLOCK
    xp = jnp.pad(x, ((0, T_pad - T), (0, 0)))
    half = PEER_QUERY_DIM // 2
    qry = (xp @ w_query).astype(jnp.float32).reshape(T_pad, PEER_HEADS, 2, half)
    scores = jnp.einsum('thpd,hpnd->thpn', qry, sub_keys.astype(jnp.float32))
    s_top, i_top = lax.top_k(scores, PEER_TOPK)
    cand = s_top[:, :, 0, :, None] + s_top[:, :, 1, None, :]
    cand_idx = i_top[:, :, 0, :, None] * N_SUBKEYS + i_top[:, :, 1, None, :]
    best, sel = lax.top_k(cand.reshape(T_pad, PEER_HEADS, PEER_TOPK * PEER_TOPK), PEER_TOPK)
    idx = jnp.take_along_axis(cand_idx.reshape(T_pad, PEER_HEADS, PEER_TOPK * PEER_TOPK), sel, axis=-1)
    gate = jax.nn.softmax(best, axis=-1)
    nblk = T_pad // PEER_BLOCK

    def apply_block(args):
        xb, ib, wb = args
        u = expert_down[ib]
        act = jax.nn.gelu(jnp.einsum('td,thkd->thk', xb, u).astype(jnp.float32), approximate=False) * wb
        return jnp.einsum('thk,thkd->td', act.astype(xb.dtype), expert_up[ib])

    y = lax.map(apply_block, (xp.reshape(nblk, PEER_BLOCK, D_MODEL),
                              idx.reshape(nblk, PEER_BLOCK, PEER_HEADS, PEER_TOPK),
                              gate.reshape(nblk, PEER_BLOCK, PEER_HEADS, PEER_TOPK)))
    return y.reshape(T_pad, D_MODEL)[:T]


def setup_inputs(seed: int = 0) -> dict:
    key = jax.random.key(seed)
    ks = jax.random.split(key, 17)

    def normal(k, shape, scale):
        return jax.random.normal(k, shape, jnp.float32) * scale

    return {
        'x': normal(ks[0], (BATCH, SEQ, D_MODEL), 1.0),
        'meta_tokens': normal(ks[1], (N_META, D_MODEL), 1.0),
        'norm_mix_g': 1.0 + normal(ks[2], (DEPTH, D_MODEL), 0.02),
        'w_in': normal(ks[3], (DEPTH, D_MODEL, IN_WIDTH), D_MODEL ** -0.5),
        'b_in': normal(ks[4], (DEPTH, IN_WIDTH), 0.02),
        'attn_sinks': normal(ks[5], (DEPTH, N_Q_HEADS), 0.5),
        'lb_logits': normal(ks[6], (DEPTH + 1, REC_K_WIDTH), 0.5),
        'rec_norm_g': 1.0 + normal(ks[7], (DEPTH, REC_V_WIDTH), 0.02),
        'w_up_attn': normal(ks[8], (DEPTH, ATTN_WIDTH, D_MODEL), ATTN_WIDTH ** -0.5),
        'w_up_rec': normal(ks[9], (DEPTH, REC_V_WIDTH, D_MODEL), REC_V_WIDTH ** -0.5),
        'w_out': normal(ks[10], (DEPTH, D_MODEL, D_MODEL), D_MODEL ** -0.5),
        'norm_ffn_g': 1.0 + normal(ks[11], (DEPTH, D_MODEL), 0.02),
        'peer_w_query': normal(ks[12], (DEPTH, D_MODEL, PEER_HEADS * PEER_QUERY_DIM), D_MODEL ** -0.5),
        'peer_sub_keys': normal(ks[13], (DEPTH, PEER_HEADS, 2, N_SUBKEYS, PEER_QUERY_DIM // 2), (PEER_QUERY_DIM // 2) ** -0.5),
        'peer_expert_down': normal(ks[14], (DEPTH, N_EXPERTS, D_MODEL), D_MODEL ** -0.5),
        'peer_expert_up': normal(ks[15], (DEPTH, N_EXPERTS, D_MODEL), PEER_HEADS ** -0.5),
        'final_norm_g': 1.0 + normal(ks[16], (D_MODEL,), 0.02),
    }


def reference(x, meta_tokens, norm_mix_g, w_in, b_in, attn_sinks, lb_logits, rec_norm_g, w_up_attn, w_up_rec,
              w_out, norm_ffn_g, peer_w_query, peer_sub_keys, peer_expert_down, peer_expert_up, final_norm_g):
    B = x.shape[0]
    h = jnp.concatenate([jnp.broadcast_to(meta_tokens[None].astype(x.dtype), (B, N_META, D_MODEL)), x], axis=1)
    pos = jnp.arange(PAD_FRONT + h.shape[1]) - PAD_FRONT
    lower_bounds = jnp.cumsum(jax.nn.softmax(lb_logits.astype(jnp.float32), axis=0), axis=0)
    for layer in range(DEPTH):
        h = h + hybrid_mixer(rmsnorm(h, norm_mix_g[layer]), w_in[layer], b_in[layer], attn_sinks[layer],
                             lower_bounds[layer], rec_norm_g[layer], w_up_attn[layer], w_up_rec[layer],
                             w_out[layer], pos)
        if layer == DEPTH - 1:
            h = h[:, N_META:]
        L = h.shape[1]
        y = peer_ffn(rmsnorm(h, norm_ffn_g[layer]).reshape(B * L, D_MODEL), peer_w_query[layer],
                     peer_sub_keys[layer], peer_expert_down[layer], peer_expert_up[layer])
        h = h + y.reshape(B, L, D_MODEL)
    return rmsnorm(h, final_norm_g)
```

```python
import numpy as np
import concourse.bass as bass
import concourse.mybir as mybir
from concourse.bass_utils import run_bass_kernel_spmd
from contextlib import ExitStack

F32 = mybir.dt.float32
BF16 = mybir.dt.bfloat16
AF = mybir.ActivationFunctionType
ALU = mybir.AluOpType
AX = mybir.AxisListType

ENGS = ("sp", "act", "dve", "pool", "pe")
NDMA_SEM = 6

D = 1024
NCORES = 8
NBLK = 17
NPRE = 16
EPS = 1e-6
NEG = -1e30
CG = [(0, 512), (512, 256), (768, 512), (1280, 512), (1792, 512), (2304, 512),
      (2816, 512), (3328, 512), (3840, 512), (4352, 512)]
G_Q, G_KV, G_RQ, G_RF, G_RI, G_RG, G_GA0, G_GA1, G_GR0, G_GR1 = range(10)
P_WUA, P_WUR, P_WO = 10, 12, 14
NPIECE = 16
NCGRP = 32
TH = 1024


class Prog:
    def __init__(self, nc):
        self.nc = nc
        self.plan = {e: [] for e in ENGS}
        self.count = {e: 0 for e in ENGS}
        self.seen = {e: {} for e in ENGS}
        self.lastw = {}
        self.readers = {}
        self.dma_n = {e: 0 for e in ENGS}
        self.ninst = 0

    def _need(self, eng, waits, tok, same_ok=False):
        key, val, peng = tok
        if peng == eng and (same_ok or eng == "pe"):
            return
        if self.seen[eng].get(key, 0) >= val:
            return
        self.seen[eng][key] = val
        waits.append((key, val))

    def _deps(self, eng, reads, writes):
        waits = []
        for b in reads:
            if b in self.lastw:
                self._need(eng, waits, self.lastw[b])
            if b[0] == "B" and b[1:].isdigit():
                for tok in self.readers.get(b, ()):
                    self._need(eng, waits, tok, same_ok=True)
        for b in writes:
            if b in self.lastw:
                self._need(eng, waits, self.lastw[b])
            for tok in self.readers.get(b, ()):
                self._need(eng, waits, tok)
        return waits

    def _commit(self, tok, reads, writes):
        for b in reads:
            self.readers.setdefault(b, []).append(tok)
        for b in writes:
            self.lastw[b] = tok
            self.readers[b] = []

    def op(self, eng, fn, reads=(), writes=()):
        waits = self._deps(eng, reads, writes)
        self.count[eng] += 1
        tok = ("c_" + eng, self.count[eng], eng)
        self.plan[eng].append((waits, fn, tok[0], 1))
        self._commit(tok, reads, writes)
        self.ninst += 1

    def dma(self, eng, fn, reads=(), writes=()):
        i = self.dma_n[eng]
        self.dma_n[eng] += 1
        slot = i % NDMA_SEM
        key = "d_%s_%d" % (eng, slot)
        val = 16 * (i // NDMA_SEM + 1)
        waits = self._deps(eng, reads, writes)
        if val > 16:
            self._need(eng, waits, (key, val - 16, "dma"))
        tok = (key, val, "dma")
        self.plan[eng].append((waits, fn, key, 16))
        self._commit(tok, reads, writes)
        self.ninst += 1

    def _all_tokens(self):
        toks = []
        for q in ENGS:
            n = self.dma_n[q]
            for slot in range(min(n, NDMA_SEM)):
                uses = (n - 1 - slot) // NDMA_SEM + 1
                toks.append(("d_%s_%d" % (q, slot), 16 * uses, "dma"))
        for e in ENGS:
            if self.count[e]:
                toks.append(("c_" + e, self.count[e], "x"))
        return toks

    def barrier(self):
        toks = self._all_tokens()
        for e in ENGS:
            waits = []
            for t in toks:
                self._need(e, waits, t)
            if waits:
                self.plan[e].append((waits, None, None, 0))
        self.lastw.clear()
        self.readers.clear()

    def finish(self, eng="sp"):
        waits = []
        for t in self._all_tokens():
            self._need(eng, waits, t)
        self.plan[eng].append((waits, None, None, 0))

    def emit(self):
        nc = self.nc
        keys = set()
        marked = {e: set() for e in ENGS}
        for e in ENGS:
            for waits, fn, k, amt in self.plan[e]:
                if k:
                    keys.add(k)
                for wk, wv in waits:
                    keys.add(wk)
                    if wk.startswith("c_"):
                        marked[wk[2:]].add(wv)
        rank = {}
        for e in ENGS:
            for i, v in enumerate(sorted(marked[e])):
                rank[("c_" + e, v)] = i + 1
        with ExitStack() as es:
            sems = {k: es.enter_context(nc.semaphore(k)) for k in sorted(keys)}
            block = es.enter_context(nc.Block())
            deco = {"sp": block.sync, "act": block.scalar, "dve": block.vector,
                    "pool": block.gpsimd, "pe": block.tensor}

            def make(e):
                def body(h):
                    idx = 0
                    for waits, fn, k, amt in self.plan[e]:
                        for wk, wv in waits:
                            h.wait_ge(sems[wk], rank[(wk, wv)] if wk.startswith("c_") else wv)
                        if fn is not None:
                            ins = fn(h)
                            if amt == 16:
                                ins.then_inc(sems[k], 16)
                            else:
                                idx += 1
                                if idx in marked[e]:
                                    ins.then_inc(sems[k], 1)
                return body

            for e in ENGS:
                if self.plan[e]:
                    deco[e](make(e))


def K(name, *args, **kw):
    return lambda e: getattr(e, name)(*args, **kw)


def _ap(t, off, pat):
    return bass.AP(t, off, [list(p) for p in pat])


class Builder:
    def __init__(self, debug_h1=False):
        self.debug_h1 = debug_h1
        self.nc = bass.Bass("TRN2", target_bir_lowering=False)
        self.P = Prog(self.nc)
        self.dr = {}

    def dram(self, name, shape, dtype=F32, kind="ExternalInput"):
        t = self.nc.dram_tensor(name, list(shape), dtype, kind=kind)
        self.dr[name] = t
        return t

    def declare(self):
        d = self.dram
        d("xm", [NBLK * 128, D]); d("xp", [NPRE * 128, D]); d("x0", [128, D])
        d("gvec", [128, 3 * D])
        d("wall", [NPIECE, 128, 4096])
        d("bias", [1, 10 * 512])
        d("cs", [NBLK * 128, 16]); d("cs0", [128, 16])
        d("amask", [128, 2 * 272])
        d("valid", [128, NPRE + 1])
        d("hc", [128, 768 + 128 + 128 + 2])
        d("idn", [128, 128])
        d("lbl", [128, 1024]); d("grec", [128, 512]); d("sinks", [128, 8])
        d("wq", [128, 8 * 1024]); d("keysT", [128, 8 * 128])
        d("wdt", [NCGRP, 128, 8 * 512]); d("wu", [NCGRP, 128, 4 * 1024])
        d("out", [2048, D], kind="ExternalOutput")
        d("wsc", [NPIECE, 128, 4096], BF16, kind="Internal")
        d("wdb", [NCGRP, 128, 4096], BF16, kind="Internal")
        d("wub", [NCGRP, 128, 4096], BF16, kind="Internal")

    def build(self, maxstep=10 ** 9):
        nc, P = self.nc, self.P
        self.declare()
        self.step = 0

        def go():
            self.step += 1
            return self.step <= maxstep

        with ExitStack() as es:
            self.es = es
            self.alloc_persistent(es)
            self.load_consts()
            if go():
                self.precast_weights()
            for g in range(1 if getattr(Builder, 'only_g0', False) else 2):
                with ExitStack() as ea:
                    self.alloc_mixer(ea)
                    if g == 0:
                        self.precast_experts(ea)
                    if g == 0:
                        if go():
                            self.meta_kv()
                        for pb in range(NPRE):
                            if go():
                                self.precast_step()
                                self.pre_block(pb)
                        lbs = range(0, 9)
                    else:
                        lbs = range(9, 17)
                    for lb in lbs:
                        if go():
                            if g == 0:
                                self.precast_step()
                            self.main_block(lb, (lb - 1) % 8 if lb >= 1 else None)
                    if g == 0:
                        while self.xpos < len(self.xq) or self.xpend:
                            self.precast_step()
                    P.barrier()
                with ExitStack() as eb:
                    if self.debug_h1:
                        if go():
                            for tt in range(8):
                                r0 = (g * 8 + tt) * 128
                                P.dma("sp", K("dma_start",
                                    out=self.dr["out"].ap()[r0:r0 + 128, :], in_=self.h1[:, tt, :]),
                                    reads=["h1_%d" % tt])
                    else:
                        if go():
                            self.peer(eb, g)
                    P.barrier()
            P.finish("sp")
            with nc.allow_low_precision(reason="bf16 gate matrix: at most a few non-zero terms per cell, it is a bf16 matmul operand"):
                P.emit()
        return nc

    def sb(self, es, name, shape, dt=F32):
        self.uid = getattr(self, "uid", 0) + 1
        return es.enter_context(self.nc.sbuf_tensor("s%d_%s" % (self.uid, name), list(shape), dt))

    def alloc_persistent(self, es):
        sb = lambda n, s, d=F32: self.sb(es, n, s, d)
        nc = self.nc
        self.h1 = sb("h1", [128, 8, D])
        self.idf = sb("idf", [128, 128]); self.idb = sb("idb", [128, 128], BF16)
        self.gv = sb("gv", [128, 3, D])
        self.KT = sb("KT", [64, 2, 256], BF16)
        self.Vs = sb("Vs", [128, 2, 128], BF16)
        self.KTm = sb("KTm", [64, 2, 16], BF16); self.Vm = sb("Vm", [128, 128], BF16)
        self.S = sb("S", [128, 4, 128]); self.Sb = sb("Sb", [128, 2, 4, 128], BF16)
        self.ones = sb("ones", [128, 128], BF16)
        self.B = [es.enter_context(nc.psum_tensor("B%d" % i, [128, 512], F32)) for i in (0, 1, 3, 4, 5, 6, 7)]
        self.B.insert(2, es.enter_context(nc.psum_tensor("B2", [128, 1024], BF16)))

    def load_consts(self):
        P, dr = self.P, self.dr
        P.dma("sp", K("dma_start", out=self.idf[:], in_=dr["idn"].ap()), writes=["idf"])
        P.op("dve", K("tensor_copy", out=self.idb[:], in_=self.idf[:]), reads=["idf"], writes=["idb"])
        gsrc = dr["gvec"].ap()
        P.dma("sp", K("dma_start", out=self.gv[:].rearrange("p a b -> p (a b)"), in_=gsrc), writes=["gv"])
        P.op("dve", K("memset", self.ones[:], 0.0), writes=["ones"])
        P.op("dve", K("memset", self.ones[0:1, :], 1.0), writes=["ones"])
        P.op("dve", K("memset", self.S[:], 0.0), writes=["S"])
        P.op("pool", K("memset", self.Sb[:], 0.0), writes=["Sb0", "Sb1"])
        P.op("pool", K("memset", self.Vm[:], 0.0), writes=["Vm"])

    def precast_weights(self):
        P, dr = self.P, self.dr
        with ExitStack() as es:
            st = [self.sb(es, "pcst%d" % i, [128, 4096], BF16) for i in range(2)]
            for p in range(NPIECE):
                s = st[p % 2]
                nm = "pcst%d" % (p % 2)
                P.dma("pool", K("dma_start",
                    out=s[:].rearrange("p (a b) -> p a b", b=512),
                    in_=dr["wall"].ap()[p].rearrange("p (a b) -> p a b", b=512)), writes=[nm])
                P.dma("sp", K("dma_start", out=dr["wsc"].ap()[p], in_=s[:]),
                      reads=[nm], writes=["wsc%d" % p])
            P.barrier()

    def precast_experts(self, es):
        self.xst = [self.sb(es, "xcst%d" % i, [128, 4096], BF16) for i in range(3)]
        self.xq = [(cg, src, dst) for cg in range(NCGRP) for src, dst in (("wdt", "wdb"), ("wu", "wub"))]
        self.xpos = 0
        self.xpend = []

    def precast_step(self, n=3, flush=False):
        P, dr = self.P, self.dr
        for (t, nm, cg, dst) in self.xpend:
            P.dma("sp", K("dma_start", out=dr[dst].ap()[cg], in_=t[:]), reads=[nm], writes=["%s%d" % (dst, cg)])
        self.xpend = []
        if flush:
            n = len(self.xq) - self.xpos if False else 0
        for _ in range(n):
            if self.xpos >= len(self.xq):
                break
            cg, src, dst = self.xq[self.xpos]
            t, nm = self.xst[self.xpos % 3], "xcst%d" % (self.xpos % 3)
            self.xpos += 1
            P.dma("pool", K("dma_start", out=t[:].rearrange("p (a b) -> p a b", b=512),
                            in_=dr[src].ap()[cg].rearrange("p (a b) -> p a b", b=512)), writes=[nm])
            self.xpend.append((t, nm, cg, dst))

    def alloc_mixer(self, es):
        sb = lambda n, s, d=F32: self.sb(es, n, s, d)
        self.wbuf = [sb("wbuf%d" % i, [128, 8, 512], BF16) for i in range(3)]
        self.wslot = 0
        self.biasb = sb("biasb", [128, 5120], BF16)
        self.xs = sb("xs", [128, D]); self.junk = sb("junk", [128, D], BF16)
        self.st = sb("st", [128, 16])
        self.xnb = sb("xnb", [128, D], BF16); self.xT = sb("xT", [128, 8, 128], BF16)
        self.qf = sb("qf", [128, 8, 64]); self.qb = sb("qb", [128, 8, 64], BF16)
        self.kf = sb("kf", [128, 2, 64]); self.kb = sb("kb", [128, 2, 64], BF16)
        self.rt = sb("rt", [128, 4, 8, 8])
        self.qT = sb("qT", [64, 8, 128], BF16)
        self.sc = [sb("sc%d" % i, [128, 272]) for i in range(2)]
        self.pf = [sb("pf%d" % i, [128, 272], BF16) for i in range(2)]
        self.pn = [sb("pn%d" % i, [128, 272], BF16) for i in range(2)]
        self.pT = [sb("pT%d" % i, [128, 3, 128], BF16) for i in range(2)]
        self.sm = [sb("sm%d" % i, [128, 8]) for i in range(2)]
        self.attnT = sb("attnT", [64, 8, 128], BF16)
        self.rqb = sb("rqb", [128, 512], BF16)
        self.sg = sb("sg", [128, 512]); self.sgn = sb("sgn", [128, 512])
        self.lf = sb("lf", [128, 512]); self.kr = sb("kr", [128, 512])
        self.krb = sb("krb", [128, 512], BF16); self.vb = sb("vb", [128, 512], BF16)
        self.gs = sb("gs", [128, 512])
        self.sga = sb("sga", [128, D], BF16); self.sgr = sb("sgr", [128, D], BF16)
        self.E = sb("E", [128, 768]); self.dec = sb("dec", [128, 4, 2])
        self.Qp = sb("Qp", [128, 128], BF16); self.Qz = sb("Qz", [128, 2, 128], BF16)
        self.Kj = sb("Kj", [128, 4, 128], BF16); self.ATb = sb("ATb", [128, 128], BF16)
        self.ekd = sb("ekd", [128, 512]); self.kdec = sb("kdec", [128, 512], BF16)
        self.ms = sb("ms", [128, 8])
        self.recb = sb("recb", [128, 512], BF16); self.recT = sb("recT", [128, 4, 128], BF16)
        self.mg = sb("mg", [128, D]); self.mgb = sb("mgb", [128, D], BF16)
        self.mT = sb("mT", [128, 8, 128], BF16)
        self.lbr = sb("lbr", [128, 512]); self.oml = sb("oml", [128, 512])
        self.ll = sb("ll", [128, 2, 512]); self.grr = sb("grr", [128, 512])
        self.snk = sb("snk", [128, 8])
        self.hc = sb("hc", [128, 1026]); self.cmb = sb("cmb", [128, 128], BF16)
        self.am = sb("am", [128, 2, 272]); self.cs = sb("cs", [128, NBLK, 16]); self.cs0 = sb("cs0", [128, 16])
        self.vld = sb("vld", [128, NPRE + 1])
        P, dr = self.P, self.dr
        ld = lambda dst, src, nm: P.dma("sp", K("dma_start", out=dst, in_=src), writes=[nm])
        ld(self.hc[:], dr["hc"].ap(), "hc")
        ld(self.am[:], dr["amask"].ap().rearrange("p (a b) -> p a b", b=272), "am")
        ld(self.cs[:], dr["cs"].ap().rearrange("(n p) c -> p n c", p=128), "cs")
        ld(self.cs0[:], dr["cs0"].ap(), "cs0")
        ld(self.vld[:], dr["valid"].ap(), "vld")
        ld(self.ll[:], dr["lbl"].ap().rearrange("p (a b) -> p a b", b=512), "ll")
        ld(self.grr[:], dr["grec"].ap(), "grr")
        ld(self.snk[:], dr["sinks"].ap(), "snk")
        P.op("pool", K("memset", self.biasb[:], 0.0), writes=["biasb"])
        P.dma("pool", K("dma_start", out=self.biasb[0:1, :].rearrange("p (a b) -> p a b", b=512),
                                            in_=dr["bias"].ap().rearrange("p (a b) -> p a b", b=512)), writes=["biasb"])
        P.op("dve", K("tensor_tensor", out=self.lf[:], in0=self.ll[:, 0, :], in1=self.ll[:, 1, :], op=ALU.subtract),
             reads=["ll"], writes=["lf"])
        P.op("act", K("activation", out=self.lbr[:], in_=self.lf[:], func=AF.Sigmoid), reads=["lf"], writes=["lbr"])
        P.op("act", K("activation", out=self.oml[:], in_=self.lf[:], func=AF.Sigmoid, scale=-1.0),
             reads=["lf"], writes=["oml"])
        P.op("dve", K("tensor_copy", out=self.cmb[:], in_=self.hc[:, 896:1024]), reads=["hc"], writes=["cmb"])
        P.op("pool", K("memset", self.Qz[:], 0.0), writes=["Qz"])
        for i in range(2):
            P.op("pool", K("memset", self.pT[i][:], 0.0), writes=["pT%d" % i])

    def wload(self, piece):
        i = self.wslot % 3
        self.wslot += 1
        t, nm = self.wbuf[i], "wbuf%d" % i
        self.P.dma("sp", K("dma_start",
            out=t[:], in_=self.dr["wsc"].ap()[piece].rearrange("p (a b) -> p a b", b=512)),
            reads=["wsc%d" % piece], writes=[nm])
        return t, nm

    def norm_transpose(self, src_ap, gidx):
        P = self.P
        P.dma("act", K("dma_start", out=self.xs[:], in_=src_ap), writes=["xs"])
        st = self.st
        P.op("act", K("activation", out=self.junk[:], in_=self.xs[:], func=AF.Square, accum_out=st[:, 0:1]),
             reads=["xs"], writes=["junk", "st"])
        P.op("act", K("activation", out=st[:, 1:2], in_=st[:, 0:1], func=AF.Sqrt, scale=1.0 / D, bias=EPS),
             reads=["st"], writes=["st"])
        P.op("dve", K("reciprocal", out=st[:, 2:3], in_=st[:, 1:2]), reads=["st"], writes=["st"])
        P.op("dve", K("scalar_tensor_tensor", out=self.xnb[:], in0=self.xs[:], scalar=st[:, 2:3],
                                                     in1=self.gv[:, gidx, :], op0=ALU.mult, op1=ALU.mult),
             reads=["xs", "st", "gv"], writes=["xnb"])
        self.transpose8(self.xnb, "xnb", self.xT, "xT")

    def transpose8(self, src, sname, dst, dname):
        P, B2 = self.P, self.B[2]
        for c in range(8):
            P.op("pe", K("transpose", out=B2[:, c * 128:(c + 1) * 128], in_=src[:, c * 128:(c + 1) * 128],
                                                  identity=self.idb[:]), reads=[sname, "idb"], writes=["B2"])
        P.op("act", K("copy", out=dst[:], in_=B2[:].rearrange("p (a b) -> p a b", b=128)),
             reads=["B2"], writes=[dname])

    def project(self, grp, bank):
        P = self.P
        c0, n = CG[grp]
        w, wn = self.wload(grp)
        pb, bn = self.B[bank], "B%d" % bank
        for dc in range(8):
            P.op("pe", K("matmul", pb[:, 0:n], lhsT=self.xT[:, dc, :], rhs=w[:, dc, 0:n],
                                                 start=(dc == 0), stop=False), reads=["xT", wn], writes=[bn])
        P.op("pe", K("matmul", pb[:, 0:n], lhsT=self.ones[:, :], rhs=self.biasb[:, grp * 512:grp * 512 + n],
                                      start=False, stop=True), reads=["ones", "biasb"], writes=[bn])
        return pb, bn

    def rope(self, srcf, dstb, nh, cs_ap, names):
        P = self.P
        sn, dn = names
        cs_t, cs_off = cs_ap.tensor, cs_ap.offset
        pstep = cs_ap.ap[0][0]
        cosb = _ap(cs_t, cs_off, [[pstep, 128], [0, nh], [1, 8]])
        sinb = _ap(cs_t, cs_off + 8, [[pstep, 128], [0, nh], [1, 8]])
        t1, t2 = srcf[:, :, 0:8], srcf[:, :, 8:16]
        rt = self.rt
        P.op("dve", K("tensor_copy", out=dstb[:], in_=srcf[:]), reads=[sn], writes=[dn])
        P.op("dve", K("tensor_tensor", out=rt[:, 0, 0:nh, :], in0=t1, in1=cosb, op=ALU.mult), reads=[sn, "cs", "cs0"], writes=["rt"])
        P.op("dve", K("tensor_tensor", out=rt[:, 1, 0:nh, :], in0=t2, in1=sinb, op=ALU.mult), reads=[sn, "cs", "cs0"], writes=["rt"])
        P.op("dve", K("tensor_tensor", out=rt[:, 2, 0:nh, :], in0=t2, in1=cosb, op=ALU.mult), reads=[sn, "cs", "cs0"], writes=["rt"])
        P.op("dve", K("tensor_tensor", out=rt[:, 3, 0:nh, :], in0=t1, in1=sinb, op=ALU.mult), reads=[sn, "cs", "cs0"], writes=["rt"])
        P.op("dve", K("tensor_tensor", out=dstb[:, :, 0:8], in0=rt[:, 0, 0:nh, :], in1=rt[:, 1, 0:nh, :], op=ALU.subtract),
             reads=["rt"], writes=[dn])
        P.op("dve", K("tensor_tensor", out=dstb[:, :, 8:16], in0=rt[:, 2, 0:nh, :], in1=rt[:, 3, 0:nh, :], op=ALU.add),
             reads=["rt"], writes=[dn])

    def kv_from_bank(self, pb, bn, cs_ap, vdst, vname):
        P = self.P
        P.op("act", K("copy", out=self.kf[:], in_=pb[:, 0:128].rearrange("p (a b) -> p a b", b=64)),
             reads=[bn], writes=["kf"])
        P.op("act", K("copy", out=vdst, in_=pb[:, 128:256]), reads=[bn], writes=[vname])
        self.rope(self.kf, self.kb, 2, cs_ap, ("kf", "kb"))

    def meta_kv(self):
        P, B2 = self.P, self.B[2]
        self.norm_transpose(self.dr["x0"].ap(), 0)
        pb, bn = self.project(G_KV, 0)
        self.kv_from_bank(pb, bn, self.cs0[:, :], self.vb[:, 0:128], "vb")
        P.op("dve", K("tensor_copy", out=self.Vm[0:16, :], in_=self.vb[0:16, 0:128]), reads=["vb"], writes=["Vm"])
        for g in range(2):
            P.op("pe", K("transpose", out=B2[0:64, g * 128:(g + 1) * 128], in_=self.kb[:, g, :],
                                                  identity=self.idb[:]), reads=["kb", "idb"], writes=["B2"])
        P.op("act", K("copy", out=self.KTm[:], in_=B2[0:64, 0:256].rearrange("p (a b) -> p a b", b=128)[:, :, 0:16]),
             reads=["B2"], writes=["KTm"])

    def hgrn_gates(self, vcol):
        P = self.P
        pb, bn = self.project(G_RF, 0)
        P.op("act", K("activation", out=self.sg[:], in_=pb[:], func=AF.Sigmoid), reads=[bn], writes=["sg"])
        P.op("act", K("activation", out=self.sgn[:], in_=pb[:], func=AF.Sigmoid, scale=-1.0), reads=[bn], writes=["sgn"])
        pb1, bn1 = self.project(G_RI, 1)
        P.op("act", K("copy", out=self.vb[:], in_=pb1[:]), reads=[bn1], writes=["vb"])
        P.op("dve", K("tensor_tensor", out=self.sg[:], in0=self.sg[:], in1=self.oml[:], op=ALU.mult),
             reads=["sg", "oml"], writes=["sg"])
        P.op("dve", K("tensor_tensor", out=self.sg[:], in0=self.sg[:], in1=self.lbr[:], op=ALU.add),
             reads=["sg", "lbr"], writes=["sg"])
        P.op("act", K("activation", out=self.lf[:], in_=self.sg[:], func=AF.Ln), reads=["sg"], writes=["lf"])
        P.op("dve", K("tensor_tensor", out=self.kr[:], in0=self.sgn[:], in1=self.oml[:], op=ALU.mult),
             reads=["sgn", "oml"], writes=["kr"])
        if vcol is not None:
            vs = self.vld[:, vcol:vcol + 1]
            P.op("dve", K("tensor_scalar", out=self.lf[:], in0=self.lf[:], scalar1=vs, scalar2=None, op0=ALU.mult),
                 reads=["lf", "vld"], writes=["lf"])
            P.op("dve", K("tensor_scalar", out=self.kr[:], in0=self.kr[:], scalar1=vs, scalar2=None, op0=ALU.mult),
                 reads=["kr", "vld"], writes=["kr"])

    def hgrn_kdec(self):
        P = self.P
        pX = self.B[7]
        P.op("pe", K("matmul", pX[:], lhsT=self.hc[:, 768:896], rhs=self.lf[:], start=True, stop=True),
             reads=["hc", "lf"], writes=["B7"])
        P.op("act", K("activation", out=self.ekd[:], in_=pX[:], func=AF.Exp), reads=["B7"], writes=["ekd"])
        P.op("dve", K("tensor_tensor", out=self.kdec[:], in0=self.ekd[:], in1=self.kr[:], op=ALU.mult),
             reads=["ekd", "kr"], writes=["kdec"])

    def hgrn_state_update(self, h, c, sb_dst):
        P = self.P
        pd = self.B[5]
        r0 = c * 64
        P.op("pe", K("matmul", pd[:, 128:256], lhsT=self.kdec[r0:r0 + 64, h * 128:(h + 1) * 128],
                                      rhs=self.vb[r0:r0 + 64, h * 128:(h + 1) * 128], start=True, stop=True),
             reads=["kdec", "vb"], writes=["B5"])
        P.op("dve", K("scalar_tensor_tensor", out=self.S[:, h, :], in0=self.S[:, h, :], scalar=self.dec[:, h, c:c + 1],
                                                     in1=pd[:, 128:256], op0=ALU.mult, op1=ALU.add),
             reads=["S", "dec", "B5"], writes=["S"])
        if sb_dst is not None:
            P.op("act", K("copy", out=self.Sb[:, sb_dst, h, :], in_=self.S[:, h, :]),
                 reads=["S"], writes=["Sb%d" % sb_dst])

    def pre_block(self, pb_i):
        P = self.P
        sub = getattr(self, "maxsub", 99)
        self.norm_transpose(self.dr["xp"].ap()[pb_i * 128:(pb_i + 1) * 128, :], 0)
        if sub < 1:
            return
        self.hgrn_gates(pb_i)
        if sub < 2:
            return
        self.hgrn_kdec()
        if sub < 3:
            return
        pE = self.B[3]
        for h in range(4):
            P.op("pe", K("matmul", pE[:, 2 * h:2 * h + 2], lhsT=self.lf[:, h * 128:(h + 1) * 128],
                                               rhs=self.hc[:, 1024:1026], start=True, stop=True),
                 reads=["lf", "hc"], writes=["B3"])
        P.op("act", K("activation", out=self.dec[:], in_=pE[:, 0:8].rearrange("p (a b) -> p a b", b=2), func=AF.Exp),
             reads=["B3"], writes=["dec"])
        if sub < 4:
            return
        last = pb_i == NPRE - 1
        for c in range(2):
            if sub < 5 and c == 1:
                return
            for h in range(4):
                self.hgrn_state_update(h, c, 0 if (last and c == 1) else None)

    def main_block(self, lb, tt):
        P, B = self.P, self.B
        do_out = lb >= 1
        cs_ap = self.cs[:, lb, :]
        self.norm_transpose(self.dr["xm"].ap()[lb * 128:(lb + 1) * 128, :], 0)
        cur = lb % 2
        if lb >= 1:
            P.op("dve", K("tensor_copy", out=self.KT[:, :, 0:128], in_=self.KT[:, :, 128:256]),
                 reads=["KT"], writes=["KT"])
        pb, bn = self.project(G_KV, 0)
        self.kv_from_bank(pb, bn, cs_ap, self.Vs[:, cur, :], "Vs%d" % cur)
        for g in range(2):
            P.op("pe", K("transpose", out=B[2][0:64, g * 128:(g + 1) * 128], in_=self.kb[:, g, :],
                                                  identity=self.idb[:]), reads=["kb", "idb"], writes=["B2"])
        P.op("act", K("copy", out=self.KT[:, :, 128:256], in_=B[2][0:64, 0:256].rearrange("p (a b) -> p a b", b=128)),
             reads=["B2"], writes=["KT"])
        sub = getattr(self, "maxsub", 99)
        if do_out and sub >= 11:
            self.attention(lb, cs_ap)
        if sub >= 13 or not do_out:
            self.hgrn(lb, do_out)
        if do_out and sub >= 14:
            self.merge_out(lb, tt)

    def attention(self, lb, cs_ap):
        P, B = self.P, self.B
        pb, bn = self.project(G_Q, 1)
        P.op("act", K("copy", out=self.qf[:], in_=pb[:].rearrange("p (a b) -> p a b", b=64)), reads=[bn], writes=["qf"])
        self.rope(self.qf, self.qb, 8, cs_ap, ("qf", "qb"))
        for h in range(8):
            P.op("pe", K("transpose", out=B[2][0:64, h * 128:(h + 1) * 128], in_=self.qb[:, h, :],
                                                  identity=self.idb[:]), reads=["qb", "idb"], writes=["B2"])
        P.op("act", K("copy", out=self.qT[:], in_=B[2][0:64, :].rearrange("p (a b) -> p a b", b=128)),
             reads=["B2"], writes=["qT"])
        mi = 0 if lb == 1 else 1
        prv, cur = (lb - 1) % 2, lb % 2
        if getattr(self, "maxsub", 99) < 12:
            return
        for h in range(8):
            g, i = h // 4, h % 2
            pS, sn = B[3 + i], "B%d" % (3 + i)
            sc, pf, pn, pT, sm = self.sc[i], self.pf[i], self.pn[i], self.pT[i], self.sm[i]
            scn, pfn, pnn, pTn, smn = "sc%d" % i, "pf%d" % i, "pn%d" % i, "pT%d" % i, "sm%d" % i
            P.op("pe", K("matmul", pS[:, 0:256], lhsT=self.qT[:, h, :], rhs=self.KT[:, g, :],
                                                           start=True, stop=True), reads=["qT", "KT"], writes=[sn])
            P.op("pe", K("matmul", pS[:, 256:272], lhsT=self.qT[:, h, :], rhs=self.KTm[:, g, :],
                                                           start=True, stop=True), reads=["qT", "KTm"], writes=[sn])
            P.op("dve", K("scalar_tensor_tensor", out=sc[:], in0=pS[:, 0:272], scalar=0.125,
                                                                       in1=self.am[:, mi, :], op0=ALU.mult, op1=ALU.add),
                 reads=[sn, "am"], writes=[scn])
            P.op("dve", K("tensor_reduce", out=sm[:, 0:1], in_=sc[:], axis=AX.X, op=ALU.max),
                 reads=[scn], writes=[smn])
            P.op("dve", K("tensor_tensor", out=sm[:, 1:2], in0=sm[:, 0:1], in1=self.snk[:, h:h + 1], op=ALU.max),
                 reads=[smn, "snk"], writes=[smn])
            P.op("dve", K("tensor_scalar", out=sm[:, 2:3], in0=sm[:, 1:2], scalar1=-1.0, scalar2=None, op0=ALU.mult),
                 reads=[smn], writes=[smn])
            P.op("act", K("activation", out=pf[:], in_=sc[:], func=AF.Exp, bias=sm[:, 2:3],
                                                                   accum_out=sm[:, 3:4]), reads=[scn, smn], writes=[pfn, smn])
            P.op("act", K("activation", out=sm[:, 4:5], in_=self.snk[:, h:h + 1], func=AF.Exp, bias=sm[:, 2:3]),
                 reads=["snk", smn], writes=[smn])
            P.op("dve", K("tensor_tensor", out=sm[:, 5:6], in0=sm[:, 3:4], in1=sm[:, 4:5], op=ALU.add),
                 reads=[smn], writes=[smn])
            P.op("dve", K("reciprocal", out=sm[:, 6:7], in_=sm[:, 5:6]), reads=[smn], writes=[smn])
            P.op("dve", K("tensor_scalar", out=pn[:], in0=pf[:], scalar1=sm[:, 6:7], scalar2=None,
                                                                      op0=ALU.mult), reads=[pfn, smn], writes=[pnn])
            for j, (c0, n) in enumerate(((0, 128), (128, 128), (256, 16))):
                P.op("pe", K("transpose", out=B[2][0:n, j * 128:(j + 1) * 128], in_=pn[:, c0:c0 + n],
                                                                          identity=self.idb[:]), reads=[pnn, "idb"], writes=["B2"])
            P.op("act", K("copy", out=pT[:, 0:2, :], in_=B[2][:, 0:256].rearrange("p (a b) -> p a b", b=128)),
                 reads=["B2"], writes=[pTn])
            P.op("act", K("copy", out=pT[0:16, 2, :], in_=B[2][0:16, 256:384]), reads=["B2"], writes=[pTn])
            po = B[5][0:64, 256:384]
            gs_ = slice(g * 64, (g + 1) * 64)
            P.op("pe", K("matmul", po, lhsT=self.Vs[:, prv, gs_], rhs=pT[:, 0, :], start=True, stop=False),
                 reads=["Vs%d" % prv, pTn], writes=["B5"])
            P.op("pe", K("matmul", po, lhsT=self.Vs[:, cur, gs_], rhs=pT[:, 1, :], start=False, stop=False),
                 reads=["Vs%d" % cur, pTn], writes=["B5"])
            P.op("pe", K("matmul", po, lhsT=self.Vm[:, gs_], rhs=pT[:, 2, :], start=False, stop=True),
                 reads=["Vm", pTn], writes=["B5"])
            P.op("act", K("copy", out=self.attnT[:, h, :], in_=po), reads=["B5"], writes=["attnT"])

    def hgrn(self, lb, do_out):
        P, B = self.P, self.B
        self.hgrn_gates(NPRE if lb == 0 else None)
        self.hgrn_kdec()
        if not do_out:
            pE = B[3]
            for h in range(4):
                P.op("pe", K("matmul", pE[:, 2 * h:2 * h + 2], lhsT=self.lf[:, h * 128:(h + 1) * 128],
                                                   rhs=self.hc[:, 1024:1026], start=True, stop=True),
                     reads=["lf", "hc"], writes=["B3"])
            P.op("act", K("activation", out=self.dec[:], in_=pE[:, 0:8].rearrange("p (a b) -> p a b", b=2), func=AF.Exp),
                 reads=["B3"], writes=["dec"])
            for c in range(2):
                for h in range(4):
                    self.hgrn_state_update(h, c, 0 if c == 1 else None)
            return
        pb, bn = self.project(G_RQ, 0)
        P.op("act", K("copy", out=self.rqb[:], in_=pb[:]), reads=[bn], writes=["rqb"])
        pb, bn = self.project(G_RG, 1)
        P.op("act", K("activation", out=self.gs[:], in_=pb[:], func=AF.Silu), reads=[bn], writes=["gs"])
        P.op("dve", K("tensor_tensor", out=self.gs[:], in0=self.gs[:], in1=self.grr[:], op=ALU.mult),
             reads=["gs", "grr"], writes=["gs"])
        P.op("dve", K("tensor_copy", out=self.krb[:], in_=self.kr[:]), reads=["kr"], writes=["krb"])
        pO = B[6]
        E = self.E
        for h in range(4):
            hs = slice(h * 128, (h + 1) * 128)
            P.op("pe", K("matmul", B[3][:], lhsT=self.lf[:, hs], rhs=self.hc[:, 0:512], start=True, stop=True),
                 reads=["lf", "hc"], writes=["B3"])
            P.op("pe", K("matmul", B[4][:, 0:256], lhsT=self.lf[:, hs], rhs=self.hc[:, 512:768], start=True, stop=True),
                 reads=["lf", "hc"], writes=["B4"])
            P.op("act", K("activation", out=E[:, 0:512], in_=B[3][:], func=AF.Exp), reads=["B3"], writes=["E"])
            P.op("act", K("activation", out=E[:, 512:768], in_=B[4][:, 0:256], func=AF.Exp), reads=["B4"], writes=["E"])
            P.op("dve", K("tensor_copy", out=self.dec[:, h, 0:1], in_=E[:, 191:192]), reads=["E"], writes=["dec"])
            P.op("dve", K("tensor_copy", out=self.dec[:, h, 1:2], in_=E[:, 255:256]), reads=["E"], writes=["dec"])
            P.op("pe", K("transpose", out=B[2][:, 0:128], in_=self.rqb[:, hs], identity=self.idb[:]),
                 reads=["rqb", "idb"], writes=["B2"])
            P.op("pe", K("transpose", out=B[2][:, 128:256], in_=self.krb[:, hs], identity=self.idb[:]),
                 reads=["krb", "idb"], writes=["B2"])
            qTp, kTp = B[2][:, 0:128], B[2][:, 128:256]
            P.op("dve", K("tensor_tensor", out=self.Qp[:], in0=E[:, 0:128], in1=qTp, op=ALU.mult),
                 reads=["E", "B2"], writes=["Qp"])
            P.op("dve", K("tensor_tensor", out=self.Qz[:, 0, 0:64], in0=E[:, 128:192], in1=B[2][:, 0:64], op=ALU.mult),
                 reads=["E", "B2"], writes=["Qz"])
            P.op("dve", K("tensor_tensor", out=self.Qz[:, 1, 64:128], in0=E[:, 192:256], in1=B[2][:, 64:128], op=ALU.mult),
                 reads=["E", "B2"], writes=["Qz"])
            kTb = _ap(B[2], 128, [[1024, 128], [0, 4], [1, 128]])
            P.op("dve", K("tensor_tensor", out=self.Kj[:], in0=E[:, 256:768].rearrange("p (a b) -> p a b", b=128),
                                                  in1=kTb, op=ALU.mult), reads=["E", "B2"], writes=["Kj"])
            pA = B[5][:, 0:128]
            for j in range(4):
                for c in range(2):
                    t0 = c * 64 + 16 * j
                    P.op("pe", K("matmul", B[5][:, t0:t0 + 16], lhsT=self.Kj[:, j, :], rhs=self.Qp[:, t0:t0 + 16],
                                                              start=True, stop=True), reads=["Kj", "Qp"], writes=["B5"])
            P.op("dve", K("tensor_tensor", out=self.ATb[:], in0=pA, in1=self.cmb[:], op=ALU.mult),
                 reads=["B5", "cmb"], writes=["ATb"])
            P.op("pe", K("matmul", pO[:, hs], lhsT=self.ATb[:], rhs=self.vb[:, hs], start=True, stop=False),
                 reads=["ATb", "vb"], writes=["B6"])
            P.op("pe", K("matmul", pO[:, hs], lhsT=self.Qz[:, 0, :], rhs=self.Sb[:, 0, h, :], start=False, stop=False),
                 reads=["Qz", "Sb0"], writes=["B6"])
            self.hgrn_state_update(h, 0, 1)
            P.op("pe", K("matmul", pO[:, hs], lhsT=self.Qz[:, 1, :], rhs=self.Sb[:, 1, h, :], start=False, stop=True),
                 reads=["Qz", "Sb1"], writes=["B6"])
            self.hgrn_state_update(h, 1, 0)
        ms = self.ms
        for h in range(4):
            hs = slice(h * 128, (h + 1) * 128)
            P.op("act", K("activation", out=self.junk[:, hs], in_=pO[:, hs], func=AF.Square, accum_out=ms[:, h:h + 1]),
                 reads=["B6"], writes=["junk", "ms"])
        P.op("act", K("activation", out=ms[:, 4:8], in_=ms[:, 0:4], func=AF.Sqrt, scale=1.0 / 128, bias=EPS),
             reads=["ms"], writes=["ms"])
        P.op("dve", K("reciprocal", out=ms[:, 0:4], in_=ms[:, 4:8]), reads=["ms"], writes=["ms"])
        for h in range(4):
            hs = slice(h * 128, (h + 1) * 128)
            P.op("dve", K("scalar_tensor_tensor", out=self.recb[:, hs], in0=pO[:, hs], scalar=ms[:, h:h + 1],
                                                                     in1=self.gs[:, hs], op0=ALU.mult, op1=ALU.mult),
                 reads=["B6", "ms", "gs"], writes=["recb"])
        for c in range(4):
            P.op("pe", K("transpose", out=B[2][:, c * 128:(c + 1) * 128], in_=self.recb[:, c * 128:(c + 1) * 128],
                                                  identity=self.idb[:]), reads=["recb", "idb"], writes=["B2"])
        P.op("act", K("copy", out=self.recT[:], in_=B[2][:, 0:512].rearrange("p (a b) -> p a b", b=128)),
             reads=["B2"], writes=["recT"])

    def merge_out(self, lb, tt):
        P, B = self.P, self.B
        for i, (grp, dst, dn) in enumerate(((G_GA0, self.sga[:, 0:512], "sga"), (G_GA1, self.sga[:, 512:1024], "sga"),
                                            (G_GR0, self.sgr[:, 0:512], "sgr"), (G_GR1, self.sgr[:, 512:1024], "sgr"))):
            pb, bn = self.project(grp, i % 2)
            P.op("act", K("activation", out=dst, in_=pb[:], func=AF.Sigmoid), reads=[bn], writes=[dn])
        for half in range(2):
            w, wn = self.wload(P_WUA + half)
            pU, un = B[6 + half], "B%d" % (6 + half)
            for h in range(8):
                P.op("pe", K("matmul", pU[:], lhsT=self.attnT[:, h, :], rhs=w[0:64, h, :],
                                                               start=(h == 0), stop=(h == 7)), reads=["attnT", wn], writes=[un])
            hs = slice(half * 512, (half + 1) * 512)
            P.op("dve", K("tensor_tensor", out=self.mg[:, hs], in0=self.sga[:, hs], in1=pU[:], op=ALU.mult),
                 reads=["sga", un], writes=["mg"])
        for half in range(2):
            w, wn = self.wload(P_WUR + half)
            pU, un = B[6 + half], "B%d" % (6 + half)
            for c in range(4):
                P.op("pe", K("matmul", pU[:], lhsT=self.recT[:, c, :], rhs=w[:, c, :],
                                                               start=(c == 0), stop=(c == 3)), reads=["recT", wn], writes=[un])
            hs = slice(half * 512, (half + 1) * 512)
            P.op("dve", K("tensor_tensor", out=self.junk[:, hs], in0=self.sgr[:, hs], in1=pU[:], op=ALU.mult),
                 reads=["sgr", un], writes=["junk"])
            P.op("dve", K("tensor_tensor", out=self.mgb[:, hs], in0=self.mg[:, hs], in1=self.junk[:, hs], op=ALU.add),
                 reads=["mg", "junk"], writes=["mgb"])
        self.transpose8(self.mgb, "mgb", self.mT, "mT")
        for half in range(2):
            w, wn = self.wload(P_WO + half)
            pU, un = B[6 + half], "B%d" % (6 + half)
            for dc in range(8):
                P.op("pe", K("matmul", pU[:], lhsT=self.mT[:, dc, :], rhs=w[:, dc, :],
                                                                 start=(dc == 0), stop=(dc == 7)), reads=["mT", wn], writes=[un])
            hs = slice(half * 512, (half + 1) * 512)
            P.op("dve", K("tensor_tensor", out=self.h1[:, tt, hs], in0=self.xs[:, hs], in1=pU[:], op=ALU.add),
                 reads=["xs", un], writes=["h1_%d" % tt])

    def peer(self, es, g):
        P, B, dr = self.P, self.B, self.dr
        sb = lambda n, s, d=F32: self.sb(es, n, s, d)
        NT = 8
        xn2T = sb("xn2T", [128, 8, 1024], BF16)
        S1 = sb("S1", [128, NT * 8 * 128]); S2 = sb("S2", [128, NT * 8 * 128])
        TAU = sb("TAU", [128, NT, 8]); NLSE = sb("NLSE", [128, NT, 8])
        pst = sb("pst", [128, 16])
        SP = NT * 8 * 128
        with ExitStack() as e1:
            sb1 = lambda n, s, d=F32: self.sb(e1, n, s, d)
            wq = sb1("wq", [128, 8, 1024]); keysT = sb1("keysT", [128, 8, 128])
            xnf = sb1("xnf", [128, D]); xTf = sb1("xTf", [128, 8, 128]); qT = sb1("qT2", [128, 8, 128])
            junk = sb1("junk2", [128, D], BF16)
            wk = sb1("wk", [128, 256]); t16 = sb1("t16", [128, 2, 8, 16])
            cand = sb1("cand", [128, 8, 256]); best = sb1("best", [128, 8, 16]); eb = sb1("eb", [128, 8, 16])
            zz = sb1("zz", [128, 16])
            P.dma("sp", K("dma_start", out=wq[:].rearrange("p a b -> p (a b)"), in_=dr["wq"].ap()), writes=["wq"])
            P.dma("sp", K("dma_start", out=keysT[:].rearrange("p a b -> p (a b)"), in_=dr["keysT"].ap()), writes=["keysT"])
            for tt in range(NT):
                hx = self.h1[:, tt, :]
                hn = "h1_%d" % tt
                if getattr(self, "maxsub", 99) < 19:
                    continue
                P.op("act", K("activation", out=junk[:], in_=hx, func=AF.Square, accum_out=pst[:, 0:1]),
                     reads=[hn], writes=["junk2", "pst"])
                P.op("act", K("activation", out=pst[:, 1:2], in_=pst[:, 0:1], func=AF.Sqrt, scale=1.0 / D, bias=EPS),
                     reads=["pst"], writes=["pst"])
                P.op("dve", K("reciprocal", out=pst[:, 2:3], in_=pst[:, 1:2]), reads=["pst"], writes=["pst"])
                P.op("dve", K("scalar_tensor_tensor", out=xnf[:], in0=hx, scalar=pst[:, 2:3], in1=self.gv[:, 1, :],
                              op0=ALU.mult, op1=ALU.mult), reads=[hn, "pst", "gv"], writes=["xnf"])
                if getattr(self, "maxsub", 99) < 20:
                    continue
                for c in range(8):
                    bk = c // 4
                    P.op("pe", K("matmul", B[bk][:, (c % 4) * 128:(c % 4 + 1) * 128], lhsT=xnf[:, c * 128:(c + 1) * 128],
                                 rhs=self.idf[:], start=True, stop=True), reads=["xnf", "idf"], writes=["B%d" % bk])
                if getattr(self, "maxsub", 99) < 21:
                    continue
                for bk in range(2):
                    src = B[bk][:].rearrange("p (a b) -> p a b", b=128)
                    P.op("act", K("copy", out=xTf[:, bk * 4:bk * 4 + 4, :], in_=src), reads=["B%d" % bk], writes=["xTf"])
                    P.op("dve", K("tensor_copy", out=xn2T[:, bk * 4:bk * 4 + 4, tt * 128:(tt + 1) * 128], in_=src),
                         reads=["B%d" % bk], writes=["xn2T"])
                if getattr(self, "maxsub", 99) < 22:
                    continue
                for h in range(8):
                    bk = 3 + h // 4
                    for dc in range(8):
                        P.op("pe", K("matmul", B[bk][:, (h % 4) * 128:(h % 4 + 1) * 128], lhsT=wq[:, dc, h * 128:(h + 1) * 128],
                                     rhs=xTf[:, dc, :], start=(dc == 0), stop=(dc == 7)), reads=["wq", "xTf"], writes=["B%d" % bk])
                for i in range(2):
                    P.op("act", K("copy", out=qT[:, i * 4:i * 4 + 4, :], in_=B[3 + i][:].rearrange("p (a b) -> p a b", b=128)),
                         reads=["B%d" % (3 + i)], writes=["qT2"])
                if getattr(self, "maxsub", 99) < 23:
                    continue
                for p, Sx, sn in ((0, S1, "S1"), (1, S2, "S2")):
                    for h in range(8):
                        bk = 5 + h // 4
                        P.op("pe", K("matmul", B[bk][:, (h % 4) * 128:(h % 4 + 1) * 128], lhsT=qT[p * 64:(p + 1) * 64, h, :],
                                     rhs=keysT[p * 64:(p + 1) * 64, h, :], start=True, stop=True),
                             reads=["qT2", "keysT"], writes=["B%d" % bk])
                    for i in range(2):
                        o0 = (tt * 8 + i * 4) * 128
                        P.op("act", K("copy", out=Sx[:, o0:o0 + 512], in_=B[5 + i][:]), reads=["B%d" % (5 + i)], writes=[sn])
                if getattr(self, "maxsub", 99) < 24:
                    continue
                for p, Sx, sn in ((0, S1, "S1"), (1, S2, "S2")):
                    for h in range(8):
                        o0 = (tt * 8 + h) * 128
                        src = Sx[:, o0:o0 + 128]
                        P.op("dve", K("max", out=t16[:, p, h, 0:8], in_=src), reads=[sn], writes=["t16"])
                        P.op("dve", K("match_replace", out=wk[:, 0:128], in_to_replace=t16[:, p, h, 0:8], in_values=src,
                                      imm_value=NEG), reads=[sn, "t16"], writes=["wk"])
                        P.op("dve", K("max", out=t16[:, p, h, 8:16], in_=wk[:, 0:128]), reads=["wk"], writes=["t16"])
                if getattr(self, "maxsub", 99) < 25:
                    continue
                a0 = _ap(t16, 0, [[256, 128], [16, 8], [1, 16], [0, 16]])
                a1 = _ap(t16, 128, [[256, 128], [16, 8], [0, 16], [1, 16]])
                co = _ap(cand, 0, [[2048, 128], [256, 8], [16, 16], [1, 16]])
                P.op("dve", K("tensor_tensor", out=co, in0=a0, in1=a1, op=ALU.add), reads=["t16"], writes=["cand"])
                for h in range(8):
                    P.op("dve", K("max", out=best[:, h, 0:8], in_=cand[:, h, :]), reads=["cand"], writes=["best"])
                    P.op("dve", K("match_replace", out=wk[:, 0:256], in_to_replace=best[:, h, 0:8], in_values=cand[:, h, :],
                                  imm_value=NEG), reads=["cand", "best"], writes=["wk"])
                    P.op("dve", K("max", out=best[:, h, 8:16], in_=wk[:, 0:256]), reads=["wk"], writes=["best"])
                P.op("dve", K("tensor_copy", out=TAU[:, tt, :], in_=best[:, :, 15]), reads=["best"], writes=["TAU"])
                bm = _ap(best, 0, [[128, 128], [16, 8], [0, 16]])
                P.op("dve", K("tensor_tensor", out=eb[:], in0=best[:], in1=bm, op=ALU.subtract), reads=["best"], writes=["eb"])
                P.op("act", K("activation", out=eb[:], in_=eb[:], func=AF.Exp), reads=["eb"], writes=["eb"])
                P.op("dve", K("tensor_reduce", out=zz[:, 0:8], in_=eb[:], axis=AX.X, op=ALU.add), reads=["eb"], writes=["zz"])
                P.op("act", K("activation", out=zz[:, 8:16], in_=zz[:, 0:8], func=AF.Ln), reads=["zz"], writes=["zz"])
                P.op("dve", K("scalar_tensor_tensor", out=NLSE[:, tt, :], in0=zz[:, 8:16], scalar=-1.0, in1=best[:, :, 0],
                              op0=ALU.mult, op1=ALU.subtract), reads=["zz", "best"], writes=["NLSE"])
            P.barrier()
        with ExitStack() as e2:
            sb2 = lambda n, s, d=F32: self.sb(e2, n, s, d)
            wd = [sb2("wd%d" % i, [128, 8, 512], BF16) for i in range(2)]
            wu = [sb2("wu%d" % i, [128, 4, 1024], BF16) for i in range(2)]
            sm = [sb2("sum%d" % i, [128, 2, 512]) for i in range(2)]
            gt = [sb2("gate%d" % i, [128, 2, 512], BF16) for i in range(2)]
            Ghs = [sb2("Gh%d" % i, [128, 8, 512], BF16) for i in range(2)]
            GTs = [sb2("GTs%d" % i, [128, 4, 512], BF16) for i in range(2)]
            gel = [sb2("gel%d" % i, [128, 512], BF16) for i in range(2)]
            AT = [sb2("AT", [128, 4, 512], BF16)] * 2
            ob = _ap(sm[0], 0, [[1024, 128], [1, 1024]])
            SUM_ENG = "pool"
            cnt = {"s": 0, "y": 0}

            def wfetch(cg):
                i = cg % 2
                P.dma("sp", K("dma_start", out=wd[i][:].rearrange("p a b -> p (a b)"), in_=dr["wdb"].ap()[cg]),
                      reads=["wdb%d" % cg], writes=["wd%d" % i])
                P.dma("sp", K("dma_start", out=wu[i][:].rearrange("p a b -> p (a b)"), in_=dr["wub"].ap()[cg]),
                      reads=["wub%d" % cg], writes=["wu%d" % i])

            def stage_a(u, j):
                cg, tb = u // 2, u % 2
                tt = tb * 4 + j
                gq = (u * 4 + j) % 2
                Gh, ghn = Ghs[gq], "Gh%d" % gq
                ks = []
                for hp in range(4):
                    ks.append(cnt["s"] % 2)
                    cnt["s"] += 1

                def e_sum(hp):
                    k = ks[hp]
                    o1 = (tt * 8 + 2 * hp) * 128 + 4 * cg
                    o2 = (tt * 8 + 2 * hp) * 128
                    s1b = _ap(S1, o1, [[SP, 128], [128, 2], [1, 4], [0, 128]])
                    s2b = _ap(S2, o2, [[SP, 128], [128, 2], [0, 4], [1, 128]])
                    so = _ap(sm[k], 0, [[1024, 128], [512, 2], [128, 4], [1, 128]])
                    P.op("pool" if hp % 2 == 1 else "dve", K("tensor_tensor", out=so, in0=s1b, in1=s2b, op=ALU.add),
                         reads=["S1", "S2"], writes=["sum%d" % k])

                def e_exp(hp):
                    k = ks[hp]
                    for hh in range(2):
                        h = 2 * hp + hh
                        P.op("act", K("activation", out=gt[k][:, hh, :], in_=sm[k][:, hh, :], func=AF.Exp,
                                      bias=NLSE[:, tt, h:h + 1]), reads=["sum%d" % k, "NLSE"], writes=["gate%d" % k])

                def e_stt(hp):
                    k = ks[hp]
                    for hh in range(2):
                        h = 2 * hp + hh
                        P.op("dve", K("scalar_tensor_tensor", out=Gh[:, h, :], in0=sm[k][:, hh, :], scalar=TAU[:, tt, h:h + 1],
                                      in1=gt[k][:, hh, :], op0=ALU.is_ge, op1=ALU.mult),
                             reads=["sum%d" % k, "gate%d" % k, "TAU"], writes=[ghn])

                e_sum(0)
                for hp in range(4):
                    e_exp(hp)
                    if hp + 1 < 4:
                        e_sum(hp + 1)
                    e_stt(hp)
                gb = 6 + j % 2
                for c4 in range(4):
                    for h in range(8):
                        P.op("pe", K("matmul", B[gb][:, c4 * 128:(c4 + 1) * 128], lhsT=Gh[:, h, c4 * 128:(c4 + 1) * 128],
                                     rhs=self.idb[:], start=(h == 0), stop=(h == 7)), reads=[ghn, "idb"], writes=["B%d" % gb])

            def stage_cp(u, j):
                gb = 6 + j % 2
                P.op("act", K("copy", out=GTs[u % 2][:, :, j * 128:(j + 1) * 128], in_=B[gb][:].rearrange("p (a b) -> p a b", b=128)),
                     reads=["B%d" % gb], writes=["GTs%d" % (u % 2)])

            def stage_bc_pe(u, c4):
                cg, tb = u // 2, u % 2
                wi = cg % 2
                k = c4 % 2
                hb_, hbn = B[k], "B%d" % k
                for dc in range(8):
                    P.op("pe", K("matmul", hb_[:], lhsT=wd[wi][:, dc, c4 * 128:(c4 + 1) * 128],
                                 rhs=xn2T[:, dc, tb * 512:(tb + 1) * 512], start=(dc == 0), stop=(dc == 7)),
                         reads=["wd%d" % wi, "xn2T"], writes=[hbn])
                P.op("act", K("activation", out=gel[k][:], in_=hb_[:], func=AF.Gelu), reads=[hbn], writes=["gel%d" % k])

            def stage_bc_dve(u, c4):
                k = c4 % 2
                P.op("dve", K("tensor_tensor", out=AT[0][:, c4, :], in0=gel[k][:], in1=GTs[u % 2][:, c4, :], op=ALU.mult),
                     reads=["gel%d" % k, "GTs%d" % (u % 2)], writes=["AT"])

            ybank = {}

            def stage_bu_pe(u, j):
                cg = u // 2
                wi = cg % 2
                for half in range(2):
                    by = 3 + cnt["y"] % 3
                    cnt["y"] += 1
                    ybank[(u, j, half)] = by
                    for c4 in range(4):
                        P.op("pe", K("matmul", B[by][:], lhsT=AT[0][:, c4, j * 128:(j + 1) * 128],
                                     rhs=wu[wi][:, c4, half * 512:(half + 1) * 512], start=(c4 == 0), stop=(c4 == 3)),
                             reads=["AT", "wu%d" % wi], writes=["B%d" % by])

            def stage_bu_dve(u, j):
                tt = (u % 2) * 4 + j
                for half in range(2):
                    by = ybank[(u, j, half)]
                    hsl = self.h1[:, tt, half * 512:(half + 1) * 512]
                    P.op("dve", K("tensor_tensor", out=hsl, in0=hsl, in1=B[by][:], op=ALU.add),
                         reads=["h1_%d" % tt, "B%d" % by], writes=["h1_%d" % tt])

            _ms = getattr(self, "maxsub", 99)
            ncg = NCGRP if _ms >= 31 else (4 if _ms == 30 else max(0, _ms - 26))
            nu = 2 * ncg
            wfetch(0)
            if nu:
                for j in range(4):
                    stage_a(0, j)
                    if j:
                        stage_cp(0, j - 1)
            for u in range(nu):
                if u % 2 == 0 and u // 2 + 1 < ncg:
                    wfetch(u // 2 + 1)
                nxt = u + 1 < nu
                A = (lambda j: stage_a(u + 1, j)) if nxt else (lambda j: None)
                CP = (lambda j: stage_cp(u + 1, j)) if nxt else (lambda j: None)
                stage_cp(u, 3)
                A(0)
                stage_bc_pe(u, 0); stage_bc_pe(u, 1)
                A(1); CP(0)
                stage_bc_dve(u, 0); stage_bc_dve(u, 1)
                stage_bc_pe(u, 2); stage_bc_pe(u, 3)
                A(2); CP(1)
                stage_bc_dve(u, 2); stage_bc_dve(u, 3)
                stage_bu_pe(u, 0)
                A(3); CP(2)
                stage_bu_dve(u, 0)
                stage_bu_pe(u, 1); stage_bu_dve(u, 1)
                stage_bu_pe(u, 2); stage_bu_dve(u, 2)
                stage_bu_pe(u, 3); stage_bu_dve(u, 3)
            for tt in range(NT):
                hx = self.h1[:, tt, :]
                hn = "h1_%d" % tt
                P.op("act", K("activation", out=ob, in_=hx, func=AF.Square, accum_out=pst[:, 4:5]),
                     reads=[hn], writes=["sum0", "pst"])
                P.op("act", K("activation", out=pst[:, 5:6], in_=pst[:, 4:5], func=AF.Sqrt, scale=1.0 / D, bias=EPS),
                     reads=["pst"], writes=["pst"])
                P.op("dve", K("reciprocal", out=pst[:, 6:7], in_=pst[:, 5:6]), reads=["pst"], writes=["pst"])
                P.op("dve", K("scalar_tensor_tensor", out=ob, in0=hx, scalar=pst[:, 6:7], in1=self.gv[:, 2, :],
                              op0=ALU.mult, op1=ALU.mult), reads=[hn, "pst", "gv"], writes=["sum0"])
                r0 = (g * 8 + tt) * 128
                P.dma("sp", K("dma_start", out=dr["out"].ap()[r0:r0 + 128, :], in_=ob), reads=["sum0"])


def _hgrn_consts():
    u = np.arange(128)[:, None]; t = np.arange(128)[None, :]
    same_chunk = (u // 64) == (t // 64)
    same_sub = (u // 16) == (t // 16)
    trisub = (same_sub & (u <= t)).astype(np.float32)
    tri = (same_chunk & (u <= t)).astype(np.float32)
    mj = []
    for j in range(4):
        m = np.zeros((128, 128), np.float32)
        for c in range(2):
            lo = c * 64 + 16 * j
            for s in range(c * 64, c * 64 + 64):
                if s >= lo:
                    hi = min(s, lo + 15)
                    m[lo:hi + 1, s] = -1.0
                else:
                    m[s + 1:lo, s] = 1.0
        mj.append(m)
    triu = (same_chunk & (u > t)).astype(np.float32)
    cmask = (same_chunk & (u <= t)).astype(np.float32)
    ind = np.stack([(np.arange(128) < 64), (np.arange(128) >= 64)], axis=1).astype(np.float32)
    return np.concatenate([trisub, tri] + mj + [triu, cmask, ind], axis=1)


def _rope_table(pos):
    half = 8
    inv = np.power(np.float32(500000.0), -np.arange(half, dtype=np.float32) * np.float32(2.0) / np.float32(16))
    ang = pos.astype(np.float32)[:, None] * inv[None, :]
    return np.concatenate([np.cos(ang), np.sin(ang)], axis=1).astype(np.float32)


def _attn_mask(first_is_pad):
    i = np.arange(128)[:, None]; j = np.arange(128)[None, :]
    prev = np.where(j > i, 0.0, NEG)
    curm = np.where(j <= i, 0.0, NEG)
    meta = np.zeros((128, 16))
    m1 = np.concatenate([prev, curm, meta], axis=1)
    m0 = m1.copy()
    if first_is_pad:
        m0[:, 0:128] = NEG
    return np.stack([m0, m1], axis=1).reshape(128, 2 * 272).astype(np.float32)


def _pack_weights(w_in, w_up_attn, w_up_rec, w_out):
    wall = np.zeros((NPIECE, 128, 8, 512), np.float32)
    wv = w_in.reshape(8, 128, 4864)
    for gi, (c0, n) in enumerate(CG):
        wall[gi, :, :, :n] = wv[:, :, c0:c0 + n].transpose(1, 0, 2)
    ua = w_up_attn.reshape(8, 64, 1024)
    ur = w_up_rec.reshape(4, 128, 1024)
    wo = w_out.reshape(8, 128, 1024)
    for half in range(2):
        hs = slice(half * 512, (half + 1) * 512)
        wall[P_WUA + half, 0:64, :, :] = ua[:, :, hs].transpose(1, 0, 2)
        wall[P_WUR + half, :, 0:4, :] = ur[:, :, hs].transpose(1, 0, 2)
        wall[P_WO + half, :, :, :] = wo[:, :, hs].transpose(1, 0, 2)
    return wall.reshape(NPIECE, 128, 4096)


_NC_CACHE = {}


def _host_inputs(x, meta_tokens, norm_mix_g, w_in, b_in, attn_sinks, lb_logits, rec_norm_g, w_up_attn, w_up_rec,
                 w_out, norm_ffn_g, peer_w_query, peer_sub_keys, peer_expert_down, peer_expert_up, final_norm_g):
    f = lambda a: np.ascontiguousarray(np.asarray(a, dtype=np.float32))
    x = f(x); meta = f(meta_tokens)
    wall = _pack_weights(f(w_in)[0], f(w_up_attn)[0], f(w_up_rec)[0], f(w_out)[0])
    bias = np.zeros((1, 10, 512), np.float32)
    for gi, (c0, n) in enumerate(CG):
        bias[0, gi, :n] = f(b_in)[0, c0:c0 + n]
    bias = bias.reshape(1, 5120)
    rep = lambda v: np.ascontiguousarray(np.broadcast_to(np.asarray(v, np.float32).reshape(1, -1), (128, np.asarray(v).size)))
    gvec = rep(np.stack([f(norm_mix_g)[0], f(norm_ffn_g)[0], f(final_norm_g)], axis=0))
    hc = _hgrn_consts()
    idn = np.eye(128, dtype=np.float32)
    x0 = np.zeros((128, D), np.float32); x0[0:16] = meta
    cs0 = _rope_table(np.arange(128))
    wq = f(peer_w_query)[0].reshape(8, 128, 1024).transpose(1, 0, 2).reshape(128, 8 * 1024)
    keysT = f(peer_sub_keys)[0].transpose(1, 3, 0, 2).reshape(128, 8 * 128)
    wd = f(peer_expert_down)[0]
    wdt = np.ascontiguousarray(wd.reshape(NCGRP, 512, 8, 128).transpose(0, 3, 2, 1)).reshape(NCGRP, 128, 8 * 512)
    wu = np.ascontiguousarray(f(peer_expert_up)[0].reshape(NCGRP, 4, 128, 1024).transpose(0, 2, 1, 3)).reshape(NCGRP, 128, 4 * 1024)
    shared = dict(gvec=gvec, wall=wall, bias=bias, hc=hc, idn=idn, x0=x0, cs0=cs0, lbl=rep(f(lb_logits)),
                  grec=rep(f(rec_norm_g)), sinks=rep(f(attn_sinks)), wq=np.ascontiguousarray(wq),
                  keysT=np.ascontiguousarray(keysT), wdt=wdt, wu=wu)
    maps = []
    for core in range(NCORES):
        b, s = core // 2, core % 2
        pad_meta = np.concatenate([np.zeros((112, D), np.float32), meta], axis=0)
        xp = np.concatenate([pad_meta, x[b, 0:1920]], axis=0)
        if s == 0:
            xm = np.concatenate([pad_meta, x[b, 0:2048]], axis=0)
            p0 = 0
            valid = np.zeros((128, NPRE + 1), np.float32)
            valid[112:, NPRE] = 1.0
        else:
            xm = x[b, 1920:4096]
            p0 = 16 * 128
            valid = np.ones((128, NPRE + 1), np.float32)
            valid[0:112, 0] = 0.0
        cs = _rope_table(np.arange(p0, p0 + NBLK * 128) - 112)
        m = dict(shared)
        m.update(xm=np.ascontiguousarray(xm), xp=np.ascontiguousarray(xp), cs=cs,
                 amask=_attn_mask(s == 0), valid=valid)
        maps.append(m)
    return maps


def _run(inputs, debug_h1=False):
    key = ("nc", debug_h1)
    if key not in _NC_CACHE:
        _NC_CACHE[key] = Builder(debug_h1=debug_h1).build()
    nc = _NC_CACHE[key]
    maps = _host_inputs(**inputs)
    res = run_bass_kernel_spmd(nc, maps, core_ids=list(range(NCORES)))
    out = np.zeros((4, 4096, D), np.float32)
    for core in range(NCORES):
        b, s = core // 2, core % 2
        out[b, s * 2048:(s + 1) * 2048] = res.results[core]["out"]
    return out


def kernel(**inputs):
    return _run(inputs)
```

```python
import numpy as np
import concourse.bass as bass
import concourse.mybir as mybir
from concourse.bass_utils import run_bass_kernel_spmd
from contextlib import ExitStack

F32 = mybir.dt.float32
BF16 = mybir.dt.bfloat16
AF = mybir.ActivationFunctionType
ALU = mybir.AluOpType
AX = mybir.AxisListType

ENGS = ("sp", "act", "dve", "pool", "pe")
NDMA_SEM = 6

D = 1024
NCORES = 8
NBLK = 17
NPRE = 16
EPS = 1e-6
NEG = -1e30
CG = [(0, 512), (512, 256), (768, 512), (1280, 512), (1792, 512), (2304, 512),
      (2816, 512), (3328, 512), (3840, 512), (4352, 512)]
G_Q, G_KV, G_RQ, G_RF, G_RI, G_RG, G_GA0, G_GA1, G_GR0, G_GR1 = range(10)
P_WUA, P_WUR, P_WO = 10, 12, 14
NPIECE = 16
NCGRP = 32
TH = 1024


class Prog:
    def __init__(self, nc):
        self.nc = nc
        self.plan = {e: [] for e in ENGS}
        self.count = {e: 0 for e in ENGS}
        self.seen = {e: {} for e in ENGS}
        self.lastw = {}
        self.readers = {}
        self.dma_n = {e: 0 for e in ENGS}
        self.ninst = 0

    def _need(self, eng, waits, tok, same_ok=False):
        key, val, peng = tok
        if peng == eng and (same_ok or eng == "pe"):
            return
        if self.seen[eng].get(key, 0) >= val:
            return
        self.seen[eng][key] = val
        waits.append((key, val))

    def _deps(self, eng, reads, writes):
        waits = []
        for b in reads:
            if b in self.lastw:
                self._need(eng, waits, self.lastw[b])
            if b[0] == "B" and b[1:].isdigit():
                for tok in self.readers.get(b, ()):
                    self._need(eng, waits, tok, same_ok=True)
        for b in writes:
            if b in self.lastw:
                self._need(eng, waits, self.lastw[b])
            for tok in self.readers.get(b, ()):
                self._need(eng, waits, tok)
        return waits

    def _commit(self, tok, reads, writes):
        for b in reads:
            self.readers.setdefault(b, []).append(tok)
        for b in writes:
            self.lastw[b] = tok
            self.readers[b] = []

    def op(self, eng, fn, reads=(), writes=()):
        waits = self._deps(eng, reads, writes)
        self.count[eng] += 1
        tok = ("c_" + eng, self.count[eng], eng)
        self.plan[eng].append((waits, fn, tok[0], 1))
        self._commit(tok, reads, writes)
        self.ninst += 1

    def dma(self, eng, fn, reads=(), writes=()):
        i = self.dma_n[eng]
        self.dma_n[eng] += 1
        slot = i % NDMA_SEM
        key = "d_%s_%d" % (eng, slot)
        val = 16 * (i // NDMA_SEM + 1)
        waits = self._deps(eng, reads, writes)
        if val > 16:
            self._need(eng, waits, (key, val - 16, "dma"))
        tok = (key, val, "dma")
        self.plan[eng].append((waits, fn, key, 16))
        self._commit(tok, reads, writes)
        self.ninst += 1

    def _all_tokens(self):
        toks = []
        for q in ENGS:
            n = self.dma_n[q]
            for slot in range(min(n, NDMA_SEM)):
                uses = (n - 1 - slot) // NDMA_SEM + 1
                toks.append(("d_%s_%d" % (q, slot), 16 * uses, "dma"))
        for e in ENGS:
            if self.count[e]:
                toks.append(("c_" + e, self.count[e], "x"))
        return toks

    def barrier(self):
        toks = self._all_tokens()
        for e in ENGS:
            waits = []
            for t in toks:
                self._need(e, waits, t)
            if waits:
                self.plan[e].append((waits, None, None, 0))
        self.lastw.clear()
        self.readers.clear()

    def finish(self, eng="sp"):
        waits = []
        for t in self._all_tokens():
            self._need(eng, waits, t)
        self.plan[eng].append((waits, None, None, 0))

    def emit(self):
        nc = self.nc
        keys = set()
        marked = {e: set() for e in ENGS}
        for e in ENGS:
            for waits, fn, k, amt in self.plan[e]:
                if k:
                    keys.add(k)
                for wk, wv in waits:
                    keys.add(wk)
                    if wk.startswith("c_"):
                        marked[wk[2:]].add(wv)
        rank = {}
        for e in ENGS:
            for i, v in enumerate(sorted(marked[e])):
                rank[("c_" + e, v)] = i + 1
        with ExitStack() as es:
            sems = {k: es.enter_context(nc.semaphore(k)) for k in sorted(keys)}
            block = es.enter_context(nc.Block())
            deco = {"sp": block.sync, "act": block.scalar, "dve": block.vector,
                    "pool": block.gpsimd, "pe": block.tensor}

            def make(e):
                def body(h):
                    idx = 0
                    for waits, fn, k, amt in self.plan[e]:
                        for wk, wv in waits:
                            h.wait_ge(sems[wk], rank[(wk, wv)] if wk.startswith("c_") else wv)
                        if fn is not None:
                            ins = fn(h)
                            if amt == 16:
                                ins.then_inc(sems[k], 16)
                            else:
                                idx += 1
                                if idx in marked[e]:
                                    ins.then_inc(sems[k], 1)
                return body

            for e in ENGS:
                if self.plan[e]:
                    deco[e](make(e))


def K(name, *args, **kw):
    return lambda e: getattr(e, name)(*args, **kw)


def _ap(t, off, pat):
    return bass.AP(t, off, [list(p) for p in pat])


class Builder:
    def __init__(self, debug_h1=False):
        self.debug_h1 = debug_h1
        self.nc = bass.Bass("TRN2", target_bir_lowering=False)
        self.P = Prog(self.nc)
        self.dr = {}

    def dram(self, name, shape, dtype=F32, kind="ExternalInput"):
        t = self.nc.dram_tensor(name, list(shape), dtype, kind=kind)
        self.dr[name] = t
        return t

    def declare(self):
        d = self.dram
        d("xm", [NBLK * 128, D]); d("xp", [NPRE * 128, D]); d("x0", [128, D])
        d("gvec", [128, 3 * D])
        d("wall", [NPIECE, 128, 4096])
        d("bias", [1, 10 * 512])
        d("cs", [NBLK * 128, 16]); d("cs0", [128, 16])
        d("amask", [128, 2 * 272])
        d("valid", [128, NPRE + 1])
        d("hc", [128, 768 + 128 + 128 + 2])
        d("idn", [128, 128])
        d("lbl", [128, 1024]); d("grec", [128, 512]); d("sinks", [128, 8])
        d("wq", [128, 8 * 1024]); d("keysT", [128, 8 * 128])
        d("wdt", [NCGRP, 128, 8 * 512]); d("wu", [NCGRP, 128, 4 * 1024])
        d("out", [2048, D], kind="ExternalOutput")
        d("wsc", [NPIECE, 128, 4096], BF16, kind="Internal")
        d("wdb", [NCGRP, 128, 4096], BF16, kind="Internal")
        d("wub", [NCGRP, 128, 4096], BF16, kind="Internal")

    def build(self, maxstep=10 ** 9):
        nc, P = self.nc, self.P
        self.declare()
        self.step = 0

        def go():
            self.step += 1
            return self.step <= maxstep

        with ExitStack() as es:
            self.es = es
            self.alloc_persistent(es)
            self.load_consts()
            if go():
                self.precast_weights()
            for g in range(1 if getattr(Builder, 'only_g0', False) else 2):
                with ExitStack() as ea:
                    self.alloc_mixer(ea)
                    if g == 0:
                        self.precast_experts(ea)
                    if g == 0:
                        if go():
                            self.meta_kv()
                        for pb in range(NPRE):
                            if go():
                                self.precast_step()
                                self.pre_block(pb)
                        lbs = range(0, 9)
                    else:
                        lbs = range(9, 17)
                    for lb in lbs:
                        if go():
                            if g == 0:
                                self.precast_step()
                            self.main_block(lb, (lb - 1) % 8 if lb >= 1 else None)
                    if g == 0:
                        while self.xpos < len(self.xq) or self.xpend:
                            self.precast_step()
                    P.barrier()
                with ExitStack() as eb:
                    if self.debug_h1:
                        if go():
                            for tt in range(8):
                                r0 = (g * 8 + tt) * 128
                                P.dma("sp", K("dma_start",
                                    out=self.dr["out"].ap()[r0:r0 + 128, :], in_=self.h1[:, tt, :]),
                                    reads=["h1_%d" % tt])
                    else:
                        if go():
                            self.peer(eb, g)
                    P.barrier()
            P.finish("sp")
            with nc.allow_low_precision(reason="bf16 gate matrix: at most a few non-zero terms per cell, it is a bf16 matmul operand"):
                P.emit()
        return nc

    def sb(self, es, name, shape, dt=F32):
        self.uid = getattr(self, "uid", 0) + 1
        return es.enter_context(self.nc.sbuf_tensor("s%d_%s" % (self.uid, name), list(shape), dt))

    def alloc_persistent(self, es):
        sb = lambda n, s, d=F32: self.sb(es, n, s, d)
        nc = self.nc
        self.h1 = sb("h1", [128, 8, D])
        self.idf = sb("idf", [128, 128]); self.idb = sb("idb", [128, 128], BF16)
        self.gv = sb("gv", [128, 3, D])
        self.KT = sb("KT", [64, 2, 256], BF16)
        self.Vs = sb("Vs", [128, 2, 128], BF16)
        self.KTm = sb("KTm", [64, 2, 16], BF16); self.Vm = sb("Vm", [128, 128], BF16)
        self.S = sb("S", [128, 4, 128]); self.Sb = sb("Sb", [128, 2, 4, 128], BF16)
        self.ones = sb("ones", [128, 128], BF16)
        self.B = [es.enter_context(nc.psum_tensor("B%d" % i, [128, 512], F32)) for i in (0, 1, 3, 4, 5, 6, 7)]
        self.B.insert(2, es.enter_context(nc.psum_tensor("B2", [128, 1024], BF16)))

    def load_consts(self):
        P, dr = self.P, self.dr
        P.dma("sp", K("dma_start", out=self.idf[:], in_=dr["idn"].ap()), writes=["idf"])
        P.op("dve", K("tensor_copy", out=self.idb[:], in_=self.idf[:]), reads=["idf"], writes=["idb"])
        gsrc = dr["gvec"].ap()
        P.dma("sp", K("dma_start", out=self.gv[:].rearrange("p a b -> p (a b)"), in_=gsrc), writes=["gv"])
        P.op("dve", K("memset", self.ones[:], 0.0), writes=["ones"])
        P.op("dve", K("memset", self.ones[0:1, :], 1.0), writes=["ones"])
        P.op("dve", K("memset", self.S[:], 0.0), writes=["S"])
        P.op("pool", K("memset", self.Sb[:], 0.0), writes=["Sb0", "Sb1"])
        P.op("pool", K("memset", self.Vm[:], 0.0), writes=["Vm"])

    def precast_weights(self):
        P, dr = self.P, self.dr
        with ExitStack() as es:
            st = [self.sb(es, "pcst%d" % i, [128, 4096], BF16) for i in range(2)]
            for p in range(NPIECE):
                s = st[p % 2]
                nm = "pcst%d" % (p % 2)
                P.dma("pool", K("dma_start",
                    out=s[:].rearrange("p (a b) -> p a b", b=512),
                    in_=dr["wall"].ap()[p].rearrange("p (a b) -> p a b", b=512)), writes=[nm])
                P.dma("sp", K("dma_start", out=dr["wsc"].ap()[p], in_=s[:]),
                      reads=[nm], writes=["wsc%d" % p])
            P.barrier()

    def precast_experts(self, es):
        self.xst = [self.sb(es, "xcst%d" % i, [128, 4096], BF16) for i in range(3)]
        self.xq = [(cg, src, dst) for cg in range(NCGRP) for src, dst in (("wdt", "wdb"), ("wu", "wub"))]
        self.xpos = 0
        self.xpend = []

    def precast_step(self, n=3, flush=False):
        P, dr = self.P, self.dr
        for (t, nm, cg, dst) in self.xpend:
            P.dma("sp", K("dma_start", out=dr[dst].ap()[cg], in_=t[:]), reads=[nm], writes=["%s%d" % (dst, cg)])
        self.xpend = []
        if flush:
            n = len(self.xq) - self.xpos if False else 0
        for _ in range(n):
            if self.xpos >= len(self.xq):
                break
            cg, src, dst = self.xq[self.xpos]
            t, nm = self.xst[self.xpos % 3], "xcst%d" % (self.xpos % 3)
            self.xpos += 1
            P.dma("pool", K("dma_start", out=t[:].rearrange("p (a b) -> p a b", b=512),
                            in_=dr[src].ap()[cg].rearrange("p (a b) -> p a b", b=512)), writes=[nm])
            self.xpend.append((t, nm, cg, dst))

    def alloc_mixer(self, es):
        sb = lambda n, s, d=F32: self.sb(es, n, s, d)
        self.wbuf = [sb("wbuf%d" % i, [128, 8, 512], BF16) for i in range(3)]
        self.wslot = 0
        self.biasb = sb("biasb", [128, 5120], BF16)
        self.xs = sb("xs", [128, D]); self.junk = sb("junk", [128, D], BF16)
        self.st = sb("st", [128, 16])
        self.xnb = sb("xnb", [128, D], BF16); self.xT = sb("xT", [128, 8, 128], BF16)
        self.qf = sb("qf", [128, 8, 64]); self.qb = sb("qb", [128, 8, 64], BF16)
        self.kf = sb("kf", [128, 2, 64]); self.kb = sb("kb", [128, 2, 64], BF16)
        self.rt = sb("rt", [128, 4, 8, 8])
        self.qT = sb("qT", [64, 8, 128], BF16)
        self.sc = [sb("sc%d" % i, [128, 272]) for i in range(2)]
        self.pf = [sb("pf%d" % i, [128, 272], BF16) for i in range(2)]
        self.pn = [sb("pn%d" % i, [128, 272], BF16) for i in range(2)]
        self.pT = [sb("pT%d" % i, [128, 3, 128], BF16) for i in range(2)]
        self.sm = [sb("sm%d" % i, [128, 8]) for i in range(2)]
        self.attnT = sb("attnT", [64, 8, 128], BF16)
        self.rqb = sb("rqb", [128, 512], BF16)
        self.sg = sb("sg", [128, 512]); self.sgn = sb("sgn", [128, 512])
        self.lf = sb("lf", [128, 512]); self.kr = sb("kr", [128, 512])
        self.krb = sb("krb", [128, 512], BF16); self.vb = sb("vb", [128, 512], BF16)
        self.gs = sb("gs", [128, 512])
        self.sga = sb("sga", [128, D], BF16); self.sgr = sb("sgr", [128, D], BF16)
        self.E = sb("E", [128, 768]); self.dec = sb("dec", [128, 4, 2])
        self.Qp = sb("Qp", [128, 128], BF16); self.Qz = sb("Qz", [128, 2, 128], BF16)
        self.Kj = sb("Kj", [128, 4, 128], BF16); self.ATb = sb("ATb", [128, 128], BF16)
        self.ekd = sb("ekd", [128, 512]); self.kdec = sb("kdec", [128, 512], BF16)
        self.ms = sb("ms", [128, 8])
        self.recb = sb("recb", [128, 512], BF16); self.recT = sb("recT", [128, 4, 128], BF16)
        self.mg = sb("mg", [128, D]); self.mgb = sb("mgb", [128, D], BF16)
        self.mT = sb("mT", [128, 8, 128], BF16)
        self.lbr = sb("lbr", [128, 512]); self.oml = sb("oml", [128, 512])
        self.ll = sb("ll", [128, 2, 512]); self.grr = sb("grr", [128, 512])
        self.snk = sb("snk", [128, 8])
        self.hc = sb("hc", [128, 1026]); self.cmb = sb("cmb", [128, 128], BF16)
        self.am = sb("am", [128, 2, 272]); self.cs = sb("cs", [128, NBLK, 16]); self.cs0 = sb("cs0", [128, 16])
        self.vld = sb("vld", [128, NPRE + 1])
        P, dr = self.P, self.dr
        ld = lambda dst, src, nm: P.dma("sp", K("dma_start", out=dst, in_=src), writes=[nm])
        ld(self.hc[:], dr["hc"].ap(), "hc")
        ld(self.am[:], dr["amask"].ap().rearrange("p (a b) -> p a b", b=272), "am")
        ld(self.cs[:], dr["cs"].ap().rearrange("(n p) c -> p n c", p=128), "cs")
        ld(self.cs0[:], dr["cs0"].ap(), "cs0")
        ld(self.vld[:], dr["valid"].ap(), "vld")
        ld(self.ll[:], dr["lbl"].ap().rearrange("p (a b) -> p a b", b=512), "ll")
        ld(self.grr[:], dr["grec"].ap(), "grr")
        ld(self.snk[:], dr["sinks"].ap(), "snk")
        P.op("pool", K("memset", self.biasb[:], 0.0), writes=["biasb"])
        P.dma("pool", K("dma_start", out=self.biasb[0:1, :].rearrange("p (a b) -> p a b", b=512),
                                            in_=dr["bias"].ap().rearrange("p (a b) -> p a b", b=512)), writes=["biasb"])
        P.op("dve", K("tensor_tensor", out=self.lf[:], in0=self.ll[:, 0, :], in1=self.ll[:, 1, :], op=ALU.subtract),
             reads=["ll"], writes=["lf"])
        P.op("act", K("activation", out=self.lbr[:], in_=self.lf[:], func=AF.Sigmoid), reads=["lf"], writes=["lbr"])
        P.op("act", K("activation", out=self.oml[:], in_=self.lf[:], func=AF.Sigmoid, scale=-1.0),
             reads=["lf"], writes=["oml"])
        P.op("dve", K("tensor_copy", out=self.cmb[:], in_=self.hc[:, 896:1024]), reads=["hc"], writes=["cmb"])
        P.op("pool", K("memset", self.Qz[:], 0.0), writes=["Qz"])
        for i in range(2):
            P.op("pool", K("memset", self.pT[i][:], 0.0), writes=["pT%d" % i])

    def wload(self, piece):
        i = self.wslot % 3
        self.wslot += 1
        t, nm = self.wbuf[i], "wbuf%d" % i
        self.P.dma("sp", K("dma_start",
            out=t[:], in_=self.dr["wsc"].ap()[piece].rearrange("p (a b) -> p a b", b=512)),
            reads=["wsc%d" % piece], writes=[nm])
        return t, nm

    def norm_transpose(self, src_ap, gidx):
        P = self.P
        P.dma("act", K("dma_start", out=self.xs[:], in_=src_ap), writes=["xs"])
        st = self.st
        P.op("act", K("activation", out=self.junk[:], in_=self.xs[:], func=AF.Square, accum_out=st[:, 0:1]),
             reads=["xs"], writes=["junk", "st"])
        P.op("act", K("activation", out=st[:, 1:2], in_=st[:, 0:1], func=AF.Sqrt, scale=1.0 / D, bias=EPS),
             reads=["st"], writes=["st"])
        P.op("dve", K("reciprocal", out=st[:, 2:3], in_=st[:, 1:2]), reads=["st"], writes=["st"])
        P.op("dve", K("scalar_tensor_tensor", out=self.xnb[:], in0=self.xs[:], scalar=st[:, 2:3],
                                                     in1=self.gv[:, gidx, :], op0=ALU.mult, op1=ALU.mult),
             reads=["xs", "st", "gv"], writes=["xnb"])
        self.transpose8(self.xnb, "xnb", self.xT, "xT")

    def transpose8(self, src, sname, dst, dname):
        P, B2 = self.P, self.B[2]
        for c in range(8):
            P.op("pe", K("transpose", out=B2[:, c * 128:(c + 1) * 128], in_=src[:, c * 128:(c + 1) * 128],
                                                  identity=self.idb[:]), reads=[sname, "idb"], writes=["B2"])
        P.op("act", K("copy", out=dst[:], in_=B2[:].rearrange("p (a b) -> p a b", b=128)),
             reads=["B2"], writes=[dname])

    def project(self, grp, bank):
        P = self.P
        c0, n = CG[grp]
        w, wn = self.wload(grp)
        pb, bn = self.B[bank], "B%d" % bank
        for dc in range(8):
            P.op("pe", K("matmul", pb[:, 0:n], lhsT=self.xT[:, dc, :], rhs=w[:, dc, 0:n],
                                                 start=(dc == 0), stop=False), reads=["xT", wn], writes=[bn])
        P.op("pe", K("matmul", pb[:, 0:n], lhsT=self.ones[:, :], rhs=self.biasb[:, grp * 512:grp * 512 + n],
                                      start=False, stop=True), reads=["ones", "biasb"], writes=[bn])
        return pb, bn

    def rope(self, srcf, dstb, nh, cs_ap, names):
        P = self.P
        sn, dn = names
        cs_t, cs_off = cs_ap.tensor, cs_ap.offset
        pstep = cs_ap.ap[0][0]
        cosb = _ap(cs_t, cs_off, [[pstep, 128], [0, nh], [1, 8]])
        sinb = _ap(cs_t, cs_off + 8, [[pstep, 128], [0, nh], [1, 8]])
        t1, t2 = srcf[:, :, 0:8], srcf[:, :, 8:16]
        rt = self.rt
        P.op("dve", K("tensor_copy", out=dstb[:], in_=srcf[:]), reads=[sn], writes=[dn])
        P.op("dve", K("tensor_tensor", out=rt[:, 0, 0:nh, :], in0=t1, in1=cosb, op=ALU.mult), reads=[sn, "cs", "cs0"], writes=["rt"])
        P.op("dve", K("tensor_tensor", out=rt[:, 1, 0:nh, :], in0=t2, in1=sinb, op=ALU.mult), reads=[sn, "cs", "cs0"], writes=["rt"])
        P.op("dve", K("tensor_tensor", out=rt[:, 2, 0:nh, :], in0=t2, in1=cosb, op=ALU.mult), reads=[sn, "cs", "cs0"], writes=["rt"])
        P.op("dve", K("tensor_tensor", out=rt[:, 3, 0:nh, :], in0=t1, in1=sinb, op=ALU.mult), reads=[sn, "cs", "cs0"], writes=["rt"])
        P.op("dve", K("tensor_tensor", out=dstb[:, :, 0:8], in0=rt[:, 0, 0:nh, :], in1=rt[:, 1, 0:nh, :], op=ALU.subtract),
             reads=["rt"], writes=[dn])
        P.op("dve", K("tensor_tensor", out=dstb[:, :, 8:16], in0=rt[:, 2, 0:nh, :], in1=rt[:, 3, 0:nh, :], op=ALU.add),
             reads=["rt"], writes=[dn])

    def kv_from_bank(self, pb, bn, cs_ap, vdst, vname):
        P = self.P
        P.op("act", K("copy", out=self.kf[:], in_=pb[:, 0:128].rearrange("p (a b) -> p a b", b=64)),
             reads=[bn], writes=["kf"])
        P.op("act", K("copy", out=vdst, in_=pb[:, 128:256]), reads=[bn], writes=[vname])
        self.rope(self.kf, self.kb, 2, cs_ap, ("kf", "kb"))

    def meta_kv(self):
        P, B2 = self.P, self.B[2]
        self.norm_transpose(self.dr["x0"].ap(), 0)
        pb, bn = self.project(G_KV, 0)
        self.kv_from_bank(pb, bn, self.cs0[:, :], self.vb[:, 0:128], "vb")
        P.op("dve", K("tensor_copy", out=self.Vm[0:16, :], in_=self.vb[0:16, 0:128]), reads=["vb"], writes=["Vm"])
        for g in range(2):
            P.op("pe", K("transpose", out=B2[0:64, g * 128:(g + 1) * 128], in_=self.kb[:, g, :],
                                                  identity=self.idb[:]), reads=["kb", "idb"], writes=["B2"])
        P.op("act", K("copy", out=self.KTm[:], in_=B2[0:64, 0:256].rearrange("p (a b) -> p a b", b=128)[:, :, 0:16]),
             reads=["B2"], writes=["KTm"])

    def hgrn_gates(self, vcol):
        P = self.P
        pb, bn = self.project(G_RF, 0)
        P.op("act", K("activation", out=self.sg[:], in_=pb[:], func=AF.Sigmoid), reads=[bn], writes=["sg"])
        P.op("act", K("activation", out=self.sgn[:], in_=pb[:], func=AF.Sigmoid, scale=-1.0), reads=[bn], writes=["sgn"])
        pb1, bn1 = self.project(G_RI, 1)
        P.op("act", K("copy", out=self.vb[:], in_=pb1[:]), reads=[bn1], writes=["vb"])
        P.op("dve", K("tensor_tensor", out=self.sg[:], in0=self.sg[:], in1=self.oml[:], op=ALU.mult),
             reads=["sg", "oml"], writes=["sg"])
        P.op("dve", K("tensor_tensor", out=self.sg[:], in0=self.sg[:], in1=self.lbr[:], op=ALU.add),
             reads=["sg", "lbr"], writes=["sg"])
        P.op("act", K("activation", out=self.lf[:], in_=self.sg[:], func=AF.Ln), reads=["sg"], writes=["lf"])
        P.op("dve", K("tensor_tensor", out=self.kr[:], in0=self.sgn[:], in1=self.oml[:], op=ALU.mult),
             reads=["sgn", "oml"], writes=["kr"])
        if vcol is not None:
            vs = self.vld[:, vcol:vcol + 1]
            P.op("dve", K("tensor_scalar", out=self.lf[:], in0=self.lf[:], scalar1=vs, scalar2=None, op0=ALU.mult),
                 reads=["lf", "vld"], writes=["lf"])
            P.op("dve", K("tensor_scalar", out=self.kr[:], in0=self.kr[:], scalar1=vs, scalar2=None, op0=ALU.mult),
                 reads=["kr", "vld"], writes=["kr"])

    def hgrn_kdec(self):
        P = self.P
        pX = self.B[7]
        P.op("pe", K("matmul", pX[:], lhsT=self.hc[:, 768:896], rhs=self.lf[:], start=True, stop=True),
             reads=["hc", "lf"], writes=["B7"])
        P.op("act", K("activation", out=self.ekd[:], in_=pX[:], func=AF.Exp), reads=["B7"], writes=["ekd"])
        P.op("dve", K("tensor_tensor", out=self.kdec[:], in0=self.ekd[:], in1=self.kr[:], op=ALU.mult),
             reads=["ekd", "kr"], writes=["kdec"])

    def hgrn_state_update(self, h, c, sb_dst):
        P = self.P
        pd = self.B[5]
        r0 = c * 64
        P.op("pe", K("matmul", pd[:, 128:256], lhsT=self.kdec[r0:r0 + 64, h * 128:(h + 1) * 128],
                                      rhs=self.vb[r0:r0 + 64, h * 128:(h + 1) * 128], start=True, stop=True),
             reads=["kdec", "vb"], writes=["B5"])
        P.op("dve", K("scalar_tensor_tensor", out=self.S[:, h, :], in0=self.S[:, h, :], scalar=self.dec[:, h, c:c + 1],
                                                     in1=pd[:, 128:256], op0=ALU.mult, op1=ALU.add),
             reads=["S", "dec", "B5"], writes=["S"])
        if sb_dst is not None:
            P.op("act", K("copy", out=self.Sb[:, sb_dst, h, :], in_=self.S[:, h, :]),
                 reads=["S"], writes=["Sb%d" % sb_dst])

    def pre_block(self, pb_i):
        P = self.P
        sub = getattr(self, "maxsub", 99)
        self.norm_transpose(self.dr["xp"].ap()[pb_i * 128:(pb_i + 1) * 128, :], 0)
        if sub < 1:
            return
        self.hgrn_gates(pb_i)
        if sub < 2:
            return
        self.hgrn_kdec()
        if sub < 3:
            return
        pE = self.B[3]
        for h in range(4):
            P.op("pe", K("matmul", pE[:, 2 * h:2 * h + 2], lhsT=self.lf[:, h * 128:(h + 1) * 128],
                                               rhs=self.hc[:, 1024:1026], start=True, stop=True),
                 reads=["lf", "hc"], writes=["B3"])
        P.op("act", K("activation", out=self.dec[:], in_=pE[:, 0:8].rearrange("p (a b) -> p a b", b=2), func=AF.Exp),
             reads=["B3"], writes=["dec"])
        if sub < 4:
            return
        last = pb_i == NPRE - 1
        for c in range(2):
            if sub < 5 and c == 1:
                return
            for h in range(4):
                self.hgrn_state_update(h, c, 0 if (last and c == 1) else None)

    def main_block(self, lb, tt):
        P, B = self.P, self.B
        do_out = lb >= 1
        cs_ap = self.cs[:, lb, :]
        self.norm_transpose(self.dr["xm"].ap()[lb * 128:(lb + 1) * 128, :], 0)
        cur = lb % 2
        if lb >= 1:
            P.op("dve", K("tensor_copy", out=self.KT[:, :, 0:128], in_=self.KT[:, :, 128:256]),
                 reads=["KT"], writes=["KT"])
        pb, bn = self.project(G_KV, 0)
        self.kv_from_bank(pb, bn, cs_ap, self.Vs[:, cur, :], "Vs%d" % cur)
        for g in range(2):
            P.op("pe", K("transpose", out=B[2][0:64, g * 128:(g + 1) * 128], in_=self.kb[:, g, :],
                                                  identity=self.idb[:]), reads=["kb", "idb"], writes=["B2"])
        P.op("act", K("copy", out=self.KT[:, :, 128:256], in_=B[2][0:64, 0:256].rearrange("p (a b) -> p a b", b=128)),
             reads=["B2"], writes=["KT"])
        sub = getattr(self, "maxsub", 99)
        if do_out and sub >= 11:
            self.attention(lb, cs_ap)
        if sub >= 13 or not do_out:
            self.hgrn(lb, do_out)
        if do_out and sub >= 14:
            self.merge_out(lb, tt)

    def attention(self, lb, cs_ap):
        P, B = self.P, self.B
        pb, bn = self.project(G_Q, 1)
        P.op("act", K("copy", out=self.qf[:], in_=pb[:].rearrange("p (a b) -> p a b", b=64)), reads=[bn], writes=["qf"])
        self.rope(self.qf, self.qb, 8, cs_ap, ("qf", "qb"))
        for h in range(8):
            P.op("pe", K("transpose", out=B[2][0:64, h * 128:(h + 1) * 128], in_=self.qb[:, h, :],
                                                  identity=self.idb[:]), reads=["qb", "idb"], writes=["B2"])
        P.op("act", K("copy", out=self.qT[:], in_=B[2][0:64, :].rearrange("p (a b) -> p a b", b=128)),
             reads=["B2"], writes=["qT"])
        mi = 0 if lb == 1 else 1
        prv, cur = (lb - 1) % 2, lb % 2
        if getattr(self, "maxsub", 99) < 12:
            return
        for h in range(8):
            g, i = h // 4, h % 2
            pS, sn = B[3 + i], "B%d" % (3 + i)
            sc, pf, pn, pT, sm = self.sc[i], self.pf[i], self.pn[i], self.pT[i], self.sm[i]
            scn, pfn, pnn, pTn, smn = "sc%d" % i, "pf%d" % i, "pn%d" % i, "pT%d" % i, "sm%d" % i
            P.op("pe", K("matmul", pS[:, 0:256], lhsT=self.qT[:, h, :], rhs=self.KT[:, g, :],
                                                           start=True, stop=True), reads=["qT", "KT"], writes=[sn])
            P.op("pe", K("matmul", pS[:, 256:272], lhsT=self.qT[:, h, :], rhs=self.KTm[:, g, :],
                                                           start=True, stop=True), reads=["qT", "KTm"], writes=[sn])
            P.op("dve", K("scalar_tensor_tensor", out=sc[:], in0=pS[:, 0:272], scalar=0.125,
                                                                       in1=self.am[:, mi, :], op0=ALU.mult, op1=ALU.add),
                 reads=[sn, "am"], writes=[scn])
            P.op("dve", K("tensor_reduce", out=sm[:, 0:1], in_=sc[:], axis=AX.X, op=ALU.max),
                 reads=[scn], writes=[smn])
            P.op("dve", K("tensor_tensor", out=sm[:, 1:2], in0=sm[:, 0:1], in1=self.snk[:, h:h + 1], op=ALU.max),
                 reads=[smn, "snk"], writes=[smn])
            P.op("dve", K("tensor_scalar", out=sm[:, 2:3], in0=sm[:, 1:2], scalar1=-1.0, scalar2=None, op0=ALU.mult),
                 reads=[smn], writes=[smn])
            P.op("act", K("activation", out=pf[:], in_=sc[:], func=AF.Exp, bias=sm[:, 2:3],
                                                                   accum_out=sm[:, 3:4]), reads=[scn, smn], writes=[pfn, smn])
            P.op("act", K("activation", out=sm[:, 4:5], in_=self.snk[:, h:h + 1], func=AF.Exp, bias=sm[:, 2:3]),
                 reads=["snk", smn], writes=[smn])
            P.op("dve", K("tensor_tensor", out=sm[:, 5:6], in0=sm[:, 3:4], in1=sm[:, 4:5], op=ALU.add),
                 reads=[smn], writes=[smn])
            P.op("dve", K("reciprocal", out=sm[:, 6:7], in_=sm[:, 5:6]), reads=[smn], writes=[smn])
            P.op("dve", K("tensor_scalar", out=pn[:], in0=pf[:], scalar1=sm[:, 6:7], scalar2=None,
                                                                      op0=ALU.mult), reads=[pfn, smn], writes=[pnn])
            for j, (c0, n) in enumerate(((0, 128), (128, 128), (256, 16))):
                P.op("pe", K("transpose", out=B[2][0:n, j * 128:(j + 1) * 128], in_=pn[:, c0:c0 + n],
                                                                          identity=self.idb[:]), reads=[pnn, "idb"], writes=["B2"])
            P.op("act", K("copy", out=pT[:, 0:2, :], in_=B[2][:, 0:256].rearrange("p (a b) -> p a b", b=128)),
                 reads=["B2"], writes=[pTn])
            P.op("act", K("copy", out=pT[0:16, 2, :], in_=B[2][0:16, 256:384]), reads=["B2"], writes=[pTn])
            po = B[5][0:64, 256:384]
            gs_ = slice(g * 64, (g + 1) * 64)
            P.op("pe", K("matmul", po, lhsT=self.Vs[:, prv, gs_], rhs=pT[:, 0, :], start=True, stop=False),
                 reads=["Vs%d" % prv, pTn], writes=["B5"])
            P.op("pe", K("matmul", po, lhsT=self.Vs[:, cur, gs_], rhs=pT[:, 1, :], start=False, stop=False),
                 reads=["Vs%d" % cur, pTn], writes=["B5"])
            P.op("pe", K("matmul", po, lhsT=self.Vm[:, gs_], rhs=pT[:, 2, :], start=False, stop=True),
                 reads=["Vm", pTn], writes=["B5"])
            P.op("act", K("copy", out=self.attnT[:, h, :], in_=po), reads=["B5"], writes=["attnT"])

    def hgrn(self, lb, do_out):
        P, B = self.P, self.B
        self.hgrn_gates(NPRE if lb == 0 else None)
        self.hgrn_kdec()
        if not do_out:
            pE = B[3]
            for h in range(4):
                P.op("pe", K("matmul", pE[:, 2 * h:2 * h + 2], lhsT=self.lf[:, h * 128:(h + 1) * 128],
                                                   rhs=self.hc[:, 1024:1026], start=True, stop=True),
                     reads=["lf", "hc"], writes=["B3"])
            P.op("act", K("activation", out=self.dec[:], in_=pE[:, 0:8].rearrange("p (a b) -> p a b", b=2), func=AF.Exp),
                 reads=["B3"], writes=["dec"])
            for c in range(2):
                for h in range(4):
                    self.hgrn_state_update(h, c, 0 if c == 1 else None)
            return
        pb, bn = self.project(G_RQ, 0)
        P.op("act", K("copy", out=self.rqb[:], in_=pb[:]), reads=[bn], writes=["rqb"])
        pb, bn = self.project(G_RG, 1)
        P.op("act", K("activation", out=self.gs[:], in_=pb[:], func=AF.Silu), reads=[bn], writes=["gs"])
        P.op("dve", K("tensor_tensor", out=self.gs[:], in0=self.gs[:], in1=self.grr[:], op=ALU.mult),
             reads=["gs", "grr"], writes=["gs"])
        P.op("dve", K("tensor_copy", out=self.krb[:], in_=self.kr[:]), reads=["kr"], writes=["krb"])
        pO = B[6]
        E = self.E
        for h in range(4):
            hs = slice(h * 128, (h + 1) * 128)
            P.op("pe", K("matmul", B[3][:], lhsT=self.lf[:, hs], rhs=self.hc[:, 0:512], start=True, stop=True),
                 reads=["lf", "hc"], writes=["B3"])
            P.op("pe", K("matmul", B[4][:, 0:256], lhsT=self.lf[:, hs], rhs=self.hc[:, 512:768], start=True, stop=True),
                 reads=["lf", "hc"], writes=["B4"])
            P.op("act", K("activation", out=E[:, 0:512], in_=B[3][:], func=AF.Exp), reads=["B3"], writes=["E"])
            P.op("act", K("activation", out=E[:, 512:768], in_=B[4][:, 0:256], func=AF.Exp), reads=["B4"], writes=["E"])
            P.op("dve", K("tensor_copy", out=self.dec[:, h, 0:1], in_=E[:, 191:192]), reads=["E"], writes=["dec"])
            P.op("dve", K("tensor_copy", out=self.dec[:, h, 1:2], in_=E[:, 255:256]), reads=["E"], writes=["dec"])
            P.op("pe", K("transpose", out=B[2][:, 0:128], in_=self.rqb[:, hs], identity=self.idb[:]),
                 reads=["rqb", "idb"], writes=["B2"])
            P.op("pe", K("transpose", out=B[2][:, 128:256], in_=self.krb[:, hs], identity=self.idb[:]),
                 reads=["krb", "idb"], writes=["B2"])
            qTp, kTp = B[2][:, 0:128], B[2][:, 128:256]
            P.op("dve", K("tensor_tensor", out=self.Qp[:], in0=E[:, 0:128], in1=qTp, op=ALU.mult),
                 reads=["E", "B2"], writes=["Qp"])
            P.op("dve", K("tensor_tensor", out=self.Qz[:, 0, 0:64], in0=E[:, 128:192], in1=B[2][:, 0:64], op=ALU.mult),
                 reads=["E", "B2"], writes=["Qz"])
            P.op("dve", K("tensor_tensor", out=self.Qz[:, 1, 64:128], in0=E[:, 192:256], in1=B[2][:, 64:128], op=ALU.mult),
                 reads=["E", "B2"], writes=["Qz"])
            kTb = _ap(B[2], 128, [[1024, 128], [0, 4], [1, 128]])
            P.op("dve", K("tensor_tensor", out=self.Kj[:], in0=E[:, 256:768].rearrange("p (a b) -> p a b", b=128),
                                                  in1=kTb, op=ALU.mult), reads=["E", "B2"], writes=["Kj"])
            pA = B[5][:, 0:128]
            for j in range(4):
                for c in range(2):
                    t0 = c * 64 + 16 * j
                    P.op("pe", K("matmul", B[5][:, t0:t0 + 16], lhsT=self.Kj[:, j, :], rhs=self.Qp[:, t0:t0 + 16],
                                                              start=True, stop=True), reads=["Kj", "Qp"], writes=["B5"])
            P.op("dve", K("tensor_tensor", out=self.ATb[:], in0=pA, in1=self.cmb[:], op=ALU.mult),
                 reads=["B5", "cmb"], writes=["ATb"])
            P.op("pe", K("matmul", pO[:, hs], lhsT=self.ATb[:], rhs=self.vb[:, hs], start=True, stop=False),
                 reads=["ATb", "vb"], writes=["B6"])
            P.op("pe", K("matmul", pO[:, hs], lhsT=self.Qz[:, 0, :], rhs=self.Sb[:, 0, h, :], start=False, stop=False),
                 reads=["Qz", "Sb0"], writes=["B6"])
            self.hgrn_state_update(h, 0, 1)
            P.op("pe", K("matmul", pO[:, hs], lhsT=self.Qz[:, 1, :], rhs=self.Sb[:, 1, h, :], start=False, stop=True),
                 reads=["Qz", "Sb1"], writes=["B6"])
            self.hgrn_state_update(h, 1, 0)
        ms = self.ms
        for h in range(4):
            hs = slice(h * 128, (h + 1) * 128)
            P.op("act", K("activation", out=self.junk[:, hs], in_=pO[:, hs], func=AF.Square, accum_out=ms[:, h:h + 1]),
                 reads=["B6"], writes=["junk", "ms"])
        P.op("act", K("activation", out=ms[:, 4:8], in_=ms[:, 0:4], func=AF.Sqrt, scale=1.0 / 128, bias=EPS),
             reads=["ms"], writes=["ms"])
        P.op("dve", K("reciprocal", out=ms[:, 0:4], in_=ms[:, 4:8]), reads=["ms"], writes=["ms"])
        for h in range(4):
            hs = slice(h * 128, (h + 1) * 128)
            P.op("dve", K("scalar_tensor_tensor", out=self.recb[:, hs], in0=pO[:, hs], scalar=ms[:, h:h + 1],
                                                                     in1=self.gs[:, hs], op0=ALU.mult, op1=ALU.mult),
                 reads=["B6", "ms", "gs"], writes=["recb"])
        for c in range(4):
            P.op("pe", K("transpose", out=B[2][:, c * 128:(c + 1) * 128], in_=self.recb[:, c * 128:(c + 1) * 128],
                                                  identity=self.idb[:]), reads=["recb", "idb"], writes=["B2"])
        P.op("act", K("copy", out=self.recT[:], in_=B[2][:, 0:512].rearrange("p (a b) -> p a b", b=128)),
             reads=["B2"], writes=["recT"])

    def merge_out(self, lb, tt):
        P, B = self.P, self.B
        for i, (grp, dst, dn) in enumerate(((G_GA0, self.sga[:, 0:512], "sga"), (G_GA1, self.sga[:, 512:1024], "sga"),
                                            (G_GR0, self.sgr[:, 0:512], "sgr"), (G_GR1, self.sgr[:, 512:1024], "sgr"))):
            pb, bn = self.project(grp, i % 2)
            P.op("act", K("activation", out=dst, in_=pb[:], func=AF.Sigmoid), reads=[bn], writes=[dn])
        for half in range(2):
            w, wn = self.wload(P_WUA + half)
            pU, un = B[6 + half], "B%d" % (6 + half)
            for h in range(8):
                P.op("pe", K("matmul", pU[:], lhsT=self.attnT[:, h, :], rhs=w[0:64, h, :],
                                                               start=(h == 0), stop=(h == 7)), reads=["attnT", wn], writes=[un])
            hs = slice(half * 512, (half + 1) * 512)
            P.op("dve", K("tensor_tensor", out=self.mg[:, hs], in0=self.sga[:, hs], in1=pU[:], op=ALU.mult),
                 reads=["sga", un], writes=["mg"])
        for half in range(2):
            w, wn = self.wload(P_WUR + half)
            pU, un = B[6 + half], "B%d" % (6 + half)
            for c in range(4):
                P.op("pe", K("matmul", pU[:], lhsT=self.recT[:, c, :], rhs=w[:, c, :],
                                                               start=(c == 0), stop=(c == 3)), reads=["recT", wn], writes=[un])
            hs = slice(half * 512, (half + 1) * 512)
            P.op("dve", K("tensor_tensor", out=self.junk[:, hs], in0=self.sgr[:, hs], in1=pU[:], op=ALU.mult),
                 reads=["sgr", un], writes=["junk"])
            P.op("dve", K("tensor_tensor", out=self.mgb[:, hs], in0=self.mg[:, hs], in1=self.junk[:, hs], op=ALU.add),
                 reads=["mg", "junk"], writes=["mgb"])
        self.transpose8(self.mgb, "mgb", self.mT, "mT")
        for half in range(2):
            w, wn = self.wload(P_WO + half)
            pU, un = B[6 + half], "B%d" % (6 + half)
            for dc in range(8):
                P.op("pe", K("matmul", pU[:], lhsT=self.mT[:, dc, :], rhs=w[:, dc, :],
                                                                 start=(dc == 0), stop=(dc == 7)), reads=["mT", wn], writes=[un])
            hs = slice(half * 512, (half + 1) * 512)
            P.op("dve", K("tensor_tensor", out=self.h1[:, tt, hs], in0=self.xs[:, hs], in1=pU[:], op=ALU.add),
                 reads=["xs", un], writes=["h1_%d" % tt])

    def peer(self, es, g):
        P, B, dr = self.P, self.B, self.dr
        sb = lambda n, s, d=F32: self.sb(es, n, s, d)
        NT = 8
        xn2T = sb("xn2T", [128, 8, 1024], BF16)
        S1 = sb("S1", [128, NT * 8 * 128]); S2 = sb("S2", [128, NT * 8 * 128])
        TAU = sb("TAU", [128, NT, 8]); NLSE = sb("NLSE", [128, NT, 8])
        pst = sb("pst", [128, 16])
        SP = NT * 8 * 128
        with ExitStack() as e1:
            sb1 = lambda n, s, d=F32: self.sb(e1, n, s, d)
            wq = sb1("wq", [128, 8, 1024]); keysT = sb1("keysT", [128, 8, 128])
            xnf = sb1("xnf", [128, D]); xTf = sb1("xTf", [128, 8, 128]); qT = sb1("qT2", [128, 8, 128])
            junk = sb1("junk2", [128, D], BF16)
            wk = sb1("wk", [128, 256]); t16 = sb1("t16", [128, 2, 8, 16])
            cand = sb1("cand", [128, 8, 256]); best = sb1("best", [128, 8, 16]); eb = sb1("eb", [128, 8, 16])
            zz = sb1("zz", [128, 16])
            P.dma("sp", K("dma_start", out=wq[:].rearrange("p a b -> p (a b)"), in_=dr["wq"].ap()), writes=["wq"])
            P.dma("sp", K("dma_start", out=keysT[:].rearrange("p a b -> p (a b)"), in_=dr["keysT"].ap()), writes=["keysT"])
            for tt in range(NT):
                hx = self.h1[:, tt, :]
                hn = "h1_%d" % tt
                if getattr(self, "maxsub", 99) < 19:
                    continue
                P.op("act", K("activation", out=junk[:], in_=hx, func=AF.Square, accum_out=pst[:, 0:1]),
                     reads=[hn], writes=["junk2", "pst"])
                P.op("act", K("activation", out=pst[:, 1:2], in_=pst[:, 0:1], func=AF.Sqrt, scale=1.0 / D, bias=EPS),
                     reads=["pst"], writes=["pst"])
                P.op("dve", K("reciprocal", out=pst[:, 2:3], in_=pst[:, 1:2]), reads=["pst"], writes=["pst"])
                P.op("dve", K("scalar_tensor_tensor", out=xnf[:], in0=hx, scalar=pst[:, 2:3], in1=self.gv[:, 1, :],
                              op0=ALU.mult, op1=ALU.mult), reads=[hn, "pst", "gv"], writes=["xnf"])
                if getattr(self, "maxsub", 99) < 20:
                    continue
                for c in range(8):
                    bk = c // 4
                    P.op("pe", K("matmul", B[bk][:, (c % 4) * 128:(c % 4 + 1) * 128], lhsT=xnf[:, c * 128:(c + 1) * 128],
                                 rhs=self.idf[:], start=True, stop=True), reads=["xnf", "idf"], writes=["B%d" % bk])
                if getattr(self, "maxsub", 99) < 21:
                    continue
                for bk in range(2):
                    src = B[bk][:].rearrange("p (a b) -> p a b", b=128)
                    P.op("act", K("copy", out=xTf[:, bk * 4:bk * 4 + 4, :], in_=src), reads=["B%d" % bk], writes=["xTf"])
                    P.op("dve", K("tensor_copy", out=xn2T[:, bk * 4:bk * 4 + 4, tt * 128:(tt + 1) * 128], in_=src),
                         reads=["B%d" % bk], writes=["xn2T"])
                if getattr(self, "maxsub", 99) < 22:
                    continue
                for h in range(8):
                    bk = 3 + h // 4
                    for dc in range(8):
                        P.op("pe", K("matmul", B[bk][:, (h % 4) * 128:(h % 4 + 1) * 128], lhsT=wq[:, dc, h * 128:(h + 1) * 128],
                                     rhs=xTf[:, dc, :], start=(dc == 0), stop=(dc == 7)), reads=["wq", "xTf"], writes=["B%d" % bk])
                for i in range(2):
                    P.op("act", K("copy", out=qT[:, i * 4:i * 4 + 4, :], in_=B[3 + i][:].rearrange("p (a b) -> p a b", b=128)),
                         reads=["B%d" % (3 + i)], writes=["qT2"])
                if getattr(self, "maxsub", 99) < 23:
                    continue
                for p, Sx, sn in ((0, S1, "S1"), (1, S2, "S2")):
                    for h in range(8):
                        bk = 5 + h // 4
                        P.op("pe", K("matmul", B[bk][:, (h % 4) * 128:(h % 4 + 1) * 128], lhsT=qT[p * 64:(p + 1) * 64, h, :],
                                     rhs=keysT[p * 64:(p + 1) * 64, h, :], start=True, stop=True),
                             reads=["qT2", "keysT"], writes=["B%d" % bk])
                    for i in range(2):
                        o0 = (tt * 8 + i * 4) * 128
                        P.op("act", K("copy", out=Sx[:, o0:o0 + 512], in_=B[5 + i][:]), reads=["B%d" % (5 + i)], writes=[sn])
                if getattr(self, "maxsub", 99) < 24:
                    continue
                for p, Sx, sn in ((0, S1, "S1"), (1, S2, "S2")):
                    for h in range(8):
                        o0 = (tt * 8 + h) * 128
                        src = Sx[:, o0:o0 + 128]
                        P.op("dve", K("max", out=t16[:, p, h, 0:8], in_=src), reads=[sn], writes=["t16"])
                        P.op("dve", K("match_replace", out=wk[:, 0:128], in_to_replace=t16[:, p, h, 0:8], in_values=src,
                                      imm_value=NEG), reads=[sn, "t16"], writes=["wk"])
                        P.op("dve", K("max", out=t16[:, p, h, 8:16], in_=wk[:, 0:128]), reads=["wk"], writes=["t16"])
                if getattr(self, "maxsub", 99) < 25:
                    continue
                a0 = _ap(t16, 0, [[256, 128], [16, 8], [1, 16], [0, 16]])
                a1 = _ap(t16, 128, [[256, 128], [16, 8], [0, 16], [1, 16]])
                co = _ap(cand, 0, [[2048, 128], [256, 8], [16, 16], [1, 16]])
                P.op("dve", K("tensor_tensor", out=co, in0=a0, in1=a1, op=ALU.add), reads=["t16"], writes=["cand"])
                for h in range(8):
                    P.op("dve", K("max", out=best[:, h, 0:8], in_=cand[:, h, :]), reads=["cand"], writes=["best"])
                    P.op("dve", K("match_replace", out=wk[:, 0:256], in_to_replace=best[:, h, 0:8], in_values=cand[:, h, :],
                                  imm_value=NEG), reads=["cand", "best"], writes=["wk"])
                    P.op("dve", K("max", out=best[:, h, 8:16], in_=wk[:, 0:256]), reads=["wk"], writes=["best"])
                P.op("dve", K("tensor_copy", out=TAU[:, tt, :], in_=best[:, :, 15]), reads=["best"], writes=["TAU"])
                bm = _ap(best, 0, [[128, 128], [16, 8], [0, 16]])
                P.op("dve", K("tensor_tensor", out=eb[:], in0=best[:], in1=bm, op=ALU.subtract), reads=["best"], writes=["eb"])
                P.op("act", K("activation", out=eb[:], in_=eb[:], func=AF.Exp), reads=["eb"], writes=["eb"])
                P.op("dve", K("tensor_reduce", out=zz[:, 0:8], in_=eb[:], axis=AX.X, op=ALU.add), reads=["eb"], writes=["zz"])
                P.op("act", K("activation", out=zz[:, 8:16], in_=zz[:, 0:8], func=AF.Ln), reads=["zz"], writes=["zz"])
                P.op("dve", K("scalar_tensor_tensor", out=NLSE[:, tt, :], in0=zz[:, 8:16], scalar=-1.0, in1=best[:, :, 0],
                              op0=ALU.mult, op1=ALU.subtract), reads=["zz", "best"], writes=["NLSE"])
            P.barrier()
        with ExitStack() as e2:
            sb2 = lambda n, s, d=F32: self.sb(e2, n, s, d)
            wd = [sb2("wd%d" % i, [128, 8, 512], BF16) for i in range(2)]
            wu = [sb2("wu%d" % i, [128, 4, 1024], BF16) for i in range(2)]
            sm = [sb2("sum%d" % i, [128, 2, 512]) for i in range(2)]
            gt = [sb2("gate%d" % i, [128, 2, 512], BF16) for i in range(2)]
            Ghs = [sb2("Gh%d" % i, [128, 8, 512], BF16) for i in range(2)]
            GTs = [sb2("GTs%d" % i, [128, 4, 512], BF16) for i in range(2)]
            gel = [sb2("gel%d" % i, [128, 512], BF16) for i in range(2)]
            AT = [sb2("AT", [128, 4, 512], BF16)] * 2
            ob = _ap(sm[0], 0, [[1024, 128], [1, 1024]])
            SUM_ENG = "pool"
            cnt = {"s": 0, "y": 0}

            def wfetch(cg):
                i = cg % 2
                P.dma("sp", K("dma_start", out=wd[i][:].rearrange("p a b -> p (a b)"), in_=dr["wdb"].ap()[cg]),
                      reads=["wdb%d" % cg], writes=["wd%d" % i])
                P.dma("sp", K("dma_start", out=wu[i][:].rearrange("p a b -> p (a b)"), in_=dr["wub"].ap()[cg]),
                      reads=["wub%d" % cg], writes=["wu%d" % i])

            def stage_a(u, j):
                cg, tb = u // 2, u % 2
                tt = tb * 4 + j
                gq = (u * 4 + j) % 2
                Gh, ghn = Ghs[gq], "Gh%d" % gq
                ks = []
                for hp in range(4):
                    ks.append(cnt["s"] % 2)
                    cnt["s"] += 1

                def e_sum(hp):
                    k = ks[hp]
                    o1 = (tt * 8 + 2 * hp) * 128 + 4 * cg
                    o2 = (tt * 8 + 2 * hp) * 128
                    s1b = _ap(S1, o1, [[SP, 128], [128, 2], [1, 4], [0, 128]])
                    s2b = _ap(S2, o2, [[SP, 128], [128, 2], [0, 4], [1, 128]])
                    so = _ap(sm[k], 0, [[1024, 128], [512, 2], [128, 4], [1, 128]])
                    P.op("pool" if hp % 2 == 1 else "dve", K("tensor_tensor", out=so, in0=s1b, in1=s2b, op=ALU.add),
                         reads=["S1", "S2"], writes=["sum%d" % k])

                def e_exp(hp):
                    k = ks[hp]
                    for hh in range(2):
                        h = 2 * hp + hh
                        P.op("act", K("activation", out=gt[k][:, hh, :], in_=sm[k][:, hh, :], func=AF.Exp,
                                      bias=NLSE[:, tt, h:h + 1]), reads=["sum%d" % k, "NLSE"], writes=["gate%d" % k])

                def e_stt(hp):
                    k = ks[hp]
                    for hh in range(2):
                        h = 2 * hp + hh
                        P.op("dve", K("scalar_tensor_tensor", out=Gh[:, h, :], in0=sm[k][:, hh, :], scalar=TAU[:, tt, h:h + 1],
                                      in1=gt[k][:, hh, :], op0=ALU.is_ge, op1=ALU.mult),
                             reads=["sum%d" % k, "gate%d" % k, "TAU"], writes=[ghn])

                e_sum(0)
                for hp in range(4):
                    e_exp(hp)
                    if hp + 1 < 4:
                        e_sum(hp + 1)
                    e_stt(hp)
                gb = 6 + j % 2
                for c4 in range(4):
                    for h in range(8):
                        P.op("pe", K("matmul", B[gb][:, c4 * 128:(c4 + 1) * 128], lhsT=Gh[:, h, c4 * 128:(c4 + 1) * 128],
                                     rhs=self.idb[:], start=(h == 0), stop=(h == 7)), reads=[ghn, "idb"], writes=["B%d" % gb])

            def stage_cp(u, j):
                gb = 6 + j % 2
                P.op("act", K("copy", out=GTs[u % 2][:, :, j * 128:(j + 1) * 128], in_=B[gb][:].rearrange("p (a b) -> p a b", b=128)),
                     reads=["B%d" % gb], writes=["GTs%d" % (u % 2)])

            def stage_bc_pe(u, c4):
                cg, tb = u // 2, u % 2
                wi = cg % 2
                k = c4 % 2
                hb_, hbn = B[k], "B%d" % k
                for dc in range(8):
                    P.op("pe", K("matmul", hb_[:], lhsT=wd[wi][:, dc, c4 * 128:(c4 + 1) * 128],
                                 rhs=xn2T[:, dc, tb * 512:(tb + 1) * 512], start=(dc == 0), stop=(dc == 7)),
                         reads=["wd%d" % wi, "xn2T"], writes=[hbn])

            def stage_bc_act(u, c4):
                k = c4 % 2
                P.op("act", K("activation", out=gel[k][:], in_=B[k][:], func=AF.Gelu), reads=["B%d" % k], writes=["gel%d" % k])

            def stage_bc_dve(u, c4):
                k = c4 % 2
                P.op("dve", K("tensor_tensor", out=AT[0][:, c4, :], in0=gel[k][:], in1=GTs[u % 2][:, c4, :], op=ALU.mult),
                     reads=["gel%d" % k, "GTs%d" % (u % 2)], writes=["AT"])

            ybank = {}

            def stage_bu_pe(u, j):
                cg = u // 2
                wi = cg % 2
                for half in range(2):
                    by = 3 + cnt["y"] % 3
                    cnt["y"] += 1
                    ybank[(u, j, half)] = by
                    for c4 in range(4):
                        P.op("pe", K("matmul", B[by][:], lhsT=AT[0][:, c4, j * 128:(j + 1) * 128],
                                     rhs=wu[wi][:, c4, half * 512:(half + 1) * 512], start=(c4 == 0), stop=(c4 == 3)),
                             reads=["AT", "wu%d" % wi], writes=["B%d" % by])

            def stage_bu_dve(u, j):
                tt = (u % 2) * 4 + j
                for half in range(2):
                    by = ybank[(u, j, half)]
                    hsl = self.h1[:, tt, half * 512:(half + 1) * 512]
                    P.op("dve", K("tensor_tensor", out=hsl, in0=hsl, in1=B[by][:], op=ALU.add),
                         reads=["h1_%d" % tt, "B%d" % by], writes=["h1_%d" % tt])

            _ms = getattr(self, "maxsub", 99)
            ncg = NCGRP if _ms >= 31 else (4 if _ms == 30 else max(0, _ms - 26))
            nu = 2 * ncg
            wfetch(0)
            if nu:
                for j in range(4):
                    stage_a(0, j)
                    if j:
                        stage_cp(0, j - 1)
            for u in range(nu):
                if u % 2 == 0 and u // 2 + 1 < ncg:
                    wfetch(u // 2 + 1)
                nxt = u + 1 < nu
                A = (lambda j: stage_a(u + 1, j)) if nxt else (lambda j: None)
                CP = (lambda j: stage_cp(u + 1, j)) if nxt else (lambda j: None)
                stage_cp(u, 3)
                A(0)
                stage_bc_pe(u, 0); stage_bc_pe(u, 1)
                A(1)
                stage_bc_act(u, 0); stage_bc_act(u, 1); CP(0)
                stage_bc_dve(u, 0); stage_bc_dve(u, 1)
                stage_bc_pe(u, 2); stage_bc_pe(u, 3)
                A(2)
                stage_bc_act(u, 2); stage_bc_act(u, 3); CP(1)
                stage_bc_dve(u, 2); stage_bc_dve(u, 3)
                stage_bu_pe(u, 0)
                A(3); CP(2)
                stage_bu_dve(u, 0)
                stage_bu_pe(u, 1); stage_bu_dve(u, 1)
                stage_bu_pe(u, 2); stage_bu_dve(u, 2)
                stage_bu_pe(u, 3); stage_bu_dve(u, 3)
            for tt in range(NT):
                hx = self.h1[:, tt, :]
                hn = "h1_%d" % tt
                P.op("act", K("activation", out=ob, in_=hx, func=AF.Square, accum_out=pst[:, 4:5]),
                     reads=[hn], writes=["sum0", "pst"])
                P.op("act", K("activation", out=pst[:, 5:6], in_=pst[:, 4:5], func=AF.Sqrt, scale=1.0 / D, bias=EPS),
                     reads=["pst"], writes=["pst"])
                P.op("dve", K("reciprocal", out=pst[:, 6:7], in_=pst[:, 5:6]), reads=["pst"], writes=["pst"])
                P.op("dve", K("scalar_tensor_tensor", out=ob, in0=hx, scalar=pst[:, 6:7], in1=self.gv[:, 2, :],
                              op0=ALU.mult, op1=ALU.mult), reads=[hn, "pst", "gv"], writes=["sum0"])
                r0 = (g * 8 + tt) * 128
                P.dma("sp", K("dma_start", out=dr["out"].ap()[r0:r0 + 128, :], in_=ob), reads=["sum0"])


def _hgrn_consts():
    u = np.arange(128)[:, None]; t = np.arange(128)[None, :]
    same_chunk = (u // 64) == (t // 64)
    same_sub = (u // 16) == (t // 16)
    trisub = (same_sub & (u <= t)).astype(np.float32)
    tri = (same_chunk & (u <= t)).astype(np.float32)
    mj = []
    for j in range(4):
        m = np.zeros((128, 128), np.float32)
        for c in range(2):
            lo = c * 64 + 16 * j
            for s in range(c * 64, c * 64 + 64):
                if s >= lo:
                    hi = min(s, lo + 15)
                    m[lo:hi + 1, s] = -1.0
                else:
                    m[s + 1:lo, s] = 1.0
        mj.append(m)
    triu = (same_chunk & (u > t)).astype(np.float32)
    cmask = (same_chunk & (u <= t)).astype(np.float32)
    ind = np.stack([(np.arange(128) < 64), (np.arange(128) >= 64)], axis=1).astype(np.float32)
    return np.concatenate([trisub, tri] + mj + [triu, cmask, ind], axis=1)


def _rope_table(pos):
    half = 8
    inv = np.power(np.float32(500000.0), -np.arange(half, dtype=np.float32) * np.float32(2.0) / np.float32(16))
    ang = pos.astype(np.float32)[:, None] * inv[None, :]
    return np.concatenate([np.cos(ang), np.sin(ang)], axis=1).astype(np.float32)


def _attn_mask(first_is_pad):
    i = np.arange(128)[:, None]; j = np.arange(128)[None, :]
    prev = np.where(j > i, 0.0, NEG)
    curm = np.where(j <= i, 0.0, NEG)
    meta = np.zeros((128, 16))
    m1 = np.concatenate([prev, curm, meta], axis=1)
    m0 = m1.copy()
    if first_is_pad:
        m0[:, 0:128] = NEG
    return np.stack([m0, m1], axis=1).reshape(128, 2 * 272).astype(np.float32)


def _pack_weights(w_in, w_up_attn, w_up_rec, w_out):
    wall = np.zeros((NPIECE, 128, 8, 512), np.float32)
    wv = w_in.reshape(8, 128, 4864)
    for gi, (c0, n) in enumerate(CG):
        wall[gi, :, :, :n] = wv[:, :, c0:c0 + n].transpose(1, 0, 2)
    ua = w_up_attn.reshape(8, 64, 1024)
    ur = w_up_rec.reshape(4, 128, 1024)
    wo = w_out.reshape(8, 128, 1024)
    for half in range(2):
        hs = slice(half * 512, (half + 1) * 512)
        wall[P_WUA + half, 0:64, :, :] = ua[:, :, hs].transpose(1, 0, 2)
        wall[P_WUR + half, :, 0:4, :] = ur[:, :, hs].transpose(1, 0, 2)
        wall[P_WO + half, :, :, :] = wo[:, :, hs].transpose(1, 0, 2)
    return wall.reshape(NPIECE, 128, 4096)


_NC_CACHE = {}


def _host_inputs(x, meta_tokens, norm_mix_g, w_in, b_in, attn_sinks, lb_logits, rec_norm_g, w_up_attn, w_up_rec,
                 w_out, norm_ffn_g, peer_w_query, peer_sub_keys, peer_expert_down, peer_expert_up, final_norm_g):
    f = lambda a: np.ascontiguousarray(np.asarray(a, dtype=np.float32))
    x = f(x); meta = f(meta_tokens)
    wall = _pack_weights(f(w_in)[0], f(w_up_attn)[0], f(w_up_rec)[0], f(w_out)[0])
    bias = np.zeros((1, 10, 512), np.float32)
    for gi, (c0, n) in enumerate(CG):
        bias[0, gi, :n] = f(b_in)[0, c0:c0 + n]
    bias = bias.reshape(1, 5120)
    rep = lambda v: np.ascontiguousarray(np.broadcast_to(np.asarray(v, np.float32).reshape(1, -1), (128, np.asarray(v).size)))
    gvec = rep(np.stack([f(norm_mix_g)[0], f(norm_ffn_g)[0], f(final_norm_g)], axis=0))
    hc = _hgrn_consts()
    idn = np.eye(128, dtype=np.float32)
    x0 = np.zeros((128, D), np.float32); x0[0:16] = meta
    cs0 = _rope_table(np.arange(128))
    wq = f(peer_w_query)[0].reshape(8, 128, 1024).transpose(1, 0, 2).reshape(128, 8 * 1024)
    keysT = f(peer_sub_keys)[0].transpose(1, 3, 0, 2).reshape(128, 8 * 128)
    wd = f(peer_expert_down)[0]
    wdt = np.ascontiguousarray(wd.reshape(NCGRP, 512, 8, 128).transpose(0, 3, 2, 1)).reshape(NCGRP, 128, 8 * 512)
    wu = np.ascontiguousarray(f(peer_expert_up)[0].reshape(NCGRP, 4, 128, 1024).transpose(0, 2, 1, 3)).reshape(NCGRP, 128, 4 * 1024)
    shared = dict(gvec=gvec, wall=wall, bias=bias, hc=hc, idn=idn, x0=x0, cs0=cs0, lbl=rep(f(lb_logits)),
                  grec=rep(f(rec_norm_g)), sinks=rep(f(attn_sinks)), wq=np.ascontiguousarray(wq),
                  keysT=np.ascontiguousarray(keysT), wdt=wdt, wu=wu)
    maps = []
    for core in range(NCORES):
        b, s = core // 2, core % 2
        pad_meta = np.concatenate([np.zeros((112, D), np.float32), meta], axis=0)
        xp = np.concatenate([pad_meta, x[b, 0:1920]], axis=0)
        if s == 0:
            xm = np.concatenate([pad_meta, x[b, 0:2048]], axis=0)
            p0 = 0
            valid = np.zeros((128, NPRE + 1), np.float32)
            valid[112:, NPRE] = 1.0
        else:
            xm = x[b, 1920:4096]
            p0 = 16 * 128
            valid = np.ones((128, NPRE + 1), np.float32)
            valid[0:112, 0] = 0.0
        cs = _rope_table(np.arange(p0, p0 + NBLK * 128) - 112)
        m = dict(shared)
        m.update(xm=np.ascontiguousarray(xm), xp=np.ascontiguousarray(xp), cs=cs,
                 amask=_attn_mask(s == 0), valid=valid)
        maps.append(m)
    return maps


def _run(inputs, debug_h1=False):
    key = ("nc", debug_h1)
    if key not in _NC_CACHE:
        _NC_CACHE[key] = Builder(debug_h1=debug_h1).build()
    nc = _NC_CACHE[key]
    maps = _host_inputs(**inputs)
    res = run_bass_kernel_spmd(nc, maps, core_ids=list(range(NCORES)))
    out = np.zeros((4, 4096, D), np.float32)
    for core in range(NCORES):
        b, s = core // 2, core % 2
        out[b, s * 2048:(s + 1) * 2048] = res.results[core]["out"]
    return out


def kernel(**inputs):
    return _run(inputs)
```

```python
import numpy as np
import concourse.bass as bass
import concourse.mybir as mybir
from concourse.bass_utils import run_bass_kernel_spmd
from contextlib import ExitStack

F32 = mybir.dt.float32
BF16 = mybir.dt.bfloat16
AF = mybir.ActivationFunctionType
ALU = mybir.AluOpType
AX = mybir.AxisListType

ENGS = ("sp", "act", "dve", "pool", "pe")
NDMA_SEM = 6

D = 1024
NCORES = 8
NBLK = 17
NPRE = 16
EPS = 1e-6
NEG = -1e30
CG = [(0, 512), (512, 256), (768, 512), (1280, 512), (1792, 512), (2304, 512),
      (2816, 512), (3328, 512), (3840, 512), (4352, 512)]
G_Q, G_KV, G_RQ, G_RF, G_RI, G_RG, G_GA0, G_GA1, G_GR0, G_GR1 = range(10)
P_WUA, P_WUR, P_WO = 10, 12, 14
NPIECE = 16
NCGRP = 32
TH = 1024


class Prog:
    def __init__(self, nc):
        self.nc = nc
        self.plan = {e: [] for e in ENGS}
        self.count = {e: 0 for e in ENGS}
        self.seen = {e: {} for e in ENGS}
        self.lastw = {}
        self.readers = {}
        self.dma_n = {e: 0 for e in ENGS}
        self.ninst = 0

    def _need(self, eng, waits, tok, same_ok=False):
        key, val, peng = tok
        if peng == eng and (same_ok or eng == "pe"):
            return
        if self.seen[eng].get(key, 0) >= val:
            return
        self.seen[eng][key] = val
        waits.append((key, val))

    def _deps(self, eng, reads, writes):
        waits = []
        for b in reads:
            if b in self.lastw:
                self._need(eng, waits, self.lastw[b])
            if b[0] == "B" and b[1:].isdigit():
                for tok in self.readers.get(b, ()):
                    self._need(eng, waits, tok, same_ok=True)
        for b in writes:
            if b in self.lastw:
                self._need(eng, waits, self.lastw[b])
            for tok in self.readers.get(b, ()):
                self._need(eng, waits, tok)
        return waits

    def _commit(self, tok, reads, writes):
        for b in reads:
            self.readers.setdefault(b, []).append(tok)
        for b in writes:
            self.lastw[b] = tok
            self.readers[b] = []

    def op(self, eng, fn, reads=(), writes=()):
        waits = self._deps(eng, reads, writes)
        self.count[eng] += 1
        tok = ("c_" + eng, self.count[eng], eng)
        self.plan[eng].append((waits, fn, tok[0], 1))
        self._commit(tok, reads, writes)
        self.ninst += 1

    def dma(self, eng, fn, reads=(), writes=()):
        i = self.dma_n[eng]
        self.dma_n[eng] += 1
        slot = i % NDMA_SEM
        key = "d_%s_%d" % (eng, slot)
        val = 16 * (i // NDMA_SEM + 1)
        waits = self._deps(eng, reads, writes)
        if val > 16:
            self._need(eng, waits, (key, val - 16, "dma"))
        tok = (key, val, "dma")
        self.plan[eng].append((waits, fn, key, 16))
        self._commit(tok, reads, writes)
        self.ninst += 1

    def _all_tokens(self):
        toks = []
        for q in ENGS:
            n = self.dma_n[q]
            for slot in range(min(n, NDMA_SEM)):
                uses = (n - 1 - slot) // NDMA_SEM + 1
                toks.append(("d_%s_%d" % (q, slot), 16 * uses, "dma"))
        for e in ENGS:
            if self.count[e]:
                toks.append(("c_" + e, self.count[e], "x"))
        return toks

    def barrier(self):
        toks = self._all_tokens()
        for e in ENGS:
            waits = []
            for t in toks:
                self._need(e, waits, t)
            if waits:
                self.plan[e].append((waits, None, None, 0))
        self.lastw.clear()
        self.readers.clear()

    def finish(self, eng="sp"):
        waits = []
        for t in self._all_tokens():
            self._need(eng, waits, t)
        self.plan[eng].append((waits, None, None, 0))

    def emit(self):
        nc = self.nc
        keys = set()
        marked = {e: set() for e in ENGS}
        for e in ENGS:
            for waits, fn, k, amt in self.plan[e]:
                if k:
                    keys.add(k)
                for wk, wv in waits:
                    keys.add(wk)
                    if wk.startswith("c_"):
                        marked[wk[2:]].add(wv)
        rank = {}
        for e in ENGS:
            for i, v in enumerate(sorted(marked[e])):
                rank[("c_" + e, v)] = i + 1
        with ExitStack() as es:
            sems = {k: es.enter_context(nc.semaphore(k)) for k in sorted(keys)}
            block = es.enter_context(nc.Block())
            deco = {"sp": block.sync, "act": block.scalar, "dve": block.vector,
                    "pool": block.gpsimd, "pe": block.tensor}

            def make(e):
                def body(h):
                    idx = 0
                    for waits, fn, k, amt in self.plan[e]:
                        for wk, wv in waits:
                            h.wait_ge(sems[wk], rank[(wk, wv)] if wk.startswith("c_") else wv)
                        if fn is not None:
                            ins = fn(h)
                            if amt == 16:
                                ins.then_inc(sems[k], 16)
                            else:
                                idx += 1
                                if idx in marked[e]:
                                    ins.then_inc(sems[k], 1)
                return body

            for e in ENGS:
                if self.plan[e]:
                    deco[e](make(e))


def K(name, *args, **kw):
    return lambda e: getattr(e, name)(*args, **kw)


def _ap(t, off, pat):
    return bass.AP(t, off, [list(p) for p in pat])


class Builder:
    def __init__(self, debug_h1=False):
        self.debug_h1 = debug_h1
        self.nc = bass.Bass("TRN2", target_bir_lowering=False)
        self.P = Prog(self.nc)
        self.dr = {}

    def dram(self, name, shape, dtype=F32, kind="ExternalInput"):
        t = self.nc.dram_tensor(name, list(shape), dtype, kind=kind)
        self.dr[name] = t
        return t

    def declare(self):
        d = self.dram
        d("xm", [NBLK * 128, D]); d("xp", [NPRE * 128, D]); d("x0", [128, D])
        d("gvec", [128, 3 * D])
        d("wall", [NPIECE, 128, 4096])
        d("bias", [1, 10 * 512])
        d("cs", [NBLK * 128, 16]); d("cs0", [128, 16])
        d("amask", [128, 2 * 272])
        d("valid", [128, NPRE + 1])
        d("hc", [128, 768 + 128 + 128 + 2])
        d("idn", [128, 128])
        d("lbl", [128, 1024]); d("grec", [128, 512]); d("sinks", [128, 8])
        d("wq", [128, 8 * 1024]); d("keysT", [128, 8 * 128])
        d("wdt", [NCGRP, 128, 8 * 512]); d("wu", [NCGRP, 128, 4 * 1024])
        d("out", [2048, D], kind="ExternalOutput")
        d("wsc", [NPIECE, 128, 4096], BF16, kind="Internal")
        d("wdb", [NCGRP, 128, 4096], BF16, kind="Internal")
        d("wub", [NCGRP, 128, 4096], BF16, kind="Internal")

    def build(self, maxstep=10 ** 9):
        nc, P = self.nc, self.P
        self.declare()
        self.step = 0

        def go():
            self.step += 1
            return self.step <= maxstep

        with ExitStack() as es:
            self.es = es
            self.alloc_persistent(es)
            self.load_consts()
            if go():
                self.precast_weights()
            for g in range(1 if getattr(Builder, 'only_g0', False) else 2):
                with ExitStack() as ea:
                    self.alloc_mixer(ea)
                    if g == 0:
                        self.precast_experts(ea)
                    if g == 0:
                        if go():
                            self.meta_kv()
                        for pb in range(NPRE):
                            if go():
                                self.precast_step()
                                self.pre_block(pb)
                        lbs = range(0, 9)
                    else:
                        lbs = range(9, 17)
                    for lb in lbs:
                        if go():
                            if g == 0:
                                self.precast_step()
                            self.main_block(lb, (lb - 1) % 8 if lb >= 1 else None)
                    if g == 0:
                        while self.xpos < len(self.xq) or self.xpend:
                            self.precast_step()
                    P.barrier()
                with ExitStack() as eb:
                    if self.debug_h1:
                        if go():
                            for tt in range(8):
                                r0 = (g * 8 + tt) * 128
                                P.dma("sp", K("dma_start",
                                    out=self.dr["out"].ap()[r0:r0 + 128, :], in_=self.h1[:, tt, :]),
                                    reads=["h1_%d" % tt])
                    else:
                        if go():
                            self.peer(eb, g)
                    P.barrier()
            P.finish("sp")
            with nc.allow_low_precision(reason="bf16 gate matrix: at most a few non-zero terms per cell, it is a bf16 matmul operand"):
                P.emit()
        return nc

    def sb(self, es, name, shape, dt=F32):
        self.uid = getattr(self, "uid", 0) + 1
        return es.enter_context(self.nc.sbuf_tensor("s%d_%s" % (self.uid, name), list(shape), dt))

    def alloc_persistent(self, es):
        sb = lambda n, s, d=F32: self.sb(es, n, s, d)
        nc = self.nc
        self.h1 = sb("h1", [128, 8, D])
        self.idf = sb("idf", [128, 128]); self.idb = sb("idb", [128, 128], BF16)
        self.gv = sb("gv", [128, 3, D])
        self.KT = sb("KT", [64, 2, 256], BF16)
        self.Vs = sb("Vs", [128, 2, 128], BF16)
        self.KTm = sb("KTm", [64, 2, 16], BF16); self.Vm = sb("Vm", [128, 128], BF16)
        self.S = sb("S", [128, 4, 128]); self.Sb = sb("Sb", [128, 2, 4, 128], BF16)
        self.ones = sb("ones", [128, 128], BF16)
        self.B = [es.enter_context(nc.psum_tensor("B%d" % i, [128, 512], F32)) for i in (0, 1, 3, 4, 5, 6, 7)]
        self.B.insert(2, es.enter_context(nc.psum_tensor("B2", [128, 1024], BF16)))

    def load_consts(self):
        P, dr = self.P, self.dr
        P.dma("sp", K("dma_start", out=self.idf[:], in_=dr["idn"].ap()), writes=["idf"])
        P.op("dve", K("tensor_copy", out=self.idb[:], in_=self.idf[:]), reads=["idf"], writes=["idb"])
        gsrc = dr["gvec"].ap()
        P.dma("sp", K("dma_start", out=self.gv[:].rearrange("p a b -> p (a b)"), in_=gsrc), writes=["gv"])
        P.op("dve", K("memset", self.ones[:], 0.0), writes=["ones"])
        P.op("dve", K("memset", self.ones[0:1, :], 1.0), writes=["ones"])
        P.op("dve", K("memset", self.S[:], 0.0), writes=["S"])
        P.op("pool", K("memset", self.Sb[:], 0.0), writes=["Sb0", "Sb1"])
        P.op("pool", K("memset", self.Vm[:], 0.0), writes=["Vm"])

    def precast_weights(self):
        P, dr = self.P, self.dr
        with ExitStack() as es:
            st = [self.sb(es, "pcst%d" % i, [128, 4096], BF16) for i in range(2)]
            for p in range(NPIECE):
                s = st[p % 2]
                nm = "pcst%d" % (p % 2)
                P.dma("pool", K("dma_start",
                    out=s[:].rearrange("p (a b) -> p a b", b=512),
                    in_=dr["wall"].ap()[p].rearrange("p (a b) -> p a b", b=512)), writes=[nm])
                P.dma("sp", K("dma_start", out=dr["wsc"].ap()[p], in_=s[:]),
                      reads=[nm], writes=["wsc%d" % p])
            P.barrier()

    def precast_experts(self, es):
        self.xst = [self.sb(es, "xcst%d" % i, [128, 4096], BF16) for i in range(3)]
        self.xq = [(cg, src, dst) for cg in range(NCGRP) for src, dst in (("wdt", "wdb"), ("wu", "wub"))]
        self.xpos = 0
        self.xpend = []

    def precast_step(self, n=3, flush=False):
        P, dr = self.P, self.dr
        for (t, nm, cg, dst) in self.xpend:
            P.dma("sp", K("dma_start", out=dr[dst].ap()[cg], in_=t[:]), reads=[nm], writes=["%s%d" % (dst, cg)])
        self.xpend = []
        if flush:
            n = len(self.xq) - self.xpos if False else 0
        for _ in range(n):
            if self.xpos >= len(self.xq):
                break
            cg, src, dst = self.xq[self.xpos]
            t, nm = self.xst[self.xpos % 3], "xcst%d" % (self.xpos % 3)
            self.xpos += 1
            P.dma("pool", K("dma_start", out=t[:].rearrange("p (a b) -> p a b", b=512),
                            in_=dr[src].ap()[cg].rearrange("p (a b) -> p a b", b=512)), writes=[nm])
            self.xpend.append((t, nm, cg, dst))

    def alloc_mixer(self, es):
        sb = lambda n, s, d=F32: self.sb(es, n, s, d)
        self.wbuf = [sb("wbuf%d" % i, [128, 8, 512], BF16) for i in range(3)]
        self.wslot = 0
        self.biasb = sb("biasb", [128, 5120], BF16)
        self.xs = sb("xs", [128, D]); self.junk = sb("junk", [128, D], BF16)
        self.st = sb("st", [128, 16])
        self.xnb = sb("xnb", [128, D], BF16); self.xT = sb("xT", [128, 8, 128], BF16)
        self.qf = sb("qf", [128, 8, 64]); self.qb = sb("qb", [128, 8, 64], BF16)
        self.kf = sb("kf", [128, 2, 64]); self.kb = sb("kb", [128, 2, 64], BF16)
        self.rt = sb("rt", [128, 4, 8, 8])
        self.qT = sb("qT", [64, 8, 128], BF16)
        self.sc = [sb("sc%d" % i, [128, 272]) for i in range(2)]
        self.pf = [sb("pf%d" % i, [128, 272], BF16) for i in range(2)]
        self.pn = [sb("pn%d" % i, [128, 272], BF16) for i in range(2)]
        self.pT = [sb("pT%d" % i, [128, 3, 128], BF16) for i in range(2)]
        self.sm = [sb("sm%d" % i, [128, 8]) for i in range(2)]
        self.attnT = sb("attnT", [64, 8, 128], BF16)
        self.rqb = sb("rqb", [128, 512], BF16)
        self.sg = sb("sg", [128, 512]); self.sgn = sb("sgn", [128, 512])
        self.lf = sb("lf", [128, 512]); self.kr = sb("kr", [128, 512])
        self.krb = sb("krb", [128, 512], BF16); self.vb = sb("vb", [128, 512], BF16)
        self.gs = sb("gs", [128, 512])
        self.sga = sb("sga", [128, D], BF16); self.sgr = sb("sgr", [128, D], BF16)
        self.E = sb("E", [128, 768]); self.dec = sb("dec", [128, 4, 2])
        self.Qp = sb("Qp", [128, 128], BF16); self.Qz = sb("Qz", [128, 2, 128], BF16)
        self.Kj = sb("Kj", [128, 4, 128], BF16); self.ATb = sb("ATb", [128, 128], BF16)
        self.ekd = sb("ekd", [128, 512]); self.kdec = sb("kdec", [128, 512], BF16)
        self.ms = sb("ms", [128, 8])
        self.recb = sb("recb", [128, 512], BF16); self.recT = sb("recT", [128, 4, 128], BF16)
        self.mg = sb("mg", [128, D]); self.mgb = sb("mgb", [128, D], BF16)
        self.mT = sb("mT", [128, 8, 128], BF16)
        self.lbr = sb("lbr", [128, 512]); self.oml = sb("oml", [128, 512])
        self.ll = sb("ll", [128, 2, 512]); self.grr = sb("grr", [128, 512])
        self.snk = sb("snk", [128, 8])
        self.hc = sb("hc", [128, 1026]); self.cmb = sb("cmb", [128, 128], BF16)
        self.am = sb("am", [128, 2, 272]); self.cs = sb("cs", [128, NBLK, 16]); self.cs0 = sb("cs0", [128, 16])
        self.vld = sb("vld", [128, NPRE + 1])
        P, dr = self.P, self.dr
        ld = lambda dst, src, nm: P.dma("sp", K("dma_start", out=dst, in_=src), writes=[nm])
        ld(self.hc[:], dr["hc"].ap(), "hc")
        ld(self.am[:], dr["amask"].ap().rearrange("p (a b) -> p a b", b=272), "am")
        ld(self.cs[:], dr["cs"].ap().rearrange("(n p) c -> p n c", p=128), "cs")
        ld(self.cs0[:], dr["cs0"].ap(), "cs0")
        ld(self.vld[:], dr["valid"].ap(), "vld")
        ld(self.ll[:], dr["lbl"].ap().rearrange("p (a b) -> p a b", b=512), "ll")
        ld(self.grr[:], dr["grec"].ap(), "grr")
        ld(self.snk[:], dr["sinks"].ap(), "snk")
        P.op("pool", K("memset", self.biasb[:], 0.0), writes=["biasb"])
        P.dma("pool", K("dma_start", out=self.biasb[0:1, :].rearrange("p (a b) -> p a b", b=512),
                                            in_=dr["bias"].ap().rearrange("p (a b) -> p a b", b=512)), writes=["biasb"])
        P.op("dve", K("tensor_tensor", out=self.lf[:], in0=self.ll[:, 0, :], in1=self.ll[:, 1, :], op=ALU.subtract),
             reads=["ll"], writes=["lf"])
        P.op("act", K("activation", out=self.lbr[:], in_=self.lf[:], func=AF.Sigmoid), reads=["lf"], writes=["lbr"])
        P.op("act", K("activation", out=self.oml[:], in_=self.lf[:], func=AF.Sigmoid, scale=-1.0),
             reads=["lf"], writes=["oml"])
        P.op("dve", K("tensor_copy", out=self.cmb[:], in_=self.hc[:, 896:1024]), reads=["hc"], writes=["cmb"])
        P.op("pool", K("memset", self.Qz[:], 0.0), writes=["Qz"])
        for i in range(2):
            P.op("pool", K("memset", self.pT[i][:], 0.0), writes=["pT%d" % i])

    def wload(self, piece):
        i = self.wslot % 3
        self.wslot += 1
        t, nm = self.wbuf[i], "wbuf%d" % i
        self.P.dma("sp", K("dma_start",
            out=t[:], in_=self.dr["wsc"].ap()[piece].rearrange("p (a b) -> p a b", b=512)),
            reads=["wsc%d" % piece], writes=[nm])
        return t, nm

    def norm_transpose(self, src_ap, gidx):
        P = self.P
        P.dma("act", K("dma_start", out=self.xs[:], in_=src_ap), writes=["xs"])
        st = self.st
        P.op("act", K("activation", out=self.junk[:], in_=self.xs[:], func=AF.Square, accum_out=st[:, 0:1]),
             reads=["xs"], writes=["junk", "st"])
        P.op("act", K("activation", out=st[:, 1:2], in_=st[:, 0:1], func=AF.Sqrt, scale=1.0 / D, bias=EPS),
             reads=["st"], writes=["st"])
        P.op("dve", K("reciprocal", out=st[:, 2:3], in_=st[:, 1:2]), reads=["st"], writes=["st"])
        P.op("dve", K("scalar_tensor_tensor", out=self.xnb[:], in0=self.xs[:], scalar=st[:, 2:3],
                                                     in1=self.gv[:, gidx, :], op0=ALU.mult, op1=ALU.mult),
             reads=["xs", "st", "gv"], writes=["xnb"])
        self.transpose8(self.xnb, "xnb", self.xT, "xT")

    def transpose8(self, src, sname, dst, dname):
        P, B2 = self.P, self.B[2]
        for c in range(8):
            P.op("pe", K("transpose", out=B2[:, c * 128:(c + 1) * 128], in_=src[:, c * 128:(c + 1) * 128],
                                                  identity=self.idb[:]), reads=[sname, "idb"], writes=["B2"])
        P.op("act", K("copy", out=dst[:], in_=B2[:].rearrange("p (a b) -> p a b", b=128)),
             reads=["B2"], writes=[dname])

    def project(self, grp, bank):
        P = self.P
        c0, n = CG[grp]
        w, wn = self.wload(grp)
        pb, bn = self.B[bank], "B%d" % bank
        for dc in range(8):
            P.op("pe", K("matmul", pb[:, 0:n], lhsT=self.xT[:, dc, :], rhs=w[:, dc, 0:n],
                                                 start=(dc == 0), stop=False), reads=["xT", wn], writes=[bn])
        P.op("pe", K("matmul", pb[:, 0:n], lhsT=self.ones[:, :], rhs=self.biasb[:, grp * 512:grp * 512 + n],
                                      start=False, stop=True), reads=["ones", "biasb"], writes=[bn])
        return pb, bn

    def rope(self, srcf, dstb, nh, cs_ap, names):
        P = self.P
        sn, dn = names
        cs_t, cs_off = cs_ap.tensor, cs_ap.offset
        pstep = cs_ap.ap[0][0]
        cosb = _ap(cs_t, cs_off, [[pstep, 128], [0, nh], [1, 8]])
        sinb = _ap(cs_t, cs_off + 8, [[pstep, 128], [0, nh], [1, 8]])
        t1, t2 = srcf[:, :, 0:8], srcf[:, :, 8:16]
        rt = self.rt
        P.op("dve", K("tensor_copy", out=dstb[:], in_=srcf[:]), reads=[sn], writes=[dn])
        P.op("dve", K("tensor_tensor", out=rt[:, 0, 0:nh, :], in0=t1, in1=cosb, op=ALU.mult), reads=[sn, "cs", "cs0"], writes=["rt"])
        P.op("dve", K("tensor_tensor", out=rt[:, 1, 0:nh, :], in0=t2, in1=sinb, op=ALU.mult), reads=[sn, "cs", "cs0"], writes=["rt"])
        P.op("dve", K("tensor_tensor", out=rt[:, 2, 0:nh, :], in0=t2, in1=cosb, op=ALU.mult), reads=[sn, "cs", "cs0"], writes=["rt"])
        P.op("dve", K("tensor_tensor", out=rt[:, 3, 0:nh, :], in0=t1, in1=sinb, op=ALU.mult), reads=[sn, "cs", "cs0"], writes=["rt"])
        P.op("dve", K("tensor_tensor", out=dstb[:, :, 0:8], in0=rt[:, 0, 0:nh, :], in1=rt[:, 1, 0:nh, :], op=ALU.subtract),
             reads=["rt"], writes=[dn])
        P.op("dve", K("tensor_tensor", out=dstb[:, :, 8:16], in0=rt[:, 2, 0:nh, :], in1=rt[:, 3, 0:nh, :], op=ALU.add),
             reads=["rt"], writes=[dn])

    def kv_from_bank(self, pb, bn, cs_ap, vdst, vname):
        P = self.P
        P.op("act", K("copy", out=self.kf[:], in_=pb[:, 0:128].rearrange("p (a b) -> p a b", b=64)),
             reads=[bn], writes=["kf"])
        P.op("act", K("copy", out=vdst, in_=pb[:, 128:256]), reads=[bn], writes=[vname])
        self.rope(self.kf, self.kb, 2, cs_ap, ("kf", "kb"))

    def meta_kv(self):
        P, B2 = self.P, self.B[2]
        self.norm_transpose(self.dr["x0"].ap(), 0)
        pb, bn = self.project(G_KV, 0)
        self.kv_from_bank(pb, bn, self.cs0[:, :], self.vb[:, 0:128], "vb")
        P.op("dve", K("tensor_copy", out=self.Vm[0:16, :], in_=self.vb[0:16, 0:128]), reads=["vb"], writes=["Vm"])
        for g in range(2):
            P.op("pe", K("transpose", out=B2[0:64, g * 128:(g + 1) * 128], in_=self.kb[:, g, :],
                                                  identity=self.idb[:]), reads=["kb", "idb"], writes=["B2"])
        P.op("act", K("copy", out=self.KTm[:], in_=B2[0:64, 0:256].rearrange("p (a b) -> p a b", b=128)[:, :, 0:16]),
             reads=["B2"], writes=["KTm"])

    def hgrn_gates(self, vcol):
        P = self.P
        pb, bn = self.project(G_RF, 0)
        P.op("act", K("activation", out=self.sg[:], in_=pb[:], func=AF.Sigmoid), reads=[bn], writes=["sg"])
        P.op("act", K("activation", out=self.sgn[:], in_=pb[:], func=AF.Sigmoid, scale=-1.0), reads=[bn], writes=["sgn"])
        pb1, bn1 = self.project(G_RI, 1)
        P.op("act", K("copy", out=self.vb[:], in_=pb1[:]), reads=[bn1], writes=["vb"])
        P.op("dve", K("tensor_tensor", out=self.sg[:], in0=self.sg[:], in1=self.oml[:], op=ALU.mult),
             reads=["sg", "oml"], writes=["sg"])
        P.op("dve", K("tensor_tensor", out=self.sg[:], in0=self.sg[:], in1=self.lbr[:], op=ALU.add),
             reads=["sg", "lbr"], writes=["sg"])
        P.op("act", K("activation", out=self.lf[:], in_=self.sg[:], func=AF.Ln), reads=["sg"], writes=["lf"])
        P.op("dve", K("tensor_tensor", out=self.kr[:], in0=self.sgn[:], in1=self.oml[:], op=ALU.mult),
             reads=["sgn", "oml"], writes=["kr"])
        if vcol is not None:
            vs = self.vld[:, vcol:vcol + 1]
            P.op("dve", K("tensor_scalar", out=self.lf[:], in0=self.lf[:], scalar1=vs, scalar2=None, op0=ALU.mult),
                 reads=["lf", "vld"], writes=["lf"])
            P.op("dve", K("tensor_scalar", out=self.kr[:], in0=self.kr[:], scalar1=vs, scalar2=None, op0=ALU.mult),
                 reads=["kr", "vld"], writes=["kr"])

    def hgrn_kdec(self):
        P = self.P
        pX = self.B[7]
        P.op("pe", K("matmul", pX[:], lhsT=self.hc[:, 768:896], rhs=self.lf[:], start=True, stop=True),
             reads=["hc", "lf"], writes=["B7"])
        P.op("act", K("activation", out=self.ekd[:], in_=pX[:], func=AF.Exp), reads=["B7"], writes=["ekd"])
        P.op("dve", K("tensor_tensor", out=self.kdec[:], in0=self.ekd[:], in1=self.kr[:], op=ALU.mult),
             reads=["ekd", "kr"], writes=["kdec"])

    def hgrn_state_update(self, h, c, sb_dst):
        P = self.P
        pd = self.B[5]
        r0 = c * 64
        P.op("pe", K("matmul", pd[:, 128:256], lhsT=self.kdec[r0:r0 + 64, h * 128:(h + 1) * 128],
                                      rhs=self.vb[r0:r0 + 64, h * 128:(h + 1) * 128], start=True, stop=True),
             reads=["kdec", "vb"], writes=["B5"])
        P.op("dve", K("scalar_tensor_tensor", out=self.S[:, h, :], in0=self.S[:, h, :], scalar=self.dec[:, h, c:c + 1],
                                                     in1=pd[:, 128:256], op0=ALU.mult, op1=ALU.add),
             reads=["S", "dec", "B5"], writes=["S"])
        if sb_dst is not None:
            P.op("act", K("copy", out=self.Sb[:, sb_dst, h, :], in_=self.S[:, h, :]),
                 reads=["S"], writes=["Sb%d" % sb_dst])

    def pre_block(self, pb_i):
        P = self.P
        sub = getattr(self, "maxsub", 99)
        self.norm_transpose(self.dr["xp"].ap()[pb_i * 128:(pb_i + 1) * 128, :], 0)
        if sub < 1:
            return
        self.hgrn_gates(pb_i)
        if sub < 2:
            return
        self.hgrn_kdec()
        if sub < 3:
            return
        pE = self.B[3]
        for h in range(4):
            P.op("pe", K("matmul", pE[:, 2 * h:2 * h + 2], lhsT=self.lf[:, h * 128:(h + 1) * 128],
                                               rhs=self.hc[:, 1024:1026], start=True, stop=True),
                 reads=["lf", "hc"], writes=["B3"])
        P.op("act", K("activation", out=self.dec[:], in_=pE[:, 0:8].rearrange("p (a b) -> p a b", b=2), func=AF.Exp),
             reads=["B3"], writes=["dec"])
        if sub < 4:
            return
        last = pb_i == NPRE - 1
        for c in range(2):
            if sub < 5 and c == 1:
                return
            for h in range(4):
                self.hgrn_state_update(h, c, 0 if (last and c == 1) else None)

    def main_block(self, lb, tt):
        P, B = self.P, self.B
        do_out = lb >= 1
        cs_ap = self.cs[:, lb, :]
        self.norm_transpose(self.dr["xm"].ap()[lb * 128:(lb + 1) * 128, :], 0)
        cur = lb % 2
        if lb >= 1:
            P.op("dve", K("tensor_copy", out=self.KT[:, :, 0:128], in_=self.KT[:, :, 128:256]),
                 reads=["KT"], writes=["KT"])
        pb, bn = self.project(G_KV, 0)
        self.kv_from_bank(pb, bn, cs_ap, self.Vs[:, cur, :], "Vs%d" % cur)
        for g in range(2):
            P.op("pe", K("transpose", out=B[2][0:64, g * 128:(g + 1) * 128], in_=self.kb[:, g, :],
                                                  identity=self.idb[:]), reads=["kb", "idb"], writes=["B2"])
        P.op("act", K("copy", out=self.KT[:, :, 128:256], in_=B[2][0:64, 0:256].rearrange("p (a b) -> p a b", b=128)),
             reads=["B2"], writes=["KT"])
        sub = getattr(self, "maxsub", 99)
        if do_out and sub >= 11:
            self.attention(lb, cs_ap)
        if sub >= 13 or not do_out:
            self.hgrn(lb, do_out)
        if do_out and sub >= 14:
            self.merge_out(lb, tt)

    def attention(self, lb, cs_ap):
        P, B = self.P, self.B
        pb, bn = self.project(G_Q, 1)
        P.op("act", K("copy", out=self.qf[:], in_=pb[:].rearrange("p (a b) -> p a b", b=64)), reads=[bn], writes=["qf"])
        self.rope(self.qf, self.qb, 8, cs_ap, ("qf", "qb"))
        for h in range(8):
            P.op("pe", K("transpose", out=B[2][0:64, h * 128:(h + 1) * 128], in_=self.qb[:, h, :],
                                                  identity=self.idb[:]), reads=["qb", "idb"], writes=["B2"])
        P.op("act", K("copy", out=self.qT[:], in_=B[2][0:64, :].rearrange("p (a b) -> p a b", b=128)),
             reads=["B2"], writes=["qT"])
        mi = 0 if lb == 1 else 1
        prv, cur = (lb - 1) % 2, lb % 2
        if getattr(self, "maxsub", 99) < 12:
            return
        for h in range(8):
            g, i = h // 4, h % 2
            pS, sn = B[3 + i], "B%d" % (3 + i)
            sc, pf, pn, pT, sm = self.sc[i], self.pf[i], self.pn[i], self.pT[i], self.sm[i]
            scn, pfn, pnn, pTn, smn = "sc%d" % i, "pf%d" % i, "pn%d" % i, "pT%d" % i, "sm%d" % i
            P.op("pe", K("matmul", pS[:, 0:256], lhsT=self.qT[:, h, :], rhs=self.KT[:, g, :],
                                                           start=True, stop=True), reads=["qT", "KT"], writes=[sn])
            P.op("pe", K("matmul", pS[:, 256:272], lhsT=self.qT[:, h, :], rhs=self.KTm[:, g, :],
                                                           start=True, stop=True), reads=["qT", "KTm"], writes=[sn])
            P.op("dve", K("scalar_tensor_tensor", out=sc[:], in0=pS[:, 0:272], scalar=0.125,
                                                                       in1=self.am[:, mi, :], op0=ALU.mult, op1=ALU.add),
                 reads=[sn, "am"], writes=[scn])
            P.op("dve", K("tensor_reduce", out=sm[:, 0:1], in_=sc[:], axis=AX.X, op=ALU.max),
                 reads=[scn], writes=[smn])
            P.op("dve", K("tensor_tensor", out=sm[:, 1:2], in0=sm[:, 0:1], in1=self.snk[:, h:h + 1], op=ALU.max),
                 reads=[smn, "snk"], writes=[smn])
            P.op("dve", K("tensor_scalar", out=sm[:, 2:3], in0=sm[:, 1:2], scalar1=-1.0, scalar2=None, op0=ALU.mult),
                 reads=[smn], writes=[smn])
            P.op("act", K("activation", out=pf[:], in_=sc[:], func=AF.Exp, bias=sm[:, 2:3],
                                                                   accum_out=sm[:, 3:4]), reads=[scn, smn], writes=[pfn, smn])
            P.op("act", K("activation", out=sm[:, 4:5], in_=self.snk[:, h:h + 1], func=AF.Exp, bias=sm[:, 2:3]),
                 reads=["snk", smn], writes=[smn])
            P.op("dve", K("tensor_tensor", out=sm[:, 5:6], in0=sm[:, 3:4], in1=sm[:, 4:5], op=ALU.add),
                 reads=[smn], writes=[smn])
            P.op("dve", K("reciprocal", out=sm[:, 6:7], in_=sm[:, 5:6]), reads=[smn], writes=[smn])
            P.op("dve", K("tensor_scalar", out=pn[:], in0=pf[:], scalar1=sm[:, 6:7], scalar2=None,
                                                                      op0=ALU.mult), reads=[pfn, smn], writes=[pnn])
            for j, (c0, n) in enumerate(((0, 128), (128, 128), (256, 16))):
                P.op("pe", K("transpose", out=B[2][0:n, j * 128:(j + 1) * 128], in_=pn[:, c0:c0 + n],
                                                                          identity=self.idb[:]), reads=[pnn, "idb"], writes=["B2"])
            P.op("act", K("copy", out=pT[:, 0:2, :], in_=B[2][:, 0:256].rearrange("p (a b) -> p a b", b=128)),
                 reads=["B2"], writes=[pTn])
            P.op("act", K("copy", out=pT[0:16, 2, :], in_=B[2][0:16, 256:384]), reads=["B2"], writes=[pTn])
            po = B[5][0:64, 256:384]
            gs_ = slice(g * 64, (g + 1) * 64)
            P.op("pe", K("matmul", po, lhsT=self.Vs[:, prv, gs_], rhs=pT[:, 0, :], start=True, stop=False),
                 reads=["Vs%d" % prv, pTn], writes=["B5"])
            P.op("pe", K("matmul", po, lhsT=self.Vs[:, cur, gs_], rhs=pT[:, 1, :], start=False, stop=False),
                 reads=["Vs%d" % cur, pTn], writes=["B5"])
            P.op("pe", K("matmul", po, lhsT=self.Vm[:, gs_], rhs=pT[:, 2, :], start=False, stop=True),
                 reads=["Vm", pTn], writes=["B5"])
            P.op("act", K("copy", out=self.attnT[:, h, :], in_=po), reads=["B5"], writes=["attnT"])

    def hgrn(self, lb, do_out):
        P, B = self.P, self.B
        self.hgrn_gates(NPRE if lb == 0 else None)
        self.hgrn_kdec()
        if not do_out:
            pE = B[3]
            for h in range(4):
                P.op("pe", K("matmul", pE[:, 2 * h:2 * h + 2], lhsT=self.lf[:, h * 128:(h + 1) * 128],
                                                   rhs=self.hc[:, 1024:1026], start=True, stop=True),
                     reads=["lf", "hc"], writes=["B3"])
            P.op("act", K("activation", out=self.dec[:], in_=pE[:, 0:8].rearrange("p (a b) -> p a b", b=2), func=AF.Exp),
                 reads=["B3"], writes=["dec"])
            for c in range(2):
                for h in range(4):
                    self.hgrn_state_update(h, c, 0 if c == 1 else None)
            return
        pb, bn = self.project(G_RQ, 0)
        P.op("act", K("copy", out=self.rqb[:], in_=pb[:]), reads=[bn], writes=["rqb"])
        pb, bn = self.project(G_RG, 1)
        P.op("act", K("activation", out=self.gs[:], in_=pb[:], func=AF.Silu), reads=[bn], writes=["gs"])
        P.op("dve", K("tensor_tensor", out=self.gs[:], in0=self.gs[:], in1=self.grr[:], op=ALU.mult),
             reads=["gs", "grr"], writes=["gs"])
        P.op("dve", K("tensor_copy", out=self.krb[:], in_=self.kr[:]), reads=["kr"], writes=["krb"])
        pO = B[6]
        E = self.E
        for h in range(4):
            hs = slice(h * 128, (h + 1) * 128)
            P.op("pe", K("matmul", B[3][:], lhsT=self.lf[:, hs], rhs=self.hc[:, 0:512], start=True, stop=True),
                 reads=["lf", "hc"], writes=["B3"])
            P.op("pe", K("matmul", B[4][:, 0:256], lhsT=self.lf[:, hs], rhs=self.hc[:, 512:768], start=True, stop=True),
                 reads=["lf", "hc"], writes=["B4"])
            P.op("act", K("activation", out=E[:, 0:512], in_=B[3][:], func=AF.Exp), reads=["B3"], writes=["E"])
            P.op("act", K("activation", out=E[:, 512:768], in_=B[4][:, 0:256], func=AF.Exp), reads=["B4"], writes=["E"])
            P.op("dve", K("tensor_copy", out=self.dec[:, h, 0:1], in_=E[:, 191:192]), reads=["E"], writes=["dec"])
            P.op("dve", K("tensor_copy", out=self.dec[:, h, 1:2], in_=E[:, 255:256]), reads=["E"], writes=["dec"])
            P.op("pe", K("transpose", out=B[2][:, 0:128], in_=self.rqb[:, hs], identity=self.idb[:]),
                 reads=["rqb", "idb"], writes=["B2"])
            P.op("pe", K("transpose", out=B[2][:, 128:256], in_=self.krb[:, hs], identity=self.idb[:]),
                 reads=["krb", "idb"], writes=["B2"])
            qTp, kTp = B[2][:, 0:128], B[2][:, 128:256]
            P.op("dve", K("tensor_tensor", out=self.Qp[:], in0=E[:, 0:128], in1=qTp, op=ALU.mult),
                 reads=["E", "B2"], writes=["Qp"])
            P.op("dve", K("tensor_tensor", out=self.Qz[:, 0, 0:64], in0=E[:, 128:192], in1=B[2][:, 0:64], op=ALU.mult),
                 reads=["E", "B2"], writes=["Qz"])
            P.op("dve", K("tensor_tensor", out=self.Qz[:, 1, 64:128], in0=E[:, 192:256], in1=B[2][:, 64:128], op=ALU.mult),
                 reads=["E", "B2"], writes=["Qz"])
            kTb = _ap(B[2], 128, [[1024, 128], [0, 4], [1, 128]])
            P.op("dve", K("tensor_tensor", out=self.Kj[:], in0=E[:, 256:768].rearrange("p (a b) -> p a b", b=128),
                                                  in1=kTb, op=ALU.mult), reads=["E", "B2"], writes=["Kj"])
            pA = B[5][:, 0:128]
            for j in range(4):
                for c in range(2):
                    t0 = c * 64 + 16 * j
                    P.op("pe", K("matmul", B[5][:, t0:t0 + 16], lhsT=self.Kj[:, j, :], rhs=self.Qp[:, t0:t0 + 16],
                                                              start=True, stop=True), reads=["Kj", "Qp"], writes=["B5"])
            P.op("dve", K("tensor_tensor", out=self.ATb[:], in0=pA, in1=self.cmb[:], op=ALU.mult),
                 reads=["B5", "cmb"], writes=["ATb"])
            P.op("pe", K("matmul", pO[:, hs], lhsT=self.ATb[:], rhs=self.vb[:, hs], start=True, stop=False),
                 reads=["ATb", "vb"], writes=["B6"])
            P.op("pe", K("matmul", pO[:, hs], lhsT=self.Qz[:, 0, :], rhs=self.Sb[:, 0, h, :], start=False, stop=False),
                 reads=["Qz", "Sb0"], writes=["B6"])
            self.hgrn_state_update(h, 0, 1)
            P.op("pe", K("matmul", pO[:, hs], lhsT=self.Qz[:, 1, :], rhs=self.Sb[:, 1, h, :], start=False, stop=True),
                 reads=["Qz", "Sb1"], writes=["B6"])
            self.hgrn_state_update(h, 1, 0)
        ms = self.ms
        for h in range(4):
            hs = slice(h * 128, (h + 1) * 128)
            P.op("act", K("activation", out=self.junk[:, hs], in_=pO[:, hs], func=AF.Square, accum_out=ms[:, h:h + 1]),
                 reads=["B6"], writes=["junk", "ms"])
        P.op("act", K("activation", out=ms[:, 4:8], in_=ms[:, 0:4], func=AF.Sqrt, scale=1.0 / 128, bias=EPS),
             reads=["ms"], writes=["ms"])
        P.op("dve", K("reciprocal", out=ms[:, 0:4], in_=ms[:, 4:8]), reads=["ms"], writes=["ms"])
        for h in range(4):
            hs = slice(h * 128, (h + 1) * 128)
            P.op("dve", K("scalar_tensor_tensor", out=self.recb[:, hs], in0=pO[:, hs], scalar=ms[:, h:h + 1],
                                                                     in1=self.gs[:, hs], op0=ALU.mult, op1=ALU.mult),
                 reads=["B6", "ms", "gs"], writes=["recb"])
        for c in range(4):
            P.op("pe", K("transpose", out=B[2][:, c * 128:(c + 1) * 128], in_=self.recb[:, c * 128:(c + 1) * 128],
                                                  identity=self.idb[:]), reads=["recb", "idb"], writes=["B2"])
        P.op("act", K("copy", out=self.recT[:], in_=B[2][:, 0:512].rearrange("p (a b) -> p a b", b=128)),
             reads=["B2"], writes=["recT"])

    def merge_out(self, lb, tt):
        P, B = self.P, self.B
        for i, (grp, dst, dn) in enumerate(((G_GA0, self.sga[:, 0:512], "sga"), (G_GA1, self.sga[:, 512:1024], "sga"),
                                            (G_GR0, self.sgr[:, 0:512], "sgr"), (G_GR1, self.sgr[:, 512:1024], "sgr"))):
            pb, bn = self.project(grp, i % 2)
            P.op("act", K("activation", out=dst, in_=pb[:], func=AF.Sigmoid), reads=[bn], writes=[dn])
        for half in range(2):
            w, wn = self.wload(P_WUA + half)
            pU, un = B[6 + half], "B%d" % (6 + half)
            for h in range(8):
                P.op("pe", K("matmul", pU[:], lhsT=self.attnT[:, h, :], rhs=w[0:64, h, :],
                                                               start=(h == 0), stop=(h == 7)), reads=["attnT", wn], writes=[un])
            hs = slice(half * 512, (half + 1) * 512)
            P.op("dve", K("tensor_tensor", out=self.mg[:, hs], in0=self.sga[:, hs], in1=pU[:], op=ALU.mult),
                 reads=["sga", un], writes=["mg"])
        for half in range(2):
            w, wn = self.wload(P_WUR + half)
            pU, un = B[6 + half], "B%d" % (6 + half)
            for c in range(4):
                P.op("pe", K("matmul", pU[:], lhsT=self.recT[:, c, :], rhs=w[:, c, :],
                                                               start=(c == 0), stop=(c == 3)), reads=["recT", wn], writes=[un])
            hs = slice(half * 512, (half + 1) * 512)
            P.op("dve", K("tensor_tensor", out=self.junk[:, hs], in0=self.sgr[:, hs], in1=pU[:], op=ALU.mult),
                 reads=["sgr", un], writes=["junk"])
            P.op("dve", K("tensor_tensor", out=self.mgb[:, hs], in0=self.mg[:, hs], in1=self.junk[:, hs], op=ALU.add),
                 reads=["mg", "junk"], writes=["mgb"])
        self.transpose8(self.mgb, "mgb", self.mT, "mT")
        for half in range(2):
            w, wn = self.wload(P_WO + half)
            pU, un = B[6 + half], "B%d" % (6 + half)
            for dc in range(8):
                P.op("pe", K("matmul", pU[:], lhsT=self.mT[:, dc, :], rhs=w[:, dc, :],
                                                                 start=(dc == 0), stop=(dc == 7)), reads=["mT", wn], writes=[un])
            hs = slice(half * 512, (half + 1) * 512)
            P.op("dve", K("tensor_tensor", out=self.h1[:, tt, hs], in0=self.xs[:, hs], in1=pU[:], op=ALU.add),
                 reads=["xs", un], writes=["h1_%d" % tt])

    def peer(self, es, g):
        P, B, dr = self.P, self.B, self.dr
        sb = lambda n, s, d=F32: self.sb(es, n, s, d)
        NT = 8
        xn2T = sb("xn2T", [128, 8, 1024], BF16)
        S1 = sb("S1", [128, NT * 8 * 128]); S2 = sb("S2", [128, NT * 8 * 128])
        TAU = sb("TAU", [128, NT, 8]); NLSE = sb("NLSE", [128, NT, 8])
        pst = sb("pst", [128, 16])
        SP = NT * 8 * 128
        with ExitStack() as e1:
            sb1 = lambda n, s, d=F32: self.sb(e1, n, s, d)
            wq = sb1("wq", [128, 8, 1024]); keysT = sb1("keysT", [128, 8, 128])
            xnf = sb1("xnf", [128, D]); xTf = sb1("xTf", [128, 8, 128]); qT = sb1("qT2", [128, 8, 128])
            junk = sb1("junk2", [128, D], BF16)
            wk = sb1("wk", [128, 256]); t16 = sb1("t16", [128, 2, 8, 16])
            cand = sb1("cand", [128, 8, 256]); best = sb1("best", [128, 8, 16]); eb = sb1("eb", [128, 8, 16])
            zz = sb1("zz", [128, 16])
            P.dma("sp", K("dma_start", out=wq[:].rearrange("p a b -> p (a b)"), in_=dr["wq"].ap()), writes=["wq"])
            P.dma("sp", K("dma_start", out=keysT[:].rearrange("p a b -> p (a b)"), in_=dr["keysT"].ap()), writes=["keysT"])
            for tt in range(NT):
                hx = self.h1[:, tt, :]
                hn = "h1_%d" % tt
                if getattr(self, "maxsub", 99) < 19:
                    continue
                P.op("act", K("activation", out=junk[:], in_=hx, func=AF.Square, accum_out=pst[:, 0:1]),
                     reads=[hn], writes=["junk2", "pst"])
                P.op("act", K("activation", out=pst[:, 1:2], in_=pst[:, 0:1], func=AF.Sqrt, scale=1.0 / D, bias=EPS),
                     reads=["pst"], writes=["pst"])
                P.op("dve", K("reciprocal", out=pst[:, 2:3], in_=pst[:, 1:2]), reads=["pst"], writes=["pst"])
                P.op("dve", K("scalar_tensor_tensor", out=xnf[:], in0=hx, scalar=pst[:, 2:3], in1=self.gv[:, 1, :],
                              op0=ALU.mult, op1=ALU.mult), reads=[hn, "pst", "gv"], writes=["xnf"])
                if getattr(self, "maxsub", 99) < 20:
                    continue
                for c in range(8):
                    bk = c // 4
                    P.op("pe", K("matmul", B[bk][:, (c % 4) * 128:(c % 4 + 1) * 128], lhsT=xnf[:, c * 128:(c + 1) * 128],
                                 rhs=self.idf[:], start=True, stop=True), reads=["xnf", "idf"], writes=["B%d" % bk])
                if getattr(self, "maxsub", 99) < 21:
                    continue
                for bk in range(2):
                    src = B[bk][:].rearrange("p (a b) -> p a b", b=128)
                    P.op("act", K("copy", out=xTf[:, bk * 4:bk * 4 + 4, :], in_=src), reads=["B%d" % bk], writes=["xTf"])
                    P.op("dve", K("tensor_copy", out=xn2T[:, bk * 4:bk * 4 + 4, tt * 128:(tt + 1) * 128], in_=src),
                         reads=["B%d" % bk], writes=["xn2T"])
                if getattr(self, "maxsub", 99) < 22:
                    continue
                for h in range(8):
                    bk = 3 + h // 4
                    for dc in range(8):
                        P.op("pe", K("matmul", B[bk][:, (h % 4) * 128:(h % 4 + 1) * 128], lhsT=wq[:, dc, h * 128:(h + 1) * 128],
                                     rhs=xTf[:, dc, :], start=(dc == 0), stop=(dc == 7)), reads=["wq", "xTf"], writes=["B%d" % bk])
                for i in range(2):
                    P.op("act", K("copy", out=qT[:, i * 4:i * 4 + 4, :], in_=B[3 + i][:].rearrange("p (a b) -> p a b", b=128)),
                         reads=["B%d" % (3 + i)], writes=["qT2"])
                if getattr(self, "maxsub", 99) < 23:
                    continue
                for p, Sx, sn in ((0, S1, "S1"), (1, S2, "S2")):
                    for h in range(8):
                        bk = 5 + h // 4
                        P.op("pe", K("matmul", B[bk][:, (h % 4) * 128:(h % 4 + 1) * 128], lhsT=qT[p * 64:(p + 1) * 64, h, :],
                                     rhs=keysT[p * 64:(p + 1) * 64, h, :], start=True, stop=True),
                             reads=["qT2", "keysT"], writes=["B%d" % bk])
                    for i in range(2):
                        o0 = (tt * 8 + i * 4) * 128
                        P.op("act", K("copy", out=Sx[:, o0:o0 + 512], in_=B[5 + i][:]), reads=["B%d" % (5 + i)], writes=[sn])
                if getattr(self, "maxsub", 99) < 24:
                    continue
                for p, Sx, sn in ((0, S1, "S1"), (1, S2, "S2")):
                    for h in range(8):
                        o0 = (tt * 8 + h) * 128
                        src = Sx[:, o0:o0 + 128]
                        P.op("dve", K("max", out=t16[:, p, h, 0:8], in_=src), reads=[sn], writes=["t16"])
                        P.op("dve", K("match_replace", out=wk[:, 0:128], in_to_replace=t16[:, p, h, 0:8], in_values=src,
                                      imm_value=NEG), reads=[sn, "t16"], writes=["wk"])
                        P.op("dve", K("max", out=t16[:, p, h, 8:16], in_=wk[:, 0:128]), reads=["wk"], writes=["t16"])
                if getattr(self, "maxsub", 99) < 25:
                    continue
                a0 = _ap(t16, 0, [[256, 128], [16, 8], [1, 16], [0, 16]])
                a1 = _ap(t16, 128, [[256, 128], [16, 8], [0, 16], [1, 16]])
                co = _ap(cand, 0, [[2048, 128], [256, 8], [16, 16], [1, 16]])
                P.op("dve", K("tensor_tensor", out=co, in0=a0, in1=a1, op=ALU.add), reads=["t16"], writes=["cand"])
                for h in range(8):
                    P.op("dve", K("max", out=best[:, h, 0:8], in_=cand[:, h, :]), reads=["cand"], writes=["best"])
                    P.op("dve", K("match_replace", out=wk[:, 0:256], in_to_replace=best[:, h, 0:8], in_values=cand[:, h, :],
                                  imm_value=NEG), reads=["cand", "best"], writes=["wk"])
                    P.op("dve", K("max", out=best[:, h, 8:16], in_=wk[:, 0:256]), reads=["wk"], writes=["best"])
                P.op("dve", K("tensor_copy", out=TAU[:, tt, :], in_=best[:, :, 15]), reads=["best"], writes=["TAU"])
                bm = _ap(best, 0, [[128, 128], [16, 8], [0, 16]])
                P.op("dve", K("tensor_tensor", out=eb[:], in0=best[:], in1=bm, op=ALU.subtract), reads=["best"], writes=["eb"])
                P.op("act", K("activation", out=eb[:], in_=eb[:], func=AF.Exp), reads=["eb"], writes=["eb"])
                P.op("dve", K("tensor_reduce", out=zz[:, 0:8], in_=eb[:], axis=AX.X, op=ALU.add), reads=["eb"], writes=["zz"])
                P.op("act", K("activation", out=zz[:, 8:16], in_=zz[:, 0:8], func=AF.Ln), reads=["zz"], writes=["zz"])
                P.op("dve", K("scalar_tensor_tensor", out=NLSE[:, tt, :], in0=zz[:, 8:16], scalar=-1.0, in1=best[:, :, 0],
                              op0=ALU.mult, op1=ALU.subtract), reads=["zz", "best"], writes=["NLSE"])
            P.barrier()
        with ExitStack() as e2:
            sb2 = lambda n, s, d=F32: self.sb(e2, n, s, d)
            wd = [sb2("wd%d" % i, [128, 8, 512], BF16) for i in range(2)]
            wu = [sb2("wu%d" % i, [128, 4, 1024], BF16) for i in range(2)]
            sm = [sb2("sum%d" % i, [128, 2, 512]) for i in range(2)]
            gt = [sb2("gate%d" % i, [128, 2, 512], BF16) for i in range(2)]
            Ghs = [sb2("Gh%d" % i, [128, 8, 512], BF16) for i in range(2)]
            GTs = [sb2("GTs%d" % i, [128, 4, 512], BF16) for i in range(2)]
            gel = [sb2("gel%d" % i, [128, 512], BF16) for i in range(2)]
            AT = [sb2("AT", [128, 4, 512], BF16)] * 2
            ob = _ap(sm[0], 0, [[1024, 128], [1, 1024]])
            SUM_ENG = "pool"
            cnt = {"s": 0, "y": 0}

            def wfetch(cg):
                i = cg % 2
                P.dma("sp", K("dma_start", out=wd[i][:].rearrange("p a b -> p (a b)"), in_=dr["wdb"].ap()[cg]),
                      reads=["wdb%d" % cg], writes=["wd%d" % i])
                P.dma("sp", K("dma_start", out=wu[i][:].rearrange("p a b -> p (a b)"), in_=dr["wub"].ap()[cg]),
                      reads=["wub%d" % cg], writes=["wu%d" % i])

            def stage_a(u, j):
                cg, tb = u // 2, u % 2
                tt = tb * 4 + j
                gq = (u * 4 + j) % 2
                Gh, ghn = Ghs[gq], "Gh%d" % gq
                ks = []
                for hp in range(4):
                    ks.append(cnt["s"] % 2)
                    cnt["s"] += 1

                def e_sum(hp):
                    k = ks[hp]
                    o1 = (tt * 8 + 2 * hp) * 128 + 4 * cg
                    o2 = (tt * 8 + 2 * hp) * 128
                    s1b = _ap(S1, o1, [[SP, 128], [128, 2], [1, 4], [0, 128]])
                    s2b = _ap(S2, o2, [[SP, 128], [128, 2], [0, 4], [1, 128]])
                    so = _ap(sm[k], 0, [[1024, 128], [512, 2], [128, 4], [1, 128]])
                    P.op("pool", K("tensor_tensor", out=so, in0=s1b, in1=s2b, op=ALU.add),
                         reads=["S1", "S2"], writes=["sum%d" % k])

                def e_exp(hp):
                    k = ks[hp]
                    for hh in range(2):
                        h = 2 * hp + hh
                        P.op("act", K("activation", out=gt[k][:, hh, :], in_=sm[k][:, hh, :], func=AF.Exp,
                                      bias=NLSE[:, tt, h:h + 1]), reads=["sum%d" % k, "NLSE"], writes=["gate%d" % k])

                def e_stt(hp):
                    k = ks[hp]
                    for hh in range(2):
                        h = 2 * hp + hh
                        P.op("dve", K("scalar_tensor_tensor", out=Gh[:, h, :], in0=sm[k][:, hh, :], scalar=TAU[:, tt, h:h + 1],
                                      in1=gt[k][:, hh, :], op0=ALU.is_ge, op1=ALU.mult),
                             reads=["sum%d" % k, "gate%d" % k, "TAU"], writes=[ghn])

                e_sum(0)
                for hp in range(4):
                    e_exp(hp)
                    if hp + 1 < 4:
                        e_sum(hp + 1)
                    e_stt(hp)
                gb = 6 + j % 2
                for c4 in range(4):
                    for h in range(8):
                        P.op("pe", K("matmul", B[gb][:, c4 * 128:(c4 + 1) * 128], lhsT=Gh[:, h, c4 * 128:(c4 + 1) * 128],
                                     rhs=self.idb[:], start=(h == 0), stop=(h == 7)), reads=[ghn, "idb"], writes=["B%d" % gb])

            def stage_cp(u, j):
                gb = 6 + j % 2
                P.op("act", K("copy", out=GTs[u % 2][:, :, j * 128:(j + 1) * 128], in_=B[gb][:].rearrange("p (a b) -> p a b", b=128)),
                     reads=["B%d" % gb], writes=["GTs%d" % (u % 2)])

            def stage_bc_pe(u, c4):
                cg, tb = u // 2, u % 2
                wi = cg % 2
                k = c4 % 2
                hb_, hbn = B[k], "B%d" % k
                for dc in range(8):
                    P.op("pe", K("matmul", hb_[:], lhsT=wd[wi][:, dc, c4 * 128:(c4 + 1) * 128],
                                 rhs=xn2T[:, dc, tb * 512:(tb + 1) * 512], start=(dc == 0), stop=(dc == 7)),
                         reads=["wd%d" % wi, "xn2T"], writes=[hbn])

            def stage_bc_act(u, c4):
                k = c4 % 2
                P.op("act", K("activation", out=gel[k][:], in_=B[k][:], func=AF.Gelu), reads=["B%d" % k], writes=["gel%d" % k])

            def stage_bc_dve(u, c4):
                k = c4 % 2
                P.op("dve", K("tensor_tensor", out=AT[0][:, c4, :], in0=gel[k][:], in1=GTs[u % 2][:, c4, :], op=ALU.mult),
                     reads=["gel%d" % k, "GTs%d" % (u % 2)], writes=["AT"])

            ybank = {}

            def stage_bu_pe(u, j):
                cg = u // 2
                wi = cg % 2
                for half in range(2):
                    by = 3 + cnt["y"] % 3
                    cnt["y"] += 1
                    ybank[(u, j, half)] = by
                    for c4 in range(4):
                        P.op("pe", K("matmul", B[by][:], lhsT=AT[0][:, c4, j * 128:(j + 1) * 128],
                                     rhs=wu[wi][:, c4, half * 512:(half + 1) * 512], start=(c4 == 0), stop=(c4 == 3)),
                             reads=["AT", "wu%d" % wi], writes=["B%d" % by])

            def stage_bu_dve(u, j):
                tt = (u % 2) * 4 + j
                for half in range(2):
                    by = ybank[(u, j, half)]
                    hsl = self.h1[:, tt, half * 512:(half + 1) * 512]
                    P.op("dve", K("tensor_tensor", out=hsl, in0=hsl, in1=B[by][:], op=ALU.add),
                         reads=["h1_%d" % tt, "B%d" % by], writes=["h1_%d" % tt])

            _ms = getattr(self, "maxsub", 99)
            ncg = NCGRP if _ms >= 31 else (4 if _ms == 30 else max(0, _ms - 26))
            nu = 2 * ncg
            wfetch(0)
            if nu:
                for j in range(4):
                    stage_a(0, j)
                    if j:
                        stage_cp(0, j - 1)
            for u in range(nu):
                if u % 2 == 0 and u // 2 + 1 < ncg:
                    wfetch(u // 2 + 1)
                nxt = u + 1 < nu
                A = (lambda j: stage_a(u + 1, j)) if nxt else (lambda j: None)
                CP = (lambda j: stage_cp(u + 1, j)) if nxt else (lambda j: None)
                stage_cp(u, 3)
                A(0)
                stage_bc_pe(u, 0); stage_bc_pe(u, 1)
                A(1)
                stage_bc_act(u, 0); stage_bc_act(u, 1); CP(0)
                stage_bc_dve(u, 0); stage_bc_dve(u, 1)
                stage_bc_pe(u, 2); stage_bc_pe(u, 3)
                A(2)
                stage_bc_act(u, 2); stage_bc_act(u, 3); CP(1)
                stage_bc_dve(u, 2); stage_bc_dve(u, 3)
                stage_bu_pe(u, 0)
                A(3); CP(2)
                stage_bu_dve(u, 0)
                stage_bu_pe(u, 1); stage_bu_dve(u, 1)
                stage_bu_pe(u, 2); stage_bu_dve(u, 2)
                stage_bu_pe(u, 3); stage_bu_dve(u, 3)
            for tt in range(NT):
                hx = self.h1[:, tt, :]
                hn = "h1_%d" % tt
                P.op("act", K("activation", out=ob, in_=hx, func=AF.Square, accum_out=pst[:, 4:5]),
                     reads=[hn], writes=["sum0", "pst"])
                P.op("act", K("activation", out=pst[:, 5:6], in_=pst[:, 4:5], func=AF.Sqrt, scale=1.0 / D, bias=EPS),
                     reads=["pst"], writes=["pst"])
                P.op("dve", K("reciprocal", out=pst[:, 6:7], in_=pst[:, 5:6]), reads=["pst"], writes=["pst"])
                P.op("dve", K("scalar_tensor_tensor", out=ob, in0=hx, scalar=pst[:, 6:7], in1=self.gv[:, 2, :],
                              op0=ALU.mult, op1=ALU.mult), reads=[hn, "pst", "gv"], writes=["sum0"])
                r0 = (g * 8 + tt) * 128
                P.dma("sp", K("dma_start", out=dr["out"].ap()[r0:r0 + 128, :], in_=ob), reads=["sum0"])


def _hgrn_consts():
    u = np.arange(128)[:, None]; t = np.arange(128)[None, :]
    same_chunk = (u // 64) == (t // 64)
    same_sub = (u // 16) == (t // 16)
    trisub = (same_sub & (u <= t)).astype(np.float32)
    tri = (same_chunk & (u <= t)).astype(np.float32)
    mj = []
    for j in range(4):
        m = np.zeros((128, 128), np.float32)
        for c in range(2):
            lo = c * 64 + 16 * j
            for s in range(c * 64, c * 64 + 64):
                if s >= lo:
                    hi = min(s, lo + 15)
                    m[lo:hi + 1, s] = -1.0
                else:
                    m[s + 1:lo, s] = 1.0
        mj.append(m)
    triu = (same_chunk & (u > t)).astype(np.float32)
    cmask = (same_chunk & (u <= t)).astype(np.float32)
    ind = np.stack([(np.arange(128) < 64), (np.arange(128) >= 64)], axis=1).astype(np.float32)
    return np.concatenate([trisub, tri] + mj + [triu, cmask, ind], axis=1)


def _rope_table(pos):
    half = 8
    inv = np.power(np.float32(500000.0), -np.arange(half, dtype=np.float32) * np.float32(2.0) / np.float32(16))
    ang = pos.astype(np.float32)[:, None] * inv[None, :]
    return np.concatenate([np.cos(ang), np.sin(ang)], axis=1).astype(np.float32)


def _attn_mask(first_is_pad):
    i = np.arange(128)[:, None]; j = np.arange(128)[None, :]
    prev = np.where(j > i, 0.0, NEG)
    curm = np.where(j <= i, 0.0, NEG)
    meta = np.zeros((128, 16))
    m1 = np.concatenate([prev, curm, meta], axis=1)
    m0 = m1.copy()
    if first_is_pad:
        m0[:, 0:128] = NEG
    return np.stack([m0, m1], axis=1).reshape(128, 2 * 272).astype(np.float32)


def _pack_weights(w_in, w_up_attn, w_up_rec, w_out):
    wall = np.zeros((NPIECE, 128, 8, 512), np.float32)
    wv = w_in.reshape(8, 128, 4864)
    for gi, (c0, n) in enumerate(CG):
        wall[gi, :, :, :n] = wv[:, :, c0:c0 + n].transpose(1, 0, 2)
    ua = w_up_attn.reshape(8, 64, 1024)
    ur = w_up_rec.reshape(4, 128, 1024)
    wo = w_out.reshape(8, 128, 1024)
    for half in range(2):
        hs = slice(half * 512, (half + 1) * 512)
        wall[P_WUA + half, 0:64, :, :] = ua[:, :, hs].transpose(1, 0, 2)
        wall[P_WUR + half, :, 0:4, :] = ur[:, :, hs].transpose(1, 0, 2)
        wall[P_WO + half, :, :, :] = wo[:, :, hs].transpose(1, 0, 2)
    return wall.reshape(NPIECE, 128, 4096)


_NC_CACHE = {}


def _host_inputs(x, meta_tokens, norm_mix_g, w_in, b_in, attn_sinks, lb_logits, rec_norm_g, w_up_attn, w_up_rec,
                 w_out, norm_ffn_g, peer_w_query, peer_sub_keys, peer_expert_down, peer_expert_up, final_norm_g):
    f = lambda a: np.ascontiguousarray(np.asarray(a, dtype=np.float32))
    x = f(x); meta = f(meta_tokens)
    wall = _pack_weights(f(w_in)[0], f(w_up_attn)[0], f(w_up_rec)[0], f(w_out)[0])
    bias = np.zeros((1, 10, 512), np.float32)
    for gi, (c0, n) in enumerate(CG):
        bias[0, gi, :n] = f(b_in)[0, c0:c0 + n]
    bias = bias.reshape(1, 5120)
    rep = lambda v: np.ascontiguousarray(np.broadcast_to(np.asarray(v, np.float32).reshape(1, -1), (128, np.asarray(v).size)))
    gvec = rep(np.stack([f(norm_mix_g)[0], f(norm_ffn_g)[0], f(final_norm_g)], axis=0))
    hc = _hgrn_consts()
    idn = np.eye(128, dtype=np.float32)
    x0 = np.zeros((128, D), np.float32); x0[0:16] = meta
    cs0 = _rope_table(np.arange(128))
    wq = f(peer_w_query)[0].reshape(8, 128, 1024).transpose(1, 0, 2).reshape(128, 8 * 1024)
    keysT = f(peer_sub_keys)[0].transpose(1, 3, 0, 2).reshape(128, 8 * 128)
    wd = f(peer_expert_down)[0]
    wdt = np.ascontiguousarray(wd.reshape(NCGRP, 512, 8, 128).transpose(0, 3, 2, 1)).reshape(NCGRP, 128, 8 * 512)
    wu = np.ascontiguousarray(f(peer_expert_up)[0].reshape(NCGRP, 4, 128, 1024).transpose(0, 2, 1, 3)).reshape(NCGRP, 128, 4 * 1024)
    shared = dict(gvec=gvec, wall=wall, bias=bias, hc=hc, idn=idn, x0=x0, cs0=cs0, lbl=rep(f(lb_logits)),
                  grec=rep(f(rec_norm_g)), sinks=rep(f(attn_sinks)), wq=np.ascontiguousarray(wq),
                  keysT=np.ascontiguousarray(keysT), wdt=wdt, wu=wu)
    maps = []
    for core in range(NCORES):
        b, s = core // 2, core % 2
        pad_meta = np.concatenate([np.zeros((112, D), np.float32), meta], axis=0)
        xp = np.concatenate([pad_meta, x[b, 0:1920]], axis=0)
        if s == 0:
            xm = np.concatenate([pad_meta, x[b, 0:2048]], axis=0)
            p0 = 0
            valid = np.zeros((128, NPRE + 1), np.float32)
            valid[112:, NPRE] = 1.0
        else:
            xm = x[b, 1920:4096]
            p0 = 16 * 128
            valid = np.ones((128, NPRE + 1), np.float32)
            valid[0:112, 0] = 0.0
        cs = _rope_table(np.arange(p0, p0 + NBLK * 128) - 112)
        m = dict(shared)
        m.update(xm=np.ascontiguousarray(xm), xp=np.ascontiguousarray(xp), cs=cs,
                 amask=_attn_mask(s == 0), valid=valid)
        maps.append(m)
    return maps


def _run(inputs, debug_h1=False):
    key = ("nc", debug_h1)
    if key not in _NC_CACHE:
        _NC_CACHE[key] = Builder(debug_h1=debug_h1).build()
    nc = _NC_CACHE[key]
    maps = _host_inputs(**inputs)
    res = run_bass_kernel_spmd(nc, maps, core_ids=list(range(NCORES)))
    out = np.zeros((4, 4096, D), np.float32)
    for core in range(NCORES):
        b, s = core // 2, core % 2
        out[b, s * 2048:(s + 1) * 2048] = res.results[core]["out"]
    return out


def kernel(**inputs):
    return _run(inputs)
```
